# Optimizing a Trainium2 kernel written in Bass

```python
import math
import jax, jax.numpy as jnp
from jax import lax
import numpy as np

D_MODEL = 1024
BATCH = 8
SEQ = 4096
DEPTH = 2

PLE_DIM = 256
GDN_HEADS = 8
GDN_HEAD_DIM = 128
GDN_WIDTH = GDN_HEADS * GDN_HEAD_DIM
GDN_CHUNK = 64
CONV_WIDTH = 4
LRU_WIDTH = D_MODEL
LRU_BLOCKS = 8
LRU_BLOCK_DIM = LRU_WIDTH // LRU_BLOCKS
LRU_C = 8.0
SWA_Q_HEADS = 16
SWA_KV_HEADS = 4
SWA_HEAD_DIM = 64
SWA_GROUP = SWA_Q_HEADS // SWA_KV_HEADS
SWA_Q_WIDTH = SWA_Q_HEADS * SWA_HEAD_DIM
SWA_KV_WIDTH = SWA_KV_HEADS * SWA_HEAD_DIM
WINDOW = 128
REL_BUCKETS = 32
REL_MAX_DISTANCE = 128
N_EXPERTS = 32
TOP_K = 4
D_EXPERT = D_MODEL
SWIGLU_LIMIT = 7.0
SWIGLU_ALPHA = 1.702
EXPERT_BLOCK = 128
N_BRANCHES = 3
IN_SPLITS = (GDN_WIDTH, GDN_WIDTH, GDN_WIDTH, GDN_WIDTH, GDN_HEADS, GDN_HEADS,
             LRU_WIDTH, LRU_WIDTH,
             SWA_Q_WIDTH, SWA_KV_WIDTH, SWA_KV_WIDTH,
             N_BRANCHES * D_MODEL)
IN_COLS = sum(IN_SPLITS)
DEEPNORM_ALPHA = (2.0 * DEPTH) ** 0.25
DEEPNORM_BETA = (8.0 * DEPTH) ** -0.25
LN_EPS = 1e-5
NORM_EPS = 1e-6

kernel_name = "hybrid_gdn_rglru_swa_moe_deepnorm"


def layer_norm(x, g, b):
    x32 = x.astype(jnp.float32)
    mu = jnp.mean(x32, axis=-1, keepdims=True)
    var = jnp.mean(jnp.square(x32 - mu), axis=-1, keepdims=True)
    return ((x32 - mu) * lax.rsqrt(var + LN_EPS) * g + b).astype(x.dtype)


def l2norm(t):
    return t * lax.rsqrt(jnp.sum(jnp.square(t), axis=-1, keepdims=True) + NORM_EPS)


def split_cols(t, sizes):
    offsets = []
    acc = 0
    for s in sizes[:-1]:
        acc += s
        offsets.append(acc)
    return jnp.split(t, offsets, axis=-1)


def causal_dwconv(x, w):
    K, S = w.shape[0], x.shape[1]
    xp = jnp.pad(x, ((0, 0), (K - 1, 0), (0, 0)))
    return sum(xp[:, j:j + S] * w[j] for j in range(K))


def gated_delta_rule_chunked(q, k, v, g, beta):
    B, S, H, dk = q.shape
    dv = v.shape[-1]
    C = GDN_CHUNK
    N = S // C

    def chunks(t):
        return jnp.moveaxis(t.reshape((B, N, C, H) + t.shape[3:]), 3, 1)

    q = chunks(q * (dk ** -0.5))
    k, v, g, beta = chunks(k), chunks(v), chunks(g), chunks(beta)
    G = jnp.cumsum(g, axis=-1)
    idx = jnp.arange(C)
    causal = idx[:, None] >= idx[None, :]
    strict = idx[:, None] > idx[None, :]
    decay = jnp.exp(jnp.where(causal, G[..., :, None] - G[..., None, :], -jnp.inf))
    kb = k * beta[..., None]
    L = jnp.where(strict, jnp.einsum('bhnid,bhnjd->bhnij', kb, k) * decay, 0.0)
    eye = jnp.eye(C, dtype=L.dtype)
    T = lax.linalg.triangular_solve(eye + L, jnp.broadcast_to(eye, L.shape), left_side=True, lower=True)
    u = T @ (v * beta[..., None])
    w = T @ (kb * jnp.exp(G)[..., None])
    intra = jnp.einsum('bhnid,bhnjd->bhnij', q, k) * decay
    q_dec = q * jnp.exp(G)[..., None]
    G_last = G[..., -1:]
    k_dec = k * jnp.exp(G_last - G)[..., None]
    chunk_decay = jnp.exp(G_last[..., 0])
    xs = tuple(jnp.moveaxis(t, 2, 0) for t in (w, u, q_dec, k_dec, intra, chunk_decay))

    def step(state, inp):
        w_n, u_n, qd, kd, a_n, cd = inp
        v_new = u_n - jnp.einsum('bhcd,bhde->bhce', w_n, state)
        o = jnp.einsum('bhcd,bhde->bhce', qd, state) + jnp.einsum('bhij,bhje->bhie', a_n, v_new)
        state = state * cd[..., None, None] + jnp.einsum('bhcd,bhce->bhde', kd, v_new)
        return state, o

    _, o = lax.scan(step, jnp.zeros((B, H, dk, dv), q.dtype), xs)
    return jnp.transpose(o, (1, 0, 3, 2, 4)).reshape(B, S, H, dv)


def gdn_branch(q, k, v, z, a, b, conv_w, a_log, dt_bias, norm_w):
    B, S, _ = q.shape
    qkv = jax.nn.silu(causal_dwconv(jnp.concatenate([q, k, v], axis=-1), conv_w))
    q, k, v = jnp.split(qkv, 3, axis=-1)

    def heads(t):
        return t.reshape(B, S, GDN_HEADS, GDN_HEAD_DIM).astype(jnp.float32)

    qh, kh, vh = l2norm(heads(q)), l2norm(heads(k)), heads(v)
    g = -jnp.exp(a_log.astype(jnp.float32)) * jax.nn.softplus(a.astype(jnp.float32) + dt_bias)
    beta = jax.nn.sigmoid(b.astype(jnp.float32))
    o = gated_delta_rule_chunked(qh, kh, vh, g, beta)
    o = o * lax.rsqrt(jnp.mean(jnp.square(o), axis=-1, keepdims=True) + NORM_EPS) * norm_w * jax.nn.silu(heads(z))
    return o.reshape(B, S, GDN_WIDTH).astype(q.dtype)


def lru_combine(c1, c2):
    a1, b1 = c1
    a2, b2 = c2
    return a1 * a2, a2 * b1 + b2


def lru_branch(xb, gate, conv_w, conv_b, w_a, b_a, w_x, b_x, lam):
    B, S, _ = xb.shape
    xc = (causal_dwconv(xb, conv_w) + conv_b).astype(jnp.float32)
    blocks = xc.reshape(B, S, LRU_BLOCKS, LRU_BLOCK_DIM)
    r = jax.nn.sigmoid(jnp.einsum('bshi,hij->bshj', blocks, w_a).reshape(B, S, LRU_WIDTH) + b_a)
    i = jax.nn.sigmoid(jnp.einsum('bshi,hij->bshj', blocks, w_x).reshape(B, S, LRU_WIDTH) + b_x)
    log_a = -LRU_C * r * jax.nn.softplus(-lam.astype(jnp.float32))
    a = jnp.exp(log_a)
    u = jnp.sqrt(-jnp.expm1(2.0 * log_a)) * (i * xc)
    _, h = lax.associative_scan(lru_combine, (a, u), axis=1)
    return (h * jax.nn.gelu(gate.astype(jnp.float32))).astype(xb.dtype)


def t5_bucket(dist):
    max_exact = REL_BUCKETS // 2
    d = jnp.maximum(dist.astype(jnp.float32), 1.0)
    large = max_exact + (jnp.log(d / max_exact) / math.log(REL_MAX_DISTANCE / max_exact)
                         * (REL_BUCKETS - max_exact)).astype(jnp.int32)
    large = jnp.minimum(large, REL_BUCKETS - 1)
    return jnp.where(dist < max_exact, dist, large)


def swa_branch(q, k, v, sinks, rel_bias):
    B, S, _ = q.shape
    NB = S // WINDOW
    qb = q.reshape(B, NB, WINDOW, SWA_KV_HEADS, SWA_GROUP, SWA_HEAD_DIM)

    def band(t):
        t = t.reshape(B, NB, WINDOW, SWA_KV_HEADS, SWA_HEAD_DIM)
        prev = jnp.pad(t, ((0, 0), (1, 0), (0, 0), (0, 0), (0, 0)))[:, :-1]
        return jnp.concatenate([prev, t], axis=2)

    kb, vb = band(k), band(v)
    scores = jnp.einsum('bnqhgd,bnshd->bnhgqs', qb, kb,
                        preferred_element_type=jnp.float32) * (SWA_HEAD_DIM ** -0.5)
    kj = jnp.arange(2 * WINDOW)[None, :]
    dist = (jnp.arange(WINDOW)[:, None] + WINDOW) - kj
    in_window = (dist >= 0) & (dist < WINDOW)
    bias = rel_bias[t5_bucket(jnp.maximum(dist, 0))].astype(jnp.float32)
    bias = jnp.transpose(bias, (2, 0, 1)).reshape(SWA_KV_HEADS, SWA_GROUP, WINDOW, 2 * WINDOW)
    key_exists = (jnp.arange(NB)[:, None] * WINDOW + kj - WINDOW) >= 0
    mask = in_window[None] & key_exists[:, None, :]
    scores = jnp.where(mask[None, :, None, None], scores + bias, -jnp.inf)
    sink = sinks.astype(jnp.float32).reshape(SWA_KV_HEADS, SWA_GROUP)[None, None, :, :, None, None]
    m = jnp.maximum(jnp.max(scores, axis=-1, keepdims=True), sink)
    pexp = jnp.exp(scores - m)
    probs = pexp / (jnp.sum(pexp, axis=-1, keepdims=True) + jnp.exp(sink - m))
    out = jnp.einsum('bnhgqs,bnshd->bnqhgd', probs.astype(v.dtype), vb)
    return out.reshape(B, S, SWA_Q_WIDTH)


def token_mixer(h, w_in, conv_qkv_w, gdn_a_log, gdn_dt_bias, gdn_norm_w,
                rg_conv_w, rg_conv_b, rg_w_a, rg_b_a, rg_w_x, rg_b_x, rg_lambda,
                attn_sinks, rel_bias, w_o_gdn, w_o_lru, w_o_swa, w_out):
    proj = h @ w_in
    (qa, ka, va, za, aa, ba, xl, gl, qc, kc, vc, gate_logits) = split_cols(proj, IN_SPLITS)
    y_a = gdn_branch(qa, ka, va, za, aa, ba, conv_qkv_w, gdn_a_log, gdn_dt_bias, gdn_norm_w) @ w_o_gdn
    y_b = lru_branch(xl, gl, rg_conv_w, rg_conv_b, rg_w_a, rg_b_a, rg_w_x, rg_b_x, rg_lambda) @ w_o_lru
    y_c = swa_branch(qc, kc, vc, attn_sinks, rel_bias) @ w_o_swa
    g_a, g_b, g_c = jnp.split(jax.nn.sigmoid(gate_logits), N_BRANCHES, axis=-1)
    return (g_a * y_a + g_b * y_b + g_c * y_c) @ w_out


def clamped_swiglu(hgu):
    gate = jnp.minimum(hgu[..., ::2], SWIGLU_LIMIT)
    lin = jnp.clip(hgu[..., 1::2], -SWIGLU_LIMIT, SWIGLU_LIMIT)
    return gate * jax.nn.sigmoid(SWIGLU_ALPHA * gate) * (lin + 1.0)


def moe(h, router_w, router_b, w_gu, b_gu, w_down, b_down):
    B, S, D = h.shape
    T = B * S
    A = T * TOP_K
    xt = h.reshape(T, D)
    logits = (xt @ router_w + router_b).astype(jnp.float32)
    top_vals, top_idx = lax.top_k(logits, TOP_K)
    gates = jax.nn.softmax(top_vals, axis=-1)
    e_flat = top_idx.reshape(A)
    order = jnp.argsort(e_flat)
    sorted_e = e_flat[order]
    counts = jnp.bincount(e_flat, length=N_EXPERTS)
    starts = jnp.cumsum(counts) - counts
    padded = ((counts + EXPERT_BLOCK - 1) // EXPERT_BLOCK) * EXPERT_BLOCK
    pad_ends = jnp.cumsum(padded)
    pad_starts = pad_ends - padded
    dest_sorted = pad_starts[sorted_e] + (jnp.arange(A) - starts[sorted_e])
    P = A + N_EXPERTS * EXPERT_BLOCK
    n_blocks = P // EXPERT_BLOCK
    buf = jnp.zeros((P, D), h.dtype).at[dest_sorted].set(xt[order // TOP_K])
    blk_e = jnp.minimum(jnp.searchsorted(pad_ends, jnp.arange(n_blocks) * EXPERT_BLOCK, side='right'),
                        N_EXPERTS - 1)

    def expert_block(args):
        xb, e = args
        return clamped_swiglu(xb @ w_gu[e] + b_gu[e]) @ w_down[e] + b_down[e]

    out_buf = lax.map(expert_block, (buf.reshape(n_blocks, EXPERT_BLOCK, D), blk_e)).reshape(P, D)
    dest = jnp.zeros((A,), dest_sorted.dtype).at[order].set(dest_sorted)
    y = jnp.einsum('tk,tkd->td', gates.astype(out_buf.dtype), out_buf[dest].reshape(T, TOP_K, D))
    return y.reshape(B, S, D).astype(h.dtype)


def setup_inputs(seed: int = 0) -> dict:
    key = jax.random.key(seed)
    ks = iter(jax.random.split(key, 48))
    f32 = jnp.float32

    def nrm(shape, scale):
        return jax.random.normal(next(ks), shape, f32) * scale

    def gain(shape):
        return 1.0 + nrm(shape, 0.02)

    x = nrm((BATCH, SEQ, D_MODEL), 1.0)
    p = nrm((DEPTH, BATCH, SEQ, PLE_DIM), 1.0)
    w_in = nrm((DEPTH, D_MODEL, IN_COLS), D_MODEL ** -0.5)
    conv_qkv_w = nrm((DEPTH, CONV_WIDTH, 3 * GDN_WIDTH), CONV_WIDTH ** -0.5)
    gdn_a_log = jnp.log(jax.random.uniform(next(ks), (DEPTH, GDN_HEADS), f32, 1.0, 16.0))
    dt = jnp.exp(jax.random.uniform(next(ks), (DEPTH, GDN_HEADS), f32, math.log(1e-3), math.log(1e-1)))
    gdn_dt_bias = dt + jnp.log(-jnp.expm1(-dt))
    gdn_norm_w = gain((DEPTH, GDN_HEAD_DIM))
    rg_conv_w = nrm((DEPTH, CONV_WIDTH, LRU_WIDTH), CONV_WIDTH ** -0.5)
    rg_conv_b = nrm((DEPTH, LRU_WIDTH), 0.02)
    rg_w_a = nrm((DEPTH, LRU_BLOCKS, LRU_BLOCK_DIM, LRU_BLOCK_DIM), LRU_BLOCK_DIM ** -0.5)
    rg_b_a = nrm((DEPTH, LRU_WIDTH), 0.02)
    rg_w_x = nrm((DEPTH, LRU_BLOCKS, LRU_BLOCK_DIM, LRU_BLOCK_DIM), LRU_BLOCK_DIM ** -0.5)
    rg_b_x = nrm((DEPTH, LRU_WIDTH), 0.02)
    s = jax.random.uniform(next(ks), (DEPTH, LRU_WIDTH), f32, 0.9, 0.999) ** (1.0 / LRU_C)
    rg_lambda = jnp.log(s) - jnp.log1p(-s)
    attn_sinks = nrm((DEPTH, SWA_Q_HEADS), 0.5)
    rel_bias = nrm((REL_BUCKETS, SWA_Q_HEADS), 0.5)
    w_o_gdn = nrm((DEPTH, GDN_WIDTH, D_MODEL), GDN_WIDTH ** -0.5)
    w_o_lru = nrm((DEPTH, LRU_WIDTH, D_MODEL), LRU_WIDTH ** -0.5)
    w_o_swa = nrm((DEPTH, SWA_Q_WIDTH, D_MODEL), SWA_Q_WIDTH ** -0.5)
    w_out = nrm((DEPTH, D_MODEL, D_MODEL), D_MODEL ** -0.5 * DEEPNORM_BETA)
    ln1_g = gain((DEPTH, D_MODEL))
    ln1_b = nrm((DEPTH, D_MODEL), 0.02)
    router_w = nrm((DEPTH, D_MODEL, N_EXPERTS), D_MODEL ** -0.5)
    router_b = nrm((DEPTH, N_EXPERTS), 0.01)
    w_gu = nrm((DEPTH, N_EXPERTS, D_MODEL, 2 * D_EXPERT), D_MODEL ** -0.5)
    b_gu = nrm((DEPTH, N_EXPERTS, 2 * D_EXPERT), 0.02)
    w_down = nrm((DEPTH, N_EXPERTS, D_EXPERT, D_MODEL), D_EXPERT ** -0.5 * DEEPNORM_BETA)
    b_down = nrm((DEPTH, N_EXPERTS, D_MODEL), 0.02)
    ln2_g = gain((DEPTH, D_MODEL))
    ln2_b = nrm((DEPTH, D_MODEL), 0.02)
    ple_w_gate = nrm((DEPTH, D_MODEL, D_MODEL), D_MODEL ** -0.5)
    ple_w_proj = nrm((DEPTH, PLE_DIM, D_MODEL), PLE_DIM ** -0.5 * DEEPNORM_BETA)
    ln3_g = gain((DEPTH, D_MODEL))
    ln3_b = nrm((DEPTH, D_MODEL), 0.02)
    return {"x": x, "p": p, "w_in": w_in, "conv_qkv_w": conv_qkv_w, "gdn_a_log": gdn_a_log,
            "gdn_dt_bias": gdn_dt_bias, "gdn_norm_w": gdn_norm_w, "rg_conv_w": rg_conv_w,
            "rg_conv_b": rg_conv_b, "rg_w_a": rg_w_a, "rg_b_a": rg_b_a, "rg_w_x": rg_w_x,
            "rg_b_x": rg_b_x, "rg_lambda": rg_lambda, "attn_sinks": attn_sinks, "rel_bias": rel_bias,
            "w_o_gdn": w_o_gdn, "w_o_lru": w_o_lru, "w_o_swa": w_o_swa, "w_out": w_out,
            "ln1_g": ln1_g, "ln1_b": ln1_b, "router_w": router_w, "router_b": router_b,
            "w_gu": w_gu, "b_gu": b_gu, "w_down": w_down, "b_down": b_down,
            "ln2_g": ln2_g, "ln2_b": ln2_b, "ple_w_gate": ple_w_gate, "ple_w_proj": ple_w_proj,
            "ln3_g": ln3_g, "ln3_b": ln3_b}


def reference(x, p, w_in, conv_qkv_w, gdn_a_log, gdn_dt_bias, gdn_norm_w, rg_conv_w, rg_conv_b,
              rg_w_a, rg_b_a, rg_w_x, rg_b_x, rg_lambda, attn_sinks, rel_bias,
              w_o_gdn, w_o_lru, w_o_swa, w_out, ln1_g, ln1_b, router_w, router_b,
              w_gu, b_gu, w_down, b_down, ln2_g, ln2_b, ple_w_gate, ple_w_proj, ln3_g, ln3_b):
    for i in range(DEPTH):
        y = token_mixer(x, w_in[i], conv_qkv_w[i], gdn_a_log[i], gdn_dt_bias[i], gdn_norm_w[i],
                        rg_conv_w[i], rg_conv_b[i], rg_w_a[i], rg_b_a[i], rg_w_x[i], rg_b_x[i], rg_lambda[i],
                        attn_sinks[i], rel_bias, w_o_gdn[i], w_o_lru[i], w_o_swa[i], w_out[i])
        x = layer_norm(DEEPNORM_ALPHA * x + y, ln1_g[i], ln1_b[i])
        y = moe(x, router_w[i], router_b[i], w_gu[i], b_gu[i], w_down[i], b_down[i])
        x = layer_norm(DEEPNORM_ALPHA * x + y, ln2_g[i], ln2_b[i])
        ple = jax.nn.sigmoid(x @ ple_w_gate[i]) * (p[i] @ ple_w_proj[i])
        x = layer_norm(DEEPNORM_ALPHA * x + ple, ln3_g[i], ln3_b[i])
    return x
```

```python
import math
from contextlib import ExitStack
import numpy as np
import concourse.bass as bass
import concourse.mybir as mybir
from concourse.bass_utils import run_bass_kernel_spmd

F32 = mybir.dt.float32
BF16 = mybir.dt.bfloat16
I32 = mybir.dt.int32
U32 = mybir.dt.uint32
AF = mybir.ActivationFunctionType
ALU = mybir.AluOpType
AX = mybir.AxisListType

S = 4096
D = 1024
NT = S // 128
DEPTH = 2
IN_COLS = 10768
ALPHA = (2.0 * DEPTH) ** 0.25
O_GQ, O_GK, O_GV, O_GZ, O_GA, O_GB = 0, 1024, 2048, 3072, 4096, 4104
O_LX, O_LG = 4112, 5136
O_SQ, O_SK, O_SV = 6160, 7184, 7440
O_MG = 7696


import os as _os
NOSELF = _os.environ.get("NOSELF", "0") == "1"


class _Rec:
    def __init__(self):
        self.call = None

    def __getattr__(self, name):
        def f(*a, **k):
            self.call = (name, a, k)
            return self
        return f


class Prog:
    def __init__(self, nc):
        self.nc = nc
        self.engs = {"pe": nc.tensor, "act": nc.scalar, "dve": nc.vector, "pool": nc.gpsimd, "sp": nc.sync}
        self.sem = {}
        self.cnt = {}
        self.es = ExitStack()
        for e in self.engs:
            self.sem[e] = self.es.enter_context(nc.semaphore("s_" + e))
            self.cnt[e] = 0
        self.ndsem = 8
        self.dsem = {}
        self.dcnt = {}
        self.dnext = {}
        for q in ("sp", "pool", "act"):
            self.dsem[q] = [self.es.enter_context(nc.semaphore("d_%s%d" % (q, i))) for i in range(self.ndsem)]
            self.dcnt[q] = [0] * self.ndsem
            self.dnext[q] = 0
        self.waited = {}
        self.last_w = {}
        self.readers = {}
        self.uid = 0
        self.all_tokens = {}
        self.recording = False
        self.recbuf = []

    def name(self, n):
        self.uid += 1
        return "%s_%d" % (n, self.uid)

    def sb(self, st, n, shape, dt):
        return st.enter_context(self.nc.sbuf_tensor(self.name(n), list(shape), dt))

    def ps(self, st, n, shape, dt):
        return st.enter_context(self.nc.psum_tensor(self.name(n), list(shape), dt))

    def dram(self, n, shape, dt):
        return self.nc.dram_tensor(n, list(shape), dt).ap()

    def _deps(self, r, w):
        deps = {}

        def add(tok):
            s, v = tok
            if deps.get(s, 0) < v:
                deps[s] = v

        for k in r:
            t = self.last_w.get(k)
            if t is not None:
                add(t)
        for k in w:
            t = self.last_w.get(k)
            if t is not None:
                add(t)
            for s, v in self.readers.get(k, {}).items():
                add((s, v))
        return deps

    def _wait(self, e, deps):
        eng = self.engs[e]
        for s, v in deps.items():
            if e == "pe" and s is self.sem["pe"]:
                continue
            if NOSELF and e in self.sem and s is self.sem[e]:
                continue
            key = (e, s.name)
            if self.waited.get(key, 0) < v:
                eng.wait_ge(s, v)
                self.waited[key] = v

    def _record(self, tok, r, w):
        s, v = tok
        self.all_tokens[s.name] = (s, v)
        for k in r:
            d = self.readers.setdefault(k, {})
            if d.get(s, 0) < v:
                d[s] = v
        for k in w:
            self.last_w[k] = tok
            self.readers[k] = {}

    def op(self, e, fn, r=(), w=()):
        if self.recording:
            rec = _Rec()
            fn(rec)
            self.recbuf.append(("op", e, rec.call, tuple(r), tuple(w)))
            return
        self._wait(e, self._deps(r, w))
        inst = fn(self.engs[e])
        self.cnt[e] += 1
        inst.then_inc(self.sem[e], 1)
        self._record((self.sem[e], self.cnt[e]), r, w)

    def emit(self, item):
        if item[0] == "op":
            _, e, (name, a, k), r, w = item
            self.op(e, lambda eng: getattr(eng, name)(*a, **k), r, w)
        elif item[0] == "dmac":
            _, q, (name, a, k), r, w = item
            self.dma_custom(q, lambda eng: getattr(eng, name)(*a, **k), r, w)
        else:
            _, q, out, in_, r, w, kw = item
            self.dma(q, out, in_, r, w, **kw)

    def dma(self, q, out, in_, r=(), w=(), **kw):
        if self.recording:
            self.recbuf.append(("dma", q, out, in_, tuple(r), tuple(w), kw))
            return
        deps = self._deps(r, w)
        j = self.dnext[q]
        self.dnext[q] = (j + 1) % self.ndsem
        s = self.dsem[q][j]
        if self.dcnt[q][j] > 0:
            deps[s] = max(deps.get(s, 0), 16 * self.dcnt[q][j])
        self._wait(q, deps)
        self.engs[q].dma_start(out=out, in_=in_, **kw).then_inc(s, 16)
        self.dcnt[q][j] += 1
        self._record((s, 16 * self.dcnt[q][j]), r, w)

    def dma_custom(self, q, fn, r=(), w=()):
        if self.recording:
            rec = _Rec()
            fn(rec)
            self.recbuf.append(("dmac", q, rec.call, tuple(r), tuple(w)))
            return
        deps = self._deps(r, w)
        j = self.dnext[q]
        self.dnext[q] = (j + 1) % self.ndsem
        s = self.dsem[q][j]
        if self.dcnt[q][j] > 0:
            deps[s] = max(deps.get(s, 0), 16 * self.dcnt[q][j])
        self._wait(q, deps)
        fn(self.engs[q]).then_inc(s, 16)
        self.dcnt[q][j] += 1
        self._record((s, 16 * self.dcnt[q][j]), r, w)

    def barrier(self):
        toks = {n: sv for n, sv in self.all_tokens.items()}
        for e in self.engs:
            d = {}
            for n, (s, v) in toks.items():
                d[s] = v
            self._wait(e, d)

    def finish(self):
        self.barrier()
        self.es.close()


def run_streams(p, gens, lags=None):
    lags = lags or [0.0] * len(gens)
    bufs = []
    for g_ in gens:
        p.recbuf = []
        p.recording = True
        next(g_)
        p.recording = False
        bufs.append(p.recbuf)
    for i_ in reversed(range(len(gens))):
        g_ = gens[i_]
        p.recbuf = bufs[i_]
        p.recording = True
        for _ in g_:
            pass
        p.recording = False
    items = []
    for si, b_ in enumerate(bufs):
        n_ = max(1, len(b_))
        for i_, it_ in enumerate(b_):
            items.append(((i_ + 0.5) / n_ + lags[si], si, i_, it_))
    items.sort(key=lambda t_: (t_[0], t_[1], t_[2]))
    for _, _, _, it_ in items:
        p.emit(it_)
    p.barrier()


def mm(p, out, lhsT, rhs, start, stop, r, w):
    p.op("pe", lambda e: e.matmul(out, lhsT, rhs, start=start, stop=stop), r=r, w=w)


class K:
    pass


def build_program(nlayers=DEPTH, stages=("init", "lru", "swa", "gdn", "merge", "moe", "ple"), dbg=None,
                  gather=False):
    nc = bass.Bass("TRN2", target_bir_lowering=False)
    p = Prog(nc)
    g = K()
    g.nc, g.p = nc, p
    ein = lambda n, shape, dt=F32: nc.dram_tensor(n, list(shape), dt, kind="ExternalInput").ap()
    g.x = ein("x", [S, D])
    if "ple" in stages:
        g.pp = ein("p", [DEPTH, S, 256])
    g.consts = ein("consts", [128, CONST_COLS])
    W = {}
    for n in needed_weights(stages):
        W[n] = ein(n, WEIGHT_SHAPES[n])
    g.W = W
    g.out = nc.dram_tensor("out", [S, D], F32, kind="ExternalOutput").ap()
    g.X32 = p.dram("X32", [S, D], F32)
    g.XT16 = p.dram("XT16", [128, 8, S], BF16)
    g.OAT = p.dram("OAT", [128, 8, S], BF16)
    g.OBT = p.dram("OBT", [128, 8, S], BF16)
    g.OCT = p.dram("OCT", [128, 8, S], BF16)
    g.EXT_h = nc.dram_tensor("EXT", [16, 383], F32)
    g.XBUF = p.dram("XBUF", [NE * CAP + S, D], BF16)
    g.OBUF = p.dram("OBUF", [NE * CAP + S, D], F32)
    g.dbg = {}
    if dbg:
        for n, (shape, dt) in dbg.items():
            g.dbg[n] = nc.dram_tensor("dbg_" + n, list(shape), dt, kind="ExternalOutput").ap()
    with nc.Block() as block, ExitStack() as gst:
        g.cst = p.sb(gst, "cst", [128, CONST_COLS], F32)
        p.dma("sp", g.cst[:, :], g.consts, r=(), w=("cst",))
        g.ident = g.cst[:, C_IDENT:C_IDENT + 128]
        g.identb = p.sb(gst, "identb", [128, 128], BF16)
        p.op("dve", lambda e: e.tensor_copy(g.identb[:, :], g.ident), r=("cst",), w=("identb",))
        g.onesb = p.sb(gst, "onesb", [128, 512], BF16)
        p.op("dve", lambda e: e.memset(g.onesb[:, :], 1.0), w=("onesb",))
        if "init" in stages:
            stage_init(g)
        for li in range(nlayers):
            if "lru" in stages:
                stage_lru(g, li)
            if "swa" in stages:
                stage_swa(g, li)
            if "gdn" in stages:
                stage_gdn(g, li)
            if "merge" in stages:
                stage_merge(g, li)
            if "moe" in stages:
                stage_moe(g, li, fuse_ple=("ple" in stages))
            elif "ple" in stages:
                stage_ple(g, li)
        for n in g.dbg:
            src = getattr(g, n)
            p.dma("sp", g.dbg[n], src, r=[(n, t) for t in range(NT)], w=("dbg_" + n,))
        for t in range(0, NT, 4):
            p.dma("sp", g.out[t * 128:(t + 4) * 128, :], g.X32[t * 128:(t + 4) * 128, :],
                  r=[("X32", t + j) for j in range(4)], w=(("out", t),))
        p.finish()
    return nc, g


C_IDENT = 0
C_OHX = 128
C_ANTI = 512
C_TRIU = 640
C_LOWS = 704
C_UPS = 768
C_UPI = 832
C_ONES = 896
C_TRI128 = 1024
C_EBASE = 1152
CONST_COLS = 1184
CAP = 768
NE = 32
BIGIDX = 1048576.0
NEG = -30000.0


def _t5_bucket(dist):
    max_exact = 16
    d = np.maximum(dist.astype(np.float32), np.float32(1.0))
    large = max_exact + (np.log(d / np.float32(max_exact)) / np.float32(math.log(128 / max_exact))
                         * np.float32(32 - max_exact)).astype(np.int32)
    large = np.minimum(large, 31)
    return np.where(dist < max_exact, dist, large)


def make_consts():
    c = np.zeros((128, CONST_COLS), np.float32)
    c[:, C_IDENT:C_IDENT + 128] = np.eye(128, dtype=np.float32)
    c[:, C_ANTI:C_ANTI + 128] = np.eye(128, dtype=np.float32)[::-1]
    r = np.arange(64)[:, None]
    cc = np.arange(64)[None, :]
    c[0:64, C_TRIU:C_TRIU + 64] = (r <= cc)
    c[0:64, C_LOWS:C_LOWS + 64] = (r > cc)
    c[0:64, C_UPS:C_UPS + 64] = (cc > r)
    c[0:64, C_UPI:C_UPI + 64] = (cc >= r)
    c[:, C_ONES:C_ONES + 128] = 1.0
    c[:, C_TRI128:C_TRI128 + 128] = (np.arange(128)[:, None] < np.arange(128)[None, :])
    c[:, C_EBASE:C_EBASE + 32] = (np.arange(32) * CAP)[None, :]
    for j in range(383):
        dist = j - 127
        if 0 <= dist < 128:
            c[int(_t5_bucket(np.array([dist]))[0]), C_OHX + j] = 1.0
        else:
            c[32, C_OHX + j] = NEG
    return c


WEIGHT_SHAPES = {
    "w_in": [DEPTH, D, IN_COLS],
    "rg_conv_w": [DEPTH, 4, D], "rg_conv_b": [DEPTH, D], "rg_w_a": [DEPTH, 8, 128, 128], "rg_b_a": [DEPTH, D],
    "rg_w_x": [DEPTH, 8, 128, 128], "rg_b_x": [DEPTH, D], "rg_lambda": [DEPTH, D],
    "attn_sinks": [DEPTH, 16], "rel_bias": [32, 16],
    "w_o_gdn": [DEPTH, D, D], "w_o_lru": [DEPTH, D, D], "w_o_swa": [DEPTH, D, D], "w_out": [DEPTH, D, D],
    "ln1_g": [DEPTH, D], "ln1_b": [DEPTH, D],
    "router_w": [DEPTH, D, NE], "router_b": [DEPTH, NE], "w_gu": [DEPTH, NE, D, 2 * D], "b_gu": [DEPTH, NE, 2 * D],
    "w_down": [DEPTH, NE, D, D], "b_down": [DEPTH, NE, D], "ln2_g": [DEPTH, D], "ln2_b": [DEPTH, D],
    "ple_w_gate": [DEPTH, D, D], "ple_w_proj": [DEPTH, 256, D], "ln3_g": [DEPTH, D], "ln3_b": [DEPTH, D],
    "conv_qkv_w": [DEPTH, 4, 3072], "gdn_a_log": [DEPTH, 8], "gdn_dt_bias": [DEPTH, 8], "gdn_norm_w": [DEPTH, 128],
}


def to_xt16(g, st, xt_sb_f32, tile_idx, keys_r, bufs, i, dst=None, dname="XT16", cast=True, tag=""):
    p = g.p
    xb, pst, xtb = bufs
    b = i % 2
    pb = b % len(pst)
    if dst is None:
        dst = g.XT16
    if cast:
        p.op("act", lambda e: e.copy(xb[b][:, :], xt_sb_f32), r=keys_r, w=("xb%s%d" % (tag, b),))
    else:
        p.op("pool", lambda e: e.tensor_copy(xb[b][:, :], xt_sb_f32), r=keys_r, w=("xb%s%d" % (tag, b),))
    for c in range(8):
        p.op("pe", lambda e: e.transpose(pst[pb][:, c * 128:(c + 1) * 128], xb[b][:, c * 128:(c + 1) * 128],
                                         g.identb[:, :]), r=("xb%s%d" % (tag, b), "identb"), w=("pst%s%d" % (tag, pb),))
    p.op("dve", lambda e: e.tensor_copy(xtb[b][:, :], pst[pb][:, :]), r=("pst%s%d" % (tag, pb),), w=("xtb%s%d" % (tag, b),))
    p.dma("sp", dst[:, :, tile_idx * 128:(tile_idx + 1) * 128],
          xtb[b][:, :].rearrange("p (c t) -> p c t", c=8), r=("xtb%s%d" % (tag, b),), w=((dname, tile_idx),))


def xt16_bufs(g, st, npst=2):
    p = g.p
    xb = [p.sb(st, "xb", [128, D], BF16) for _ in range(2)]
    pst = [p.ps(st, "pst", [128, D], BF16) for _ in range(npst)]
    xtb = [p.sb(st, "xtb", [128, D], BF16) for _ in range(2)]
    return xb, pst, xtb


def stage_init(g):
    p = g.p
    with ExitStack() as st:
        xin = [p.sb(st, "xin", [128, D], F32) for _ in range(2)]
        bufs = xt16_bufs(g, st)
        for t in range(NT):
            b = t % 2
            p.dma("sp", xin[b][:, :], g.x[t * 128:(t + 1) * 128, :], r=(), w=("xin%d" % b,))
            p.dma("sp", g.X32[t * 128:(t + 1) * 128, :], xin[b][:, :], r=("xin%d" % b,), w=(("X32", t),))
            to_xt16(g, st, xin[b][:, :], t, ("xin%d" % b,), bufs, t)
        p.barrier()


def stream_lru(g, li):
    p = g.p
    W = g.W
    TT = 512
    with ExitStack() as st:
        prm = p.sb(st, "lprm", [128, 8, 8], F32)
        for k in range(4):
            p.dma("sp", prm[:, :, k], W["rg_conv_w"][li, k, :].rearrange("(c p) -> p c", p=128),
                  w=("lprm",), allow_slow_non_contiguous=True)
        for k, n in ((4, "rg_conv_b"), (5, "rg_b_a"), (6, "rg_b_x"), (7, "rg_lambda")):
            p.dma("sp", prm[:, :, k], W[n][li, :].rearrange("(c p) -> p c", p=128), w=("lprm",),
                  allow_slow_non_contiguous=True)
        nsp = p.sb(st, "nsp", [128, 8], F32)
        p.op("act", lambda e: e.activation(nsp[:, :], prm[:, :, 7], AF.Exp, scale=-1.0), r=("lprm",), w=("nsp",))
        p.op("act", lambda e: e.activation(nsp[:, :], nsp[:, :], AF.Ln, bias=1.0), r=("nsp",), w=("nsp",))
        p.op("dve", lambda e: e.tensor_scalar(nsp[:, :], nsp[:, :], -8.0, None, ALU.mult), r=("nsp",), w=("nsp",))
        wx = p.sb(st, "lwx", [128, 8, 2048], BF16)
        for kc in range(8):
            p.dma("pool", wx[:, kc, :], W["w_in"][li, kc * 128:(kc + 1) * 128, O_LX:O_LX + 2048], w=("lwx",))
        wa = p.sb(st, "lwa", [128, 8, 128], BF16)
        wxx = p.sb(st, "lwxx", [128, 8, 128], BF16)
        p.dma("pool", wa[:, :, :], W["rg_w_a"][li].rearrange("h i j -> i h j"), w=("lwa",))
        p.dma("pool", wxx[:, :, :], W["rg_w_x"][li].rearrange("h i j -> i h j"), w=("lwxx",))
        xt = [p.sb(st, "lxt", [128, 8, TT], BF16) for _ in range(2)]
        xl = [p.sb(st, "lxl", [128, TT + 3], F32) for _ in range(2)]
        xc = [p.sb(st, "lxc", [128, TT], F32) for _ in range(2)]
        xcb = [p.sb(st, "lxcb", [128, TT], BF16) for _ in range(2)]
        ra = [p.sb(st, "lra", [128, TT], F32) for _ in range(2)]
        ri = [p.sb(st, "lri", [128, TT], F32) for _ in range(2)]
        gu = [p.sb(st, "lgu", [128, TT], F32) for _ in range(2)]
        hh = [p.sb(st, "lhh", [128, TT], F32) for _ in range(2)]
        ob = [p.sb(st, "lob", [128, 8, TT], BF16) for _ in range(2)]
        halo = p.sb(st, "lhalo", [128, 8, 3], F32)
        hst = p.sb(st, "lhst", [128, 8], F32)
        p.op("dve", lambda e: e.memset(halo[:, :, :], 0.0), w=("lhalo",))
        p.op("dve", lambda e: e.memset(hst[:, :], 0.0), w=("lhst",))
        ps1 = [p.ps(st, "lps1", [128, TT], F32) for _ in range(2)]
        ps2 = [p.ps(st, "lps2", [128, TT], F32) for _ in range(2)]
        ps3 = [p.ps(st, "lps3", [128, TT], F32) for _ in range(2)]
        it = 0
        yield
        for tt in range(S // TT):
            tb = tt % 2
            p.dma("sp", xt[tb][:, :, :], g.XT16[:, :, tt * TT:(tt + 1) * TT],
                  r=[("XT16", tt * 4 + j) for j in range(4)], w=("lxt%d" % tb,))
            for c in range(8):
                b = it % 2
                it += 1
                kx, kg = "lps1%d" % b, "lps2%d" % b
                for kc in range(8):
                    mm(p, ps1[b][:, :], wx[:, kc, c * 128:(c + 1) * 128], xt[tb][:, kc, :], kc == 0, kc == 7,
                       r=("lwx", "lxt%d" % tb), w=(kx,))
                for kc in range(8):
                    mm(p, ps2[b][:, :], wx[:, kc, 1024 + c * 128:1024 + (c + 1) * 128], xt[tb][:, kc, :], kc == 0,
                       kc == 7, r=("lwx", "lxt%d" % tb), w=(kg,))
                p.op("dve", lambda e: e.tensor_copy(xl[b][:, 0:3], halo[:, c, :]), r=("lhalo",), w=("lxl%d" % b,))
                p.op("act", lambda e: e.copy(xl[b][:, 3:], ps1[b][:, :]), r=(kx,), w=("lxl%d" % b,))
                p.op("dve", lambda e: e.tensor_copy(halo[:, c, :], xl[b][:, TT:TT + 3]), r=("lxl%d" % b,),
                     w=("lhalo",))
                p.op("dve", lambda e: e.tensor_scalar(xc[b][:, :], xl[b][:, 0:TT], prm[:, c, 0:1], prm[:, c, 4:5],
                                                      ALU.mult, ALU.add), r=("lxl%d" % b, "lprm"), w=("lxc%d" % b,))
                for j in range(1, 4):
                    p.op("dve", lambda e, j=j: e.scalar_tensor_tensor(xc[b][:, :], xl[b][:, j:j + TT],
                                                                       prm[:, c, j:j + 1], xc[b][:, :], ALU.mult,
                                                                       ALU.add),
                         r=("lxl%d" % b, "lprm", "lxc%d" % b), w=("lxc%d" % b,))
                p.op("act", lambda e: e.copy(xcb[b][:, :], xc[b][:, :]), r=("lxc%d" % b,), w=("lxcb%d" % b,))
                mm(p, ps3[b][:, :], wa[:, c, :], xcb[b][:, :], True, True, r=("lwa", "lxcb%d" % b), w=("lps3%d" % b,))
                p.op("act", lambda e: e.activation(ra[b][:, :], ps3[b][:, :], AF.Sigmoid, bias=prm[:, c, 5:6]),
                     r=("lps3%d" % b, "lprm"), w=("lra%d" % b,))
                mm(p, ps3[b][:, :], wxx[:, c, :], xcb[b][:, :], True, True, r=("lwxx", "lxcb%d" % b),
                   w=("lps3%d" % b,))
                p.op("act", lambda e: e.activation(ri[b][:, :], ps3[b][:, :], AF.Sigmoid, bias=prm[:, c, 6:7]),
                     r=("lps3%d" % b, "lprm"), w=("lri%d" % b,))
                p.op("act", lambda e: e.activation(ra[b][:, :], ra[b][:, :], AF.Exp, scale=nsp[:, c:c + 1]),
                     r=("lra%d" % b, "nsp"), w=("lra%d" % b,))
                p.op("dve", lambda e: e.tensor_tensor(gu[b][:, :], ra[b][:, :], ra[b][:, :], ALU.mult),
                     r=("lra%d" % b,), w=("lgu%d" % b,))
                p.op("act", lambda e: e.activation(gu[b][:, :], gu[b][:, :], AF.Sqrt, scale=-1.0, bias=1.0),
                     r=("lgu%d" % b,), w=("lgu%d" % b,))
                p.op("dve", lambda e: e.tensor_tensor(ri[b][:, :], ri[b][:, :], xc[b][:, :], ALU.mult),
                     r=("lri%d" % b, "lxc%d" % b), w=("lri%d" % b,))
                p.op("dve", lambda e: e.tensor_tensor(gu[b][:, :], gu[b][:, :], ri[b][:, :], ALU.mult),
                     r=("lgu%d" % b, "lri%d" % b), w=("lgu%d" % b,))
                p.op("dve", lambda e: e.tensor_tensor_scan(hh[b][:, :], ra[b][:, :], gu[b][:, :], hst[:, c:c + 1],
                                                           ALU.mult, ALU.add),
                     r=("lra%d" % b, "lgu%d" % b, "lhst"), w=("lhh%d" % b,))
                p.op("dve", lambda e: e.tensor_copy(hst[:, c:c + 1], hh[b][:, TT - 1:TT]), r=("lhh%d" % b,),
                     w=("lhst",))
                p.op("act", lambda e: e.activation(ri[b][:, :], ps2[b][:, :], AF.Gelu), r=(kg,), w=("lri%d" % b,))
                p.op("dve", lambda e: e.tensor_tensor(ob[tb][:, c, :], hh[b][:, :], ri[b][:, :], ALU.mult),
                     r=("lhh%d" % b, "lri%d" % b), w=("lob%d" % tb,))
            p.dma("sp", g.OBT[:, :, tt * TT:(tt + 1) * TT], ob[tb][:, :, :], r=("lob%d" % tb,),
                  w=[("OBT", tt * 4 + j) for j in range(4)])


def stream_swa(g, li):
    p = g.p
    W = g.W
    TT = 512
    with ExitStack() as st:
        rb = p.sb(st, "srb", [33, 16], F32)
        p.op("dve", lambda e: e.memset(rb[:, :], 1.0), w=("srb",))
        p.dma("sp", rb[0:32, :], W["rel_bias"], w=("srb",))
        sps = [p.ps(st, "ssps", [128, 512], F32) for _ in range(2)]
        pext = sps[1][0:16, 0:383]
        pext2 = sps[0]
        mm(p, pext, rb[:, :], g.cst[0:33, C_OHX:C_OHX + 383], True, True, r=("srb", "cst"), w=("ssps1",))
        ext = p.sb(st, "sext", [16, 383], F32)
        p.op("dve", lambda e: e.tensor_copy(ext[:, :], pext), r=("ssps1",), w=("sext",))
        p.dma("sp", g.EXT_h.ap(), ext[:, :], r=("sext",), w=("EXT",))
        bias = p.sb(st, "sbias", [128, 4, 2, 512], F32)
        hank = p.sb(st, "shank", [128, 512], F32)
        for hk in range(4):
            for blk, off in ((0, 128), (1, 0)):
                for gq in range(4):
                    h = 4 * hk + gq
                    src = bass.AP(g.EXT_h, h * 383 + off, [[1, 128], [1, 128]])
                    p.dma("sp", hank[:, gq * 128:(gq + 1) * 128], src, r=("EXT",), w=("shank",))
                mm(p, pext2[:, :], g.cst[:, C_ANTI:C_ANTI + 128], hank[:, :], True, True, r=("cst", "shank"),
                   w=("ssps0",))
                p.op("dve", lambda e: e.tensor_copy(bias[:, hk, blk, :], pext2[:, :]), r=("ssps0",), w=("sbias",))
        esink = p.sb(st, "sesink", [128, 16], F32)
        p.dma("sp", esink[:, :], bass.AP(W["attn_sinks"].tensor, li * 16, [[0, 128], [1, 16]]), w=("sesink",))
        p.op("act", lambda e: e.activation(esink[:, :], esink[:, :], AF.Exp), r=("sesink",), w=("sesink",))
        wq = p.sb(st, "swq", [128, 8, 1024], BF16)
        wk = p.sb(st, "swk", [128, 8, 256], BF16)
        wv = p.sb(st, "swv", [128, 8, 256], BF16)
        for kc in range(8):
            rows = W["w_in"][li, kc * 128:(kc + 1) * 128, :]
            p.dma("pool", wq[:, kc, :], rows[:, O_SQ:O_SQ + 1024], w=("swq",))
            p.dma("pool", wk[:, kc, :], rows[:, O_SK:O_SK + 256], w=("swk",))
            p.dma("pool", wv[:, kc, :], rows[:, O_SV:O_SV + 256], w=("swv",))
        xt = [p.sb(st, "sxt", [128, 8, TT], BF16) for _ in range(2)]
        qT = p.sb(st, "sqT", [64, 16, TT], BF16)
        kT = p.sb(st, "skT", [64, 4, 128 + TT], BF16)
        va = p.sb(st, "sva", [128, 5, 4, 65], BF16)
        p.op("dve", lambda e: e.memset(va[:, :, :, :], 1.0), w=("sva",))
        p.op("dve", lambda e: e.memset(kT[:, :, :], 0.0), w=("skT",))
        tmp = [p.sb(st, "stmp", [128, 512], F32) for _ in range(2)]
        pT = [p.sb(st, "spT", [128, 512], BF16) for _ in range(4)]
        den = p.sb(st, "sden", [128, 4], F32)
        oc = [p.sb(st, "soc", [128, 1024], BF16) for _ in range(2)]
        qps = [p.ps(st, "sqps", [64, TT], F32) for _ in range(2)]
        pvb = p.ps(st, "spvb", [128, 512], F32)
        pv = pvb[:, 0:260].rearrange("p (g d) -> p g d", g=4)
        vps = pvb[:, 384:512]
        xb = [oc[0], oc[1]]
        pst = [p.ps(st, "spst", [128, D], BF16)]
        xtb = [p.sb(st, "sxtb", [128, D], BF16) for _ in range(2)]
        iq = 0
        isc = 0
        yield
        for tt in range(S // TT):
            tb = tt % 2
            p.dma("sp", xt[tb][:, :, :], g.XT16[:, :, tt * TT:(tt + 1) * TT],
                  r=[("XT16", tt * 4 + j) for j in range(4)], w=("sxt%d" % tb,))
            xk = "sxt%d" % tb
            for h in range(16):
                b = iq % 2
                iq += 1
                for kc in range(8):
                    mm(p, qps[b][:, :], wq[:, kc, h * 64:(h + 1) * 64], xt[tb][:, kc, :], kc == 0, kc == 7,
                       r=("swq", xk), w=("sqps%d" % b,))
                p.op("act", lambda e: e.copy(qT[:, h, :], qps[b][:, :]), r=("sqps%d" % b,), w=("sqT",))
            for hk in range(4):
                b = iq % 2
                iq += 1
                for kc in range(8):
                    mm(p, qps[b][:, :], wk[:, kc, hk * 64:(hk + 1) * 64], xt[tb][:, kc, :], kc == 0, kc == 7,
                       r=("swk", xk), w=("sqps%d" % b,))
                p.op("act", lambda e: e.copy(kT[:, hk, 128:], qps[b][:, :]), r=("sqps%d" % b,), w=("skT",))
            for nb in range(4):
                for hv in range(2):
                    for kc in range(8):
                        mm(p, vps, xt[tb][:, kc, nb * 128:(nb + 1) * 128], wv[:, kc, hv * 128:(hv + 1) * 128], kc == 0,
                           kc == 7, r=("swv", xk), w=("spv",))
                    p.op("act", lambda e: e.copy(va[:, nb + 1, 2 * hv:2 * hv + 2, 0:64],
                                                 vps.rearrange("p (h d) -> p h d", h=2)), r=("spv",), w=("sva",))
            for nb in range(4):
                n = tt * 4 + nb
                ob = n % 2
                for hk in range(4):
                    blks = [1] if n == 0 else [0, 1]
                    pts = []
                    for blk in blks:
                        b = isc % 2
                        pb = isc % 4
                        isc += 1
                        koff = nb * 128 + (0 if blk == 0 else 128)
                        mm(p, sps[b][:, :], kT[:, hk, koff:koff + 128],
                           qT[:, 4 * hk:4 * hk + 4, nb * 128:(nb + 1) * 128], True, True,
                           r=("skT", "sqT"), w=("ssps%d" % b,))
                        p.op("dve", lambda e: e.scalar_tensor_tensor(tmp[b][:, :], sps[b][:, :], 0.125,
                                                                      bias[:, hk, blk, :], ALU.mult, ALU.add),
                             r=("ssps%d" % b, "sbias"), w=("stmp%d" % b,))
                        p.op("act", lambda e: e.activation(pT[pb][:, :], tmp[b][:, :], AF.Exp),
                             r=("stmp%d" % b,), w=("spT%d" % pb,))
                        pts.append((pb, nb + blk))
                    for gq in range(4):
                        for i, (pb, vb) in enumerate(pts):
                            mm(p, pv[:, gq, :], pT[pb][:, gq * 128:(gq + 1) * 128], va[:, vb, hk, :], i == 0,
                               i == len(pts) - 1, r=("spT%d" % pb, "sva"), w=("spv",))
                    p.op("dve", lambda e: e.tensor_tensor(den[:, :], pv[:, :, 64], esink[:, 4 * hk:4 * hk + 4],
                                                          ALU.add), r=("spv", "sesink"), w=("sden",))
                    p.op("dve", lambda e: e.reciprocal(den[:, :], den[:, :]), r=("sden",), w=("sden",))
                    p.op("dve", lambda e: e.tensor_tensor(
                        oc[ob][:, hk * 256:(hk + 1) * 256].rearrange("p (g d) -> p g d", g=4), pv[:, :, 0:64],
                        den[:, :].unsqueeze(2).to_broadcast([128, 4, 64]), ALU.mult),
                         r=("spv", "sden"), w=("soc%d" % ob,))
                for c in range(8):
                    p.op("pe", lambda e: e.transpose(pst[0][:, c * 128:(c + 1) * 128],
                                                     oc[ob][:, c * 128:(c + 1) * 128], g.identb[:, :]),
                         r=("soc%d" % ob, "identb"), w=("spst",))
                p.op("act", lambda e: e.copy(xtb[ob][:, :], pst[0][:, :]), r=("spst",), w=("sxtb%d" % ob,))
                p.dma("sp", g.OCT[:, :, n * 128:(n + 1) * 128], xtb[ob][:, :].rearrange("p (c t) -> p c t", c=8),
                      r=("sxtb%d" % ob,), w=(("OCT", n),))
            p.op("dve", lambda e: e.tensor_copy(kT[:, :, 0:128], kT[:, :, TT:TT + 128]), r=("skT",), w=("skT",))
            p.op("dve", lambda e: e.tensor_copy(va[:, 0, :, :], va[:, 4, :, :]), r=("sva",), w=("sva",))


def stage_lru(g, li):
    run_streams(g.p, [stream_lru(g, li)])


def stage_swa(g, li):
    run_streams(g.p, [stream_swa(g, li)])


def stage_lru_swa(g, li):
    run_streams(g.p, [stream_swa(g, li), stream_lru(g, li)])


import os
GDN_CUT = float(os.environ.get('GDN_CUT', '99'))


def bcast_rows(handle_ap, offset, n, parts):
    return bass.AP(handle_ap.tensor, offset, [[0, parts], [1, n]])


def stage_gdn(g, li, nchunks=S // 64):
    p = g.p
    with ExitStack() as st:
        PSall = p.ps(st, "gps", [128, 8, 512], F32)
        xt = [p.sb(st, "gxt", [128, 8, 512], BF16) for _ in range(2)]
        th = [gdn_thread(g, li, st, PSall[:, 4 * i:4 * i + 4, :], 4 * i, 4, "ab"[i], nchunks, xt) for i in range(2)]
        live = list(th)
        while live:
            bufs = []
            for t_ in list(live):
                p.recbuf = []
                p.recording = True
                try:
                    next(t_)
                except StopIteration:
                    live.remove(t_)
                p.recording = False
                bufs.append(p.recbuf)
            n_ = max(len(b_) for b_ in bufs) if bufs else 0
            for i_ in range(n_):
                for b_ in bufs:
                    if i_ < len(b_):
                        p.emit(b_[i_])
        p.barrier()


def gdn_thread(g, li, st, PS, h0, H, TG, nchunks, xt):
    p = g.p
    W = g.W
    TT = 512
    C = 64
    cst = g.cst
    HC = H * 64
    NBK = PS.shape[1]
    bank_i = [0]

    def bank():
        b = bank_i[0] % NBK
        bank_i[0] += 1
        return b

    def bk(b):
        return ["gbank%s%d" % (TG, b)]

    K = lambda n: n + TG
    sbt = lambda n, shape, dt: p.sb(st, n + TG, shape, dt)
    cw = sbt("gcw", [128, 3 * H, 4], F32)
    for j in range(3):
        for c in range(H):
            col = j * 1024 + (h0 + c) * 128
            p.dma("sp", cw[:, j * H + c, :], W["conv_qkv_w"][li, :, col:col + 128].rearrange("k p -> p k"),
                  w=(K("gcw"),), allow_slow_non_contiguous=True)
    hp = sbt("ghp", [64, 2 * H], F32)
    p.dma("sp", hp[:, 0:H], bcast_rows(W["gdn_dt_bias"], li * 8 + h0, H, 64), w=(K("ghp"),))
    p.dma("sp", hp[:, H:2 * H], bcast_rows(W["gdn_a_log"], li * 8 + h0, H, 64), w=(K("ghp"),))
    p.op("act", lambda e: e.activation(hp[:, H:2 * H], hp[:, H:2 * H], AF.Exp), r=(K("ghp"),), w=(K("ghp"),))
    p.op("dve", lambda e: e.tensor_scalar(hp[:, H:2 * H], hp[:, H:2 * H], -1.0, None, ALU.mult), r=(K("ghp"),),
         w=(K("ghp"),))
    nw = sbt("gnw", [64, 128], F32)
    p.dma("sp", nw[:, :], bcast_rows(W["gdn_norm_w"], li * 128, 128, 64), w=(K("gnw"),))
    wqkv = sbt("gwqkv", [128, 8, 3 * H * 128], BF16)
    wz = sbt("gwz", [128, 8, H * 128], BF16)
    wab = sbt("gwab", [128, 8, 2 * H], BF16)
    for kc in range(8):
        rows = W["w_in"][li, kc * 128:(kc + 1) * 128, :]
        for j in range(3):
            c0 = j * 1024 + h0 * 128
            p.dma("pool", wqkv[:, kc, j * H * 128:(j + 1) * H * 128], rows[:, c0:c0 + H * 128], w=(K("gwqkv"),))
        p.dma("pool", wz[:, kc, :], rows[:, O_GZ + h0 * 128:O_GZ + (h0 + H) * 128], w=(K("gwz"),))
        p.dma("pool", wab[:, kc, 0:H], rows[:, O_GA + h0:O_GA + h0 + H], w=(K("gwab"),))
        p.dma("pool", wab[:, kc, H:2 * H], rows[:, O_GB + h0:O_GB + h0 + H], w=(K("gwab"),))
    pc = sbt("gpc", [128, 3 + TT], F32)
    halo = sbt("ghalo", [128, 3 * H, 3], F32)
    p.op("dve", lambda e: e.memset(halo[:, :, :], 0.0), w=(K("ghalo"),))
    acc = sbt("gacc", [128, TT], F32)
    cv = sbt("gcv", [128, 3 * H, TT], BF16)
    Sst = sbt("gS", [128, H, 128], F32)
    Sb = sbt("gSb", [128, H, 128], BF16)
    p.op("dve", lambda e: e.memset(Sst[:, :, :], 0.0), w=(K("gS"),))
    p.op("dve", lambda e: e.memset(Sb[:, :, :], 0.0), w=(K("gSb"),))
    sm = sbt("gsm", [64, 12, H], F32)
    bcr = sbt("gbcr", [64, 4, H, 64], F32)
    sq = sbt("gsq", [128, 2 * H, 64], BF16)
    qkn = sbt("gqkn", [128, 2 * H, 64], BF16)
    qd = sbt("gqd", [128, H, 64], BF16)
    eg128 = sbt("geg128", [128, H, 64], F32)
    eg128b = sbt("geg128b", [128, H, 64], BF16)
    Dm = sbt("gDm", [64, H, 64], F32)
    Gsb = sbt("gGsb", [64, H, 64], F32)
    bqs = sbt("gbqs", [128, 2 * H, 64], F32)
    BBs = sbt("gBBs", [64, H, 64], F32)
    E = sbt("gE", [64, H, 64], F32)
    dls = sbt("gdls", [64, H, 64], F32)
    dus = sbt("gdus", [64, H, 64], F32)
    dui = sbt("gdui", [64, H, 64], F32)
    Am = [sbt("gA", [64, H, 64], BF16) for _ in range(2)]
    IA = sbt("gIA", [64, H, 64], BF16)
    Bm = [sbt("gB", [64, H, 64], BF16) for _ in range(2)]
    Pt = [sbt("gPt", [64, H, 64], BF16) for _ in range(2)]
    intraT = sbt("gintraT", [64, H, 64], BF16)
    ktok = sbt("gktok", [64, H, 128], F32)
    vtok = sbt("gvtok", [64, H, 128], F32)
    vb = sbt("gvb", [64, H, 128], BF16)
    kbg = sbt("gkbg", [64, H, 128], BF16)
    kdec = sbt("gkdec", [64, H, 128], BF16)
    usb = sbt("gusb", [64, H, 128], F32)
    vnew = sbt("gvnew", [64, H, 128], BF16)
    wT = sbt("gwT", [128, H, 64], BF16)
    osb = usb
    osq = sbt("gosq", [64, H, 128], F32)
    zs = sbt("gzs", [64, H, 128], F32)
    oa = sbt("goa", [64, H, 128], BF16)
    oaT = [sbt("goaT", [128, H, 64], BF16) for _ in range(2)]
    ident64 = cst[0:64, C_IDENT:C_IDENT + 64]
    ones64 = cst[0:64, C_ONES:C_ONES + 128]
    bc3 = lambda ap2, n: ap2.unsqueeze(2).to_broadcast([ap2.shape[0], ap2.shape[1], n])
    mk3 = lambda col: cst[0:64, col:col + 64].unsqueeze(1).to_broadcast([64, H, 64])
    hj = lambda ap2: ap2.rearrange("p (h j) -> p h j", h=H)
    hd = lambda ap2: ap2.rearrange("p (h d) -> p h d", h=H)
    yield

    for n in range(nchunks):
        tt, cc = divmod(n, TT // C)
        tb = tt % 2
        xk = "gxt%d" % tb
        if cc == 0:
            if TG == "a":
                p.dma("sp", xt[tb][:, :, :], g.XT16[:, :, tt * TT:(tt + 1) * TT],
                      r=[("XT16", tt * 4 + j) for j in range(4)], w=(xk,))
            for c in range(3 * H):
                b = bank()
                for kc in range(8):
                    mm(p, PS[:, b, :], wqkv[:, kc, c * 128:(c + 1) * 128], xt[tb][:, kc, :], kc == 0, kc == 7,
                       r=(K("gwqkv"), xk), w=bk(b))
                p.op("dve", lambda e: e.tensor_copy(pc[:, 0:3], halo[:, c, :]), r=(K("ghalo"),), w=(K("gpc"),))
                p.op("act", lambda e: e.copy(pc[:, 3:], PS[:, b, :]), r=bk(b), w=(K("gpc"),))
                p.op("dve", lambda e: e.tensor_copy(halo[:, c, :], pc[:, TT:TT + 3]), r=(K("gpc"),), w=(K("ghalo"),))
                p.op("dve", lambda e: e.tensor_scalar(acc[:, :], pc[:, 0:TT], cw[:, c, 0:1], None, ALU.mult),
                     r=(K("gpc"), K("gcw")), w=(K("gacc"),))
                for j in range(1, 4):
                    p.op("dve", lambda e, j=j: e.scalar_tensor_tensor(acc[:, :], pc[:, j:j + TT], cw[:, c, j:j + 1],
                                                                       acc[:, :], ALU.mult, ALU.add),
                         r=(K("gpc"), K("gcw"), K("gacc")), w=(K("gacc"),))
                p.op("act", lambda e: e.activation(cv[:, c, :], acc[:, :], AF.Silu), r=(K("gacc"),), w=(K("gcv"),))
                yield
        cs = slice(cc * C, (cc + 1) * C)
        b0 = bank()
        for kc in range(8):
            mm(p, PS[0:64, b0, 0:2 * H], xt[tb][:, kc, cs], wab[:, kc, :], kc == 0, kc == 7, r=(K("gwab"), xk),
               w=bk(b0))
        p.op("dve", lambda e: e.tensor_tensor(sm[:, 0, :], PS[0:64, b0, 0:H], hp[:, 0:H], ALU.add),
             r=bk(b0) + [K("ghp")], w=(K("gsm0"),))
        p.op("act", lambda e: e.activation(sm[:, 0, :], sm[:, 0, :], AF.Exp), r=(K("gsm0"),), w=(K("gsm0"),))
        p.op("act", lambda e: e.activation(sm[:, 0, :], sm[:, 0, :], AF.Ln, bias=1.0), r=(K("gsm0"),),
             w=(K("gsm0"),))
        p.op("dve", lambda e: e.tensor_tensor(sm[:, 0, :], sm[:, 0, :], hp[:, H:2 * H], ALU.mult),
             r=(K("gsm0"), K("ghp")), w=(K("gsm0"),))
        p.op("act", lambda e: e.activation(sm[:, 1, :], PS[0:64, b0, H:2 * H], AF.Sigmoid), r=bk(b0),
             w=(K("gsm1"),))
        p.op("dve", lambda e: e.tensor_scalar(sm[:, 7, :], sm[:, 1, :], -1.0, None, ALU.mult), r=(K("gsm1"),),
             w=(K("gsm7"),))
        yield
        bG = bank()
        p.op("pe", lambda e: e.matmul(PS[0:64, bG, 0:H], cst[0:64, C_TRIU:C_TRIU + 64], sm[:, 0, :], start=True,
                                      stop=True), r=("cst", K("gsm0")), w=bk(bG))
        p.op("dve", lambda e: e.tensor_copy(sm[:, 2, :], PS[0:64, bG, 0:H]), r=bk(bG), w=(K("gsm2"),))
        p.op("act", lambda e: e.activation(sm[:, 3, :], sm[:, 2, :], AF.Exp), r=(K("gsm2"),), w=(K("gsm3"),))
        p.op("dve", lambda e: e.tensor_tensor(bcr[:, 0, :, :], mk3(C_TRIU), bc3(sm[:, 0, :], 64), ALU.mult),
             r=("cst", K("gsm0")), w=(K("gbcr0"),))
        bGb = bank()
        p.op("pe", lambda e: e.matmul(PS[:, bGb, 0:HC], ones64, bcr[:, 0, :, :].rearrange("p h j -> p (h j)"),
                                      start=True, stop=True), r=("cst", K("gbcr0")), w=bk(bGb))
        Gbc = hj(PS[:, bGb, 0:HC])
        p.op("act", lambda e: e.activation(eg128[:, :, :], Gbc, AF.Exp), r=bk(bGb), w=(K("geg128"),))
        p.op("act", lambda e: e.activation(eg128b[:, :, :], Gbc, AF.Exp), r=bk(bGb), w=(K("geg128b"),))
        p.op("act", lambda e: e.copy(Gsb[:, :, :], Gbc[0:64]), r=bk(bGb), w=(K("gGsb"),))
        yield
        p.op("dve", lambda e: e.tensor_tensor(Dm[:, :, :], Gsb[:, :, :], bc3(sm[:, 2, :], 64), ALU.subtract),
             r=(K("gGsb"), K("gsm2")), w=(K("gDm"),))
        p.op("dve", lambda e: e.tensor_tensor(sm[:, 4, :], Gsb[:, :, 63], sm[:, 2, :], ALU.subtract),
             r=(K("gGsb"), K("gsm2")), w=(K("gsm4"),))
        p.op("act", lambda e: e.activation(sm[:, 4, :], sm[:, 4, :], AF.Exp), r=(K("gsm4"),), w=(K("gsm4"),))
        p.op("act", lambda e: e.activation(Dm[:, :, :], Dm[:, :, :], AF.Abs), r=(K("gDm"),), w=(K("gDm"),))
        p.op("act", lambda e: e.activation(E[:, :, :], Dm[:, :, :], AF.Exp, scale=-1.0), r=(K("gDm"),), w=(K("gE"),))
        p.op("dve", lambda e: e.tensor_tensor(dls[:, :, :], E[:, :, :], mk3(C_LOWS), ALU.mult), r=(K("gE"), "cst"),
             w=(K("gdls"),))
        p.op("dve", lambda e: e.tensor_tensor(dus[:, :, :], E[:, :, :], mk3(C_UPS), ALU.mult), r=(K("gE"), "cst"),
             w=(K("gdus"),))
        p.op("dve", lambda e: e.tensor_tensor(dui[:, :, :], E[:, :, :], mk3(C_UPI), ALU.mult), r=(K("gE"), "cst"),
             w=(K("gdui"),))
        yield
        qk_raw = cv[:, 0:2 * H, cs]
        p.op("dve", lambda e: e.tensor_tensor(sq[:, :, :], qk_raw, qk_raw, ALU.mult), r=(K("gcv"),), w=(K("gsq"),))
        bs = bank()
        for h in range(2 * H):
            p.op("pe", lambda e, h=h: e.matmul(PS[0:64, bs, h:h + 1], sq[:, h, :], g.onesb[:, 0:1], start=True,
                                               stop=True), r=(K("gsq"), "onesb"), w=bk(bs))
        p.op("act", lambda e: e.activation(sm[:, 5:7, :], PS[0:64, bs, 0:2 * H].rearrange("p (a h) -> p a h", a=2),
                                           AF.Sqrt, bias=1e-6), r=bk(bs), w=(K("gsm5"), K("gsm6")))
        p.op("dve", lambda e: e.reciprocal(sm[:, 5:7, :], sm[:, 5:7, :]), r=(K("gsm5"), K("gsm6")),
             w=(K("gsm5"), K("gsm6")))
        p.op("dve", lambda e: e.tensor_scalar(sm[:, 5, :], sm[:, 5, :], 128.0 ** -0.5, None, ALU.mult),
             r=(K("gsm5"),), w=(K("gsm5"),))
        for i, src in ((1, 5), (2, 6), (3, 1)):
            p.op("dve", lambda e, i=i, src=src: e.tensor_tensor(
                bcr[:, i, :, :], ident64.unsqueeze(1).to_broadcast([64, H, 64]), bc3(sm[:, src, :], 64), ALU.mult),
                 r=("cst", K("gsm%d" % src)), w=(K("gbcr%d" % i),))
        yield
        bq = bank()
        for i in (1, 2):
            p.op("pe", lambda e, i=i: e.matmul(PS[:, bq, (i - 1) * HC:i * HC], ones64,
                                               bcr[:, i, :, :].rearrange("p h j -> p (h j)"), start=True, stop=True),
                 r=("cst", K("gbcr%d" % i)), w=bk(bq))
        p.op("act", lambda e: e.copy(bqs[:, :, :], PS[:, bq, 0:2 * HC].rearrange("p (a j) -> p a j", j=64)),
             r=bk(bq), w=(K("gbqs"),))
        p.op("dve", lambda e: e.tensor_tensor(qkn[:, :, :], qk_raw, bqs[:, :, :], ALU.mult),
             r=(K("gbqs"), K("gcv")), w=(K("gqkn"),))
        bB = bank()
        p.op("pe", lambda e: e.matmul(PS[0:64, bB, 0:HC], ones64[:, 0:64],
                                      bcr[:, 3, :, :].rearrange("p h j -> p (h j)"), start=True, stop=True),
             r=("cst", K("gbcr3")), w=bk(bB))
        p.op("act", lambda e: e.copy(BBs[:, :, :], hj(PS[0:64, bB, 0:HC])), r=bk(bB), w=(K("gBBs"),))
        p.op("dve", lambda e: e.tensor_tensor(qd[:, :, :], qkn[:, 0:H, :], eg128b[:, :, :], ALU.mult),
             r=(K("gqkn"), K("geg128b")), w=(K("gqd"),))
        yield
        bt = bank()
        PSb = PS[0:64, bt, :].bitcast(BF16)
        for h in range(H):
            p.op("pe", lambda e, h=h: e.transpose(PSb[:, h * 128:(h + 1) * 128], qkn[:, H + h, :], g.identb[:, :]),
                 r=(K("gqkn"), "identb"), w=bk(bt))
        for h in range(H):
            p.op("pe", lambda e, h=h: e.transpose(PSb[:, (H + h) * 128:(H + h + 1) * 128], cv[:, 2 * H + h, cs],
                                                  g.identb[:, :]), r=(K("gcv"), "identb"), w=bk(bt))
        p.op("act", lambda e: e.copy(ktok[:, :, :], hd(PSb[:, 0:H * 128])), r=bk(bt), w=(K("gktok"),))
        p.op("act", lambda e: e.copy(vtok[:, :, :], hd(PSb[:, H * 128:2 * H * 128])), r=bk(bt), w=(K("gvtok"),))
        p.op("dve", lambda e: e.tensor_tensor(vb[:, :, :], vtok[:, :, :], bc3(sm[:, 1, :], 128), ALU.mult),
             r=(K("gvtok"), K("gsm1")), w=(K("gvb"),))
        p.op("dve", lambda e: e.tensor_tensor(sm[:, 8, :], sm[:, 1, :], sm[:, 3, :], ALU.mult),
             r=(K("gsm1"), K("gsm3")), w=(K("gsm8"),))
        p.op("dve", lambda e: e.tensor_tensor(kbg[:, :, :], ktok[:, :, :], bc3(sm[:, 8, :], 128), ALU.mult),
             r=(K("gktok"), K("gsm8")), w=(K("gkbg"),))
        p.op("dve", lambda e: e.tensor_tensor(kdec[:, :, :], ktok[:, :, :], bc3(sm[:, 4, :], 128), ALU.mult),
             r=(K("gktok"), K("gsm4")), w=(K("gkdec"),))
        yield
        bKK = bank()
        bQK = bank()
        for h in range(H):
            mm(p, PS[0:64, bKK, h * 64:(h + 1) * 64], qkn[:, H + h, :], qkn[:, H + h, :], True, True,
               r=(K("gqkn"),), w=bk(bKK))
        for h in range(H):
            mm(p, PS[0:64, bQK, h * 64:(h + 1) * 64], qkn[:, H + h, :], qkn[:, h, :], True, True,
               r=(K("gqkn"),), w=bk(bQK))
        KKp = hj(PS[0:64, bKK, 0:HC])
        QKp = hj(PS[0:64, bQK, 0:HC])
        p.op("dve", lambda e: e.tensor_tensor(Dm[:, :, :], KKp, dls[:, :, :], ALU.mult), r=bk(bKK) + [K("gdls")],
             w=(K("gDm"),))
        p.op("dve", lambda e: e.tensor_tensor(Am[0][:, :, :], Dm[:, :, :], bc3(sm[:, 7, :], 64), ALU.mult),
             r=(K("gDm"), K("gsm7")), w=(K("gA0"),))
        p.op("dve", lambda e: e.tensor_tensor(Gsb[:, :, :], KKp, dus[:, :, :], ALU.mult), r=bk(bKK) + [K("gdus")],
             w=(K("gGsb"),))
        p.op("dve", lambda e: e.scalar_tensor_tensor(Bm[0][:, :, :], Gsb[:, :, :], -1.0, BBs[:, :, :], ALU.mult,
                                                     ALU.mult), r=(K("gGsb"), K("gBBs")), w=(K("gB0"),))
        p.op("dve", lambda e: e.tensor_tensor(intraT[:, :, :], QKp, dui[:, :, :], ALU.mult), r=bk(bQK) + [K("gdui")],
             w=(K("gintraT"),))
        p.op("dve", lambda e: e.tensor_tensor(Pt[0][:, :, :], Bm[0][:, :, :],
                                              ident64.unsqueeze(1).to_broadcast([64, H, 64]), ALU.add),
             r=(K("gB0"), "cst"), w=(K("gPt0"),))
        yield
        for k in range(5):
            ka, kb = k % 2, (k + 1) % 2
            bA = bank()
            for h in range(H):
                mm(p, PS[0:64, bA, h * 64:(h + 1) * 64], Bm[ka][:, h, :], Am[ka][:, h, :], True, True,
                   r=(K("gB%d" % ka), K("gA%d" % ka)), w=bk(bA))
            if k < 4:
                bBk = bank()
                for h in range(H):
                    mm(p, PS[0:64, bBk, h * 64:(h + 1) * 64], Am[ka][:, h, :], Bm[ka][:, h, :], True, True,
                       r=(K("gB%d" % ka), K("gA%d" % ka)), w=bk(bBk))
            p.op("act", lambda e: e.copy(Am[kb][:, :, :], hj(PS[0:64, bA, 0:HC])), r=bk(bA), w=(K("gA%d" % kb),))
            p.op("dve", lambda e: e.tensor_tensor(IA[:, :, :], Am[kb][:, :, :],
                                                  ident64.unsqueeze(1).to_broadcast([64, H, 64]), ALU.add),
                 r=(K("gA%d" % kb), "cst"), w=(K("gIA"),))
            if k < 4:
                p.op("act", lambda e: e.copy(Bm[kb][:, :, :], hj(PS[0:64, bBk, 0:HC])), r=bk(bBk),
                     w=(K("gB%d" % kb),))
            bP = bank()
            for h in range(H):
                mm(p, PS[0:64, bP, h * 64:(h + 1) * 64], IA[:, h, :], Pt[ka][:, h, :], True, True,
                   r=(K("gIA"), K("gPt%d" % ka)), w=bk(bP))
            p.op("dve", lambda e: e.tensor_copy(Pt[kb][:, :, :], hj(PS[0:64, bP, 0:HC])), r=bk(bP),
                 w=(K("gPt%d" % kb),))
            yield
        TT_ = Pt[1]
        tk = K("gPt1")
        bu = bank()
        for h in range(H):
            mm(p, PS[0:64, bu, h * 128:(h + 1) * 128], TT_[:, h, :], vb[:, h, :], True, True, r=(tk, K("gvb")),
               w=bk(bu))
        bw = bank()
        for h in range(H):
            mm(p, PS[:, bw, h * 64:(h + 1) * 64], kbg[:, h, :], TT_[:, h, :], True, True, r=(tk, K("gkbg")), w=bk(bw))
        p.op("act", lambda e: e.copy(usb[:, :, :], hd(PS[0:64, bu, :])), r=bk(bu), w=(K("gusb"),))
        p.op("act", lambda e: e.copy(wT[:, :, :], hj(PS[:, bw, 0:HC])), r=bk(bw), w=(K("gwT"),))
        yield
        bws = bank()
        for h in range(H):
            mm(p, PS[0:64, bws, h * 128:(h + 1) * 128], wT[:, h, :], Sb[:, h, :], True, True,
               r=(K("gwT"), K("gSb")), w=bk(bws))
        p.op("dve", lambda e: e.tensor_tensor(vnew[:, :, :], usb[:, :, :], hd(PS[0:64, bws, :]), ALU.subtract),
             r=bk(bws) + [K("gusb")], w=(K("gvnew"),))
        bo = bank()
        for h in range(H):
            oo = PS[0:64, bo, h * 128:(h + 1) * 128]
            mm(p, oo, qd[:, h, :], Sb[:, h, :], True, False, r=(K("gqd"), K("gSb")), w=bk(bo))
            mm(p, oo, intraT[:, h, :], vnew[:, h, :], False, True, r=(K("gintraT"), K("gvnew")), w=bk(bo))
        bd = bank()
        for h in range(H):
            mm(p, PS[:, bd, h * 128:(h + 1) * 128], kdec[:, h, :], vnew[:, h, :], True, True,
               r=(K("gkdec"), K("gvnew")), w=bk(bd))
        p.op("dve", lambda e: e.tensor_tensor(Sst[:, :, :], Sst[:, :, :],
                                              eg128[:, :, 63:64].to_broadcast([128, H, 128]), ALU.mult),
             r=(K("gS"), K("geg128")), w=(K("gS"),))
        p.op("dve", lambda e: e.tensor_tensor(Sst[:, :, :], Sst[:, :, :], hd(PS[:, bd, :]), ALU.add),
             r=bk(bd) + [K("gS")], w=(K("gS"),))
        p.op("act", lambda e: e.copy(Sb[:, :, :], Sst[:, :, :]), r=(K("gS"),), w=(K("gSb"),))
        p.op("act", lambda e: e.copy(osb[:, :, :], hd(PS[0:64, bo, :])), r=bk(bo), w=(K("gusb"),))
        yield
        bz = bank()
        for kc in range(8):
            mm(p, PS[0:64, bz, :], xt[tb][:, kc, cs], wz[:, kc, :], kc == 0, kc == 7, r=(K("gwz"), xk), w=bk(bz))
        p.op("act", lambda e: e.activation(zs[:, :, :], hd(PS[0:64, bz, :]), AF.Silu), r=bk(bz), w=(K("gzs"),))
        p.op("dve", lambda e: e.tensor_tensor(osq[:, :, :], osb[:, :, :], osb[:, :, :], ALU.mult), r=(K("gusb"),),
             w=(K("gosq"),))
        p.op("dve", lambda e: e.tensor_reduce(sm[:, 9, :], osq[:, :, :], AX.X, ALU.add), r=(K("gosq"),),
             w=(K("gsm9"),))
        p.op("dve", lambda e: e.tensor_scalar(sm[:, 9, :], sm[:, 9, :], 1.0 / 128.0, 1e-6, ALU.mult, ALU.add),
             r=(K("gsm9"),), w=(K("gsm9"),))
        p.op("act", lambda e: e.activation(sm[:, 9, :], sm[:, 9, :], AF.Sqrt), r=(K("gsm9"),), w=(K("gsm9"),))
        p.op("dve", lambda e: e.reciprocal(sm[:, 9, :], sm[:, 9, :]), r=(K("gsm9"),), w=(K("gsm9"),))
        p.op("dve", lambda e: e.tensor_tensor(osb[:, :, :], osb[:, :, :], bc3(sm[:, 9, :], 128), ALU.mult),
             r=(K("gusb"), K("gsm9")), w=(K("gusb"),))
        p.op("dve", lambda e: e.tensor_tensor(zs[:, :, :], zs[:, :, :],
                                              nw[:, :].unsqueeze(1).to_broadcast([64, H, 128]), ALU.mult),
             r=(K("gzs"), K("gnw")), w=(K("gzs"),))
        p.op("dve", lambda e: e.tensor_tensor(oa[:, :, :], osb[:, :, :], zs[:, :, :], ALU.mult),
             r=(K("gusb"), K("gzs")), w=(K("goa"),))
        bT = bank()
        PT = PS[:, bT, :].bitcast(BF16)
        ob = n % 2
        for h in range(H):
            p.op("pe", lambda e, h=h: e.transpose(PT[:, h * 64:(h + 1) * 64], oa[:, h, :], g.identb[0:64, 0:64]),
                 r=(K("goa"), "identb"), w=bk(bT))
        p.op("act", lambda e: e.copy(oaT[ob][:, :, :], hj(PT[:, 0:HC])), r=bk(bT), w=(K("goaT%d" % ob),))
        p.dma("sp", g.OAT[:, h0:h0 + H, n * C:(n + 1) * C], oaT[ob][:, :, :], r=(K("goaT%d" % ob),),
              w=((K("OAT"), n // 2),))
        yield


STAGE_W = {
    "init": [],
    "lru": ["w_in", "rg_conv_w", "rg_conv_b", "rg_w_a", "rg_b_a", "rg_w_x", "rg_b_x", "rg_lambda"],
    "swa": ["w_in", "attn_sinks", "rel_bias"],
    "gdn": ["w_in", "conv_qkv_w", "gdn_a_log", "gdn_dt_bias", "gdn_norm_w"],
    "merge": ["w_in", "w_o_gdn", "w_o_lru", "w_o_swa", "w_out", "ln1_g", "ln1_b"],
    "moe": ["router_w", "router_b", "w_gu", "b_gu", "w_down", "b_down", "ln2_g", "ln2_b"],
    "ple": ["ple_w_gate", "ple_w_proj", "ln3_g", "ln3_b"],
}


def needed_weights(stages):
    out = []
    for n in WEIGHT_SHAPES:
        if any(n in STAGE_W[s_] for s_ in stages):
            out.append(n)
    return out


class LN:
    def __init__(self, g, st, gname, bname, li, npst=1, tag=""):
        p = g.p
        self.g = g
        self.T = tag
        self.lg = p.sb(st, "lng", [128, D], F32)
        self.lb = p.sb(st, "lnb", [128, D], F32)
        p.dma("sp", self.lg[:, :], bcast_rows(g.W[gname], li * D, D, 128), w=("lng" + tag,))
        p.dma("sp", self.lb[:, :], bcast_rows(g.W[bname], li * D, D, 128), w=("lnb" + tag,))
        self.stats = p.sb(st, "lnst", [128, 2, 6], F32)
        self.mv = p.sb(st, "lnmv", [128, 2], F32)
        self.xo = [p.sb(st, "lnxo", [128, D], F32) for _ in range(2)]
        self.bufs = xt16_bufs(g, st, npst)
        self.i = 0

    def apply(self, z, zkey, t):
        g, p, T = self.g, self.g.p, self.T
        b = self.i % 2
        self.i += 1
        kst, kmv, klg, klb = "lnst" + T, "lnmv" + T, "lng" + T, "lnb" + T
        for hf in range(2):
            p.op("dve", lambda e: e.bn_stats(self.stats[:, hf, :], z[:, hf * 512:(hf + 1) * 512]), r=(zkey,),
                 w=(kst,))
        p.op("dve", lambda e: e.bn_aggr(self.mv[:, :], self.stats[:, :, :].rearrange("p a s -> p (a s)")),
             r=(kst,), w=(kmv,))
        p.op("act", lambda e: e.activation(self.mv[:, 1:2], self.mv[:, 1:2], AF.Sqrt, bias=1e-5), r=(kmv,), w=(kmv,))
        p.op("dve", lambda e: e.reciprocal(self.mv[:, 1:2], self.mv[:, 1:2]), r=(kmv,), w=(kmv,))
        xo = self.xo[b]
        xk = "lnxo%s%d" % (T, b)
        p.op("dve", lambda e: e.tensor_scalar(xo[:, :], z, self.mv[:, 0:1], self.mv[:, 1:2], ALU.subtract,
                                              ALU.mult), r=(zkey, kmv), w=(xk,))
        p.op("dve", lambda e: e.tensor_tensor(xo[:, :], xo[:, :], self.lg[:, :], ALU.mult), r=(xk, klg), w=(xk,))
        p.op("dve", lambda e: e.tensor_tensor(xo[:, :], xo[:, :], self.lb[:, :], ALU.add), r=(xk, klb), w=(xk,))
        p.dma("sp", g.X32[t * 128:(t + 1) * 128, :], xo[:, :], r=(xk,), w=(("X32", t),))
        to_xt16(g, None, xo[:, :], t, (xk,), self.bufs, self.i, tag=T)


def stage_merge(g, li):
    p = g.p
    W = g.W
    TT = 512
    with ExitStack() as st:
        wo = []
        for bi, n in enumerate(("w_o_gdn", "w_o_lru", "w_o_swa")):
            w_ = p.sb(st, "mwo", [128, 8, D], BF16)
            for kc in range(8):
                p.dma("pool", w_[:, kc, :], W[n][li, kc * 128:(kc + 1) * 128, :], w=("mwo%d" % bi,))
            wo.append(w_)
        wout = p.sb(st, "mwout", [128, 8, D], BF16)
        for kc in range(8):
            p.dma("pool", wout[:, kc, :], W["w_out"][li, kc * 128:(kc + 1) * 128, :], w=("mwout",))
        wg = [p.sb(st, "mwg", [128, 8, 128], BF16) for _ in range(2)]
        ln = LN(g, st, "ln1_g", "ln1_b", li, npst=1)
        xt = p.sb(st, "mxt", [128, 8, TT], BF16)
        ot = [p.sb(st, "mot", [128, 8, TT], BF16) for _ in range(3)]
        gs = [p.sb(st, "mgs", [128, TT], F32) for _ in range(2)]
        tmp = p.sb(st, "mtmp", [128, TT], F32)
        macc = p.sb(st, "macc", [128, TT], F32)
        mT = p.sb(st, "mmT", [128, 8, TT], BF16)
        x32 = [p.sb(st, "mx32", [128, D], F32) for _ in range(2)]
        z = [p.sb(st, "mz", [128, D], F32) for _ in range(2)]
        psg = [p.ps(st, "mpsg", [128, TT], F32) for _ in range(2)]
        psy = [p.ps(st, "mpsy", [128, TT], F32) for _ in range(2)]
        psm = p.ps(st, "mpsm", [128, 2, 512], F32)
        srcs = (("OAT", g.OAT), ("OBT", g.OBT), ("OCT", g.OCT))
        ig = 0
        for tt in range(S // TT):
            tk = [("XT16", tt * 4 + j) for j in range(4)]
            p.dma("sp", xt[:, :, :], g.XT16[:, :, tt * TT:(tt + 1) * TT], r=tk, w=("mxt",))
            for bi, (nm, src) in enumerate(srcs):
                p.dma("sp", ot[bi][:, :, :], src[:, :, tt * TT:(tt + 1) * TT],
                      r=[(nm, tt * 4 + j) for j in range(4)], w=("mot%d" % bi,))
            for c in range(8):
                for bi in range(3):
                    b = ig % 2
                    ig += 1
                    col = O_MG + bi * 1024 + c * 128
                    p.dma("pool", wg[b][:, :, :],
                          W["w_in"][li, :, col:col + 128].rearrange("(kc p) m -> p kc m", p=128), w=("mwg%d" % b,))
                    for kc in range(8):
                        mm(p, psg[b][:, :], wg[b][:, kc, :], xt[:, kc, :], kc == 0, kc == 7,
                           r=("mwg%d" % b, "mxt"), w=("mpsg%d" % b,))
                    for kc in range(8):
                        mm(p, psy[b][:, :], wo[bi][:, kc, c * 128:(c + 1) * 128], ot[bi][:, kc, :], kc == 0, kc == 7,
                           r=("mwo%d" % bi, "mot%d" % bi), w=("mpsy%d" % b,))
                    p.op("act", lambda e: e.activation(gs[b][:, :], psg[b][:, :], AF.Sigmoid), r=("mpsg%d" % b,),
                         w=("mgs%d" % b,))
                    if bi == 0:
                        p.op("dve", lambda e: e.tensor_tensor(macc[:, :], gs[b][:, :], psy[b][:, :], ALU.mult),
                             r=("mgs%d" % b, "mpsy%d" % b), w=("macc",))
                    else:
                        p.op("dve", lambda e: e.tensor_tensor(tmp[:, :], gs[b][:, :], psy[b][:, :], ALU.mult),
                             r=("mgs%d" % b, "mpsy%d" % b), w=("mtmp",))
                        if bi == 1:
                            p.op("dve", lambda e: e.tensor_tensor(macc[:, :], macc[:, :], tmp[:, :], ALU.add),
                                 r=("macc", "mtmp"), w=("macc",))
                        else:
                            p.op("dve", lambda e: e.tensor_tensor(mT[:, c, :], macc[:, :], tmp[:, :], ALU.add),
                                 r=("macc", "mtmp"), w=("mmT",))
            for nb in range(4):
                t = tt * 4 + nb
                b = t % 2
                p.dma("sp", x32[b][:, :], g.X32[t * 128:(t + 1) * 128, :], r=(("X32", t),), w=("mx32%d" % b,))
                for hf in range(2):
                    for kc in range(8):
                        mm(p, psm[:, hf, :], mT[:, kc, nb * 128:(nb + 1) * 128], wout[:, kc, hf * 512:(hf + 1) * 512],
                           kc == 0, kc == 7, r=("mmT", "mwout"), w=("mpsm",))
                p.op("dve", lambda e: e.scalar_tensor_tensor(z[b][:, :], x32[b][:, :], ALPHA,
                                                             psm[:, :, :].rearrange("p a f -> p (a f)"), ALU.mult,
                                                             ALU.add), r=("mx32%d" % b, "mpsm"), w=("mz%d" % b,))
                ln.apply(z[b][:, :], "mz%d" % b, t)
        p.barrier()


def stage_moe(g, li, nexp=NE, fuse_ple=False):
    p = g.p
    W = g.W
    cst = g.cst
    NG = 2
    GS = CAP // NG
    NB = CAP // 128
    with ExitStack() as st0:
        dest = st0.enter_context(g.nc.sbuf_tensor(p.name("qdest"), [128, NT, 4], I32))
        gate4 = st0.enter_context(g.nc.sbuf_tensor(p.name("qgate"), [128, NT, 4], F32))
        with ExitStack() as st:
            zt = p.sb(st, "qz", [128, D], BF16)
            p.op("dve", lambda e: e.memset(zt[:, :], 0.0), w=("qz",))
            for r0 in range(0, NE * CAP, 1024):
                p.dma("sp", g.XBUF[r0:r0 + 1024, :].rearrange("(a p) d -> p a d", p=128),
                      zt[:, :].unsqueeze(1).to_broadcast([128, 8, D]), r=("qz",), w=("XBUF",))
            rw = p.sb(st, "qrw", [128, 8, NE], F32)
            p.dma("sp", rw[:, :, :], W["router_w"][li].rearrange("(kc p) e -> p kc e", p=128), w=("qrw",))
            rb = p.sb(st, "qrb", [1, NE], F32)
            p.dma("sp", rb[:, :], W["router_b"][li:li + 1, :], w=("qrb",))
            cnt = p.sb(st, "qcnt", [1, NE], F32)
            p.op("dve", lambda e: e.memset(cnt[:, :], 0.0), w=("qcnt",))
            x32 = [p.sb(st, "qx32", [128, D], F32) for _ in range(2)]
            xb = [p.sb(st, "qxb", [128, D], BF16) for _ in range(2)]
            xTf = p.sb(st, "qxTf", [128, 8, 128], F32)
            lg = p.sb(st, "qlg", [128, NE], F32)
            top8 = p.sb(st, "qtop8", [128, 8], F32)
            mask = p.sb(st, "qmask", [128, NE], F32)
            vv = p.sb(st, "qvv", [128, NE], F32)
            junk = p.sb(st, "qjunk", [128, NE], F32)
            d4f = p.sb(st, "qd4f", [128, 4], F32)
            sm = p.sb(st, "qsm", [128, 4], F32)
            pT = p.ps(st, "qpT", [128, 2, 512], F32)
            pl = p.ps(st, "qpl", [128, NE], F32)
            pp = p.ps(st, "qpp", [128, NE], F32)
            pc = p.ps(st, "qpc", [1, NE], F32)
            ones1 = cst[0:1, C_ONES:C_ONES + 128]
            for t in range(NT):
                b = t % 2
                xk = "qx32%d" % b
                p.dma("sp", x32[b][:, :], g.X32[t * 128:(t + 1) * 128, :], r=(("X32", t),), w=(xk,))
                p.op("act", lambda e: e.copy(xb[b][:, :], x32[b][:, :]), r=(xk,), w=("qxb%d" % b,))
                for c in range(8):
                    p.op("pe", lambda e, c=c: e.transpose(pT[:, c // 4, (c % 4) * 128:(c % 4 + 1) * 128],
                                                          x32[b][:, c * 128:(c + 1) * 128], g.ident),
                         r=(xk, "cst"), w=("qpT",))
                p.op("dve", lambda e: e.tensor_copy(xTf[:, :, :], pT[:, :, :].rearrange("p a (c t) -> p (a c) t", c=4)),
                     r=("qpT",), w=("qxTf",))
                for kc in range(8):
                    mm(p, pl[:, :], xTf[:, kc, :], rw[:, kc, :], kc == 0, False, r=("qxTf", "qrw"), w=("qpl",))
                mm(p, pl[:, :], ones1, rb[:, :], False, True, r=("cst", "qrb"), w=("qpl",))
                p.op("act", lambda e: e.copy(lg[:, :], pl[:, :]), r=("qpl",), w=("qlg",))
                p.op("dve", lambda e: e.max(top8[:, :], lg[:, :]), r=("qlg",), w=("qtop8",))
                p.op("dve", lambda e: e.tensor_scalar(mask[:, :], lg[:, :], top8[:, 3:4], None, ALU.is_ge),
                     r=("qlg", "qtop8"), w=("qmask",))
                p.op("dve", lambda e: e.tensor_scalar(sm[:, :], top8[:, 0:4], top8[:, 0:1], None, ALU.subtract),
                     r=("qtop8",), w=("qsm",))
                p.op("act", lambda e: e.activation(sm[:, :], sm[:, :], AF.Exp), r=("qsm",), w=("qsm",))
                p.op("dve", lambda e: e.tensor_reduce(d4f[:, 0:1], sm[:, :], AX.X, ALU.add), r=("qsm",), w=("qd4f",))
                p.op("dve", lambda e: e.reciprocal(d4f[:, 0:1], d4f[:, 0:1]), r=("qd4f",), w=("qd4f",))
                p.op("dve", lambda e: e.tensor_scalar(gate4[:, t, :], sm[:, :], d4f[:, 0:1], None, ALU.mult),
                     r=("qsm", "qd4f"), w=("qgate",))
                mm(p, pp[:, :], cst[:, C_TRI128:C_TRI128 + 128], mask[:, :], True, False, r=("cst", "qmask"), w=("qpp",))
                mm(p, pp[:, :], ones1, cnt[:, :], False, True, r=("cst", "qcnt"), w=("qpp",))
                mm(p, pc[:, :], cst[:, C_ONES:C_ONES + 1], mask[:, :], True, True, r=("cst", "qmask"), w=("qpc",))
                p.op("dve", lambda e: e.tensor_tensor(vv[:, :], pp[:, :], cst[:, C_EBASE:C_EBASE + NE], ALU.add),
                     r=("qpp", "cst"), w=("qvv",))
                p.op("dve", lambda e: e.tensor_tensor(cnt[:, :], cnt[:, :], pc[:, :], ALU.add), r=("qcnt", "qpc"),
                     w=("qcnt",))
                for k in range(4):
                    p.op("dve", lambda e, k=k: e.scalar_tensor_tensor(junk[:, :], lg[:, :], top8[:, k:k + 1], vv[:, :],
                                                                       ALU.is_equal, ALU.mult,
                                                                       accum_out=d4f[:, k:k + 1]),
                         r=("qlg", "qtop8", "qvv", "qd4f"), w=("qjunk", "qd4f"))
                p.op("dve", lambda e: e.tensor_copy(dest[:, t, :], d4f[:, :]), r=("qd4f",), w=("qdest",))
                for k in range(4):
                    p.dma_custom("pool", lambda e, k=k: e.indirect_dma_start(
                        out=g.XBUF[:, :], out_offset=bass.IndirectOffsetOnAxis(ap=dest[:, t, k:k + 1], axis=0),
                        in_=xb[b][:, :], in_offset=None),
                        r=("qxb%d" % b, "qdest"), w=("XBUF",))
            p.barrier()
        with ExitStack() as st:
            wgu = [p.sb(st, "qwgu", [128, 8, 2 * D], BF16) for _ in range(2)]
            wdn = [p.sb(st, "qwdn", [128, 8, D], BF16) for _ in range(2)]
            bgu = [p.sb(st, "qbgu", [1, 2 * D], BF16) for _ in range(2)]
            bdn = [p.sb(st, "qbdn", [1, D], BF16) for _ in range(2)]
            xin = [p.sb(st, "qxin", [128, D], BF16) for _ in range(2)]
            xbT = p.sb(st, "qxbT", [128, 8, CAP], BF16)
            aT = p.sb(st, "qaT", [128, 8, CAP], BF16)
            ta = [p.sb(st, "qta", [128, GS], F32) for _ in range(2)]
            tsg = [p.sb(st, "qtsg", [128, GS], F32) for _ in range(2)]
            tl = [p.sb(st, "qtl", [128, GS], F32) for _ in range(2)]
            osb = [p.sb(st, "qosb", [128, D], F32) for _ in range(2)]
            ptr = [p.ps(st, "qptr", [128, D], BF16) for _ in range(2)]
            pg = [p.ps(st, "qpg", [128, GS], F32) for _ in range(2)]
            pln = [p.ps(st, "qpln", [128, GS], F32) for _ in range(2)]
            pd = p.ps(st, "qpd", [128, 2, 512], F32)
            onesb1 = g.onesb[0:1, :]
            ix = 0
            ia = 0
            io = 0
            for ei in range(nexp):
                wb = ei % 2
                kw = ("qwgu%d" % wb, "qwdn%d" % wb, "qbgu%d" % wb, "qbdn%d" % wb)
                for kc in range(8):
                    p.dma("pool", wgu[wb][:, kc, :], W["w_gu"][li, ei, kc * 128:(kc + 1) * 128, :], w=(kw[0],))
                    p.dma("pool", wdn[wb][:, kc, :], W["w_down"][li, ei, kc * 128:(kc + 1) * 128, :], w=(kw[1],))
                p.dma("pool", bgu[wb][:, :], W["b_gu"][li, ei:ei + 1, :], w=(kw[2],))
                p.dma("pool", bdn[wb][:, :], W["b_down"][li, ei:ei + 1, :], w=(kw[3],))
                for blk in range(NB):
                    b = ix % 2
                    ix += 1
                    r0 = ei * CAP + blk * 128
                    p.dma("sp", xin[b][:, :], g.XBUF[r0:r0 + 128, :], r=("XBUF",), w=("qxin%d" % b,))
                    for c in range(8):
                        p.op("pe", lambda e, c=c: e.transpose(ptr[b][:, c * 128:(c + 1) * 128],
                                                              xin[b][:, c * 128:(c + 1) * 128], g.identb[:, :]),
                             r=("qxin%d" % b, "identb"), w=("qptr%d" % b,))
                    p.op("act", lambda e: e.copy(xbT[:, :, blk * 128:(blk + 1) * 128],
                                                 ptr[b][:, :].rearrange("p (c t) -> p c t", c=8)),
                         r=("qptr%d" % b,), w=("qxbT",))
                for j in range(8):
                    for gi in range(NG):
                        b = ia % 2
                        ia += 1
                        sl = slice(gi * GS, (gi + 1) * GS)
                        for two, pst_, key in ((0, pg[b], "qpg%d" % b), (1, pln[b], "qpln%d" % b)):
                            c0 = 256 * j + two
                            for kc in range(8):
                                mm(p, pst_[:, :], wgu[wb][:, kc, c0:c0 + 255:2], xbT[:, kc, sl], kc == 0, False,
                                   r=(kw[0], "qxbT"), w=(key,))
                            mm(p, pst_[:, :], bgu[wb][0:1, c0:c0 + 255:2], g.onesb[0:1, 0:GS], False, True,
                               r=(kw[2], "onesb"), w=(key,))
                        p.op("dve", lambda e: e.tensor_scalar(ta[b][:, :], pg[b][:, :], 7.0, None, ALU.min),
                             r=("qpg%d" % b,), w=("qta%d" % b,))
                        p.op("act", lambda e: e.activation(tsg[b][:, :], ta[b][:, :], AF.Sigmoid, scale=1.702),
                             r=("qta%d" % b,), w=("qtsg%d" % b,))
                        p.op("dve", lambda e: e.tensor_scalar(tl[b][:, :], pln[b][:, :], -7.0, 7.0, ALU.max, ALU.min),
                             r=("qpln%d" % b,), w=("qtl%d" % b,))
                        p.op("dve", lambda e: e.tensor_tensor(ta[b][:, :], ta[b][:, :], tsg[b][:, :], ALU.mult),
                             r=("qta%d" % b, "qtsg%d" % b), w=("qta%d" % b,))
                        p.op("dve", lambda e: e.scalar_tensor_tensor(aT[:, j, sl], tl[b][:, :], 1.0, ta[b][:, :],
                                                                      ALU.add, ALU.mult),
                             r=("qtl%d" % b, "qta%d" % b), w=("qaT",))
                for blk in range(NB):
                    b = io % 2
                    io += 1
                    for hf in range(2):
                        for kc in range(8):
                            mm(p, pd[:, hf, :], aT[:, kc, blk * 128:(blk + 1) * 128],
                               wdn[wb][:, kc, hf * 512:(hf + 1) * 512], kc == 0, False, r=("qaT", kw[1]), w=("qpd",))
                        mm(p, pd[:, hf, :], g.onesb[0:1, 0:128], bdn[wb][0:1, hf * 512:(hf + 1) * 512], False, True,
                           r=("onesb", kw[3]), w=("qpd",))
                    p.op("act", lambda e: e.copy(osb[b][:, :], pd[:, :, :].rearrange("p a f -> p (a f)")),
                         r=("qpd",), w=("qosb%d" % b,))
                    r0 = ei * CAP + blk * 128
                    p.dma("sp", g.OBUF[r0:r0 + 128, :], osb[b][:, :], r=("qosb%d" % b,), w=("OBUF",))
            p.barrier()
        if fuse_ple:
            run_streams(p, [stream_combine(g, li, dest, gate4), stream_ple(g, li)], lags=[0.0, 2.5 / NT])
        else:
            run_streams(p, [stream_combine(g, li, dest, gate4)])


def stream_combine(g, li, dest, gate4):
    p = g.p
    with ExitStack() as st:
        ln = LN(g, st, "ln2_g", "ln2_b", li, npst=1, tag="c")
        x32 = [p.sb(st, "qcx", [128, D], F32) for _ in range(2)]
        gt = [p.sb(st, "qgt", [128, D], F32) for _ in range(4)]
        z = [p.sb(st, "qcz", [128, D], F32) for _ in range(2)]
        ig = 0
        yield
        for t in range(NT):
            b = t % 2
            p.dma("sp", x32[b][:, :], g.X32[t * 128:(t + 1) * 128, :], r=(("X32", t),), w=("qcx%d" % b,))
            p.op("act", lambda e: e.activation(z[b][:, :], x32[b][:, :], AF.Copy, scale=ALPHA),
                 r=("qcx%d" % b,), w=("qcz%d" % b,))
            for k in range(4):
                gb = ig % 4
                ig += 1
                p.dma_custom("pool", lambda e, k=k: e.indirect_dma_start(
                    out=gt[gb][:, :], out_offset=None, in_=g.OBUF[:, :],
                    in_offset=bass.IndirectOffsetOnAxis(ap=dest[:, t, k:k + 1], axis=0)),
                    r=("OBUF", "qdest"), w=("qgt%d" % gb,))
                p.op("dve", lambda e, k=k: e.scalar_tensor_tensor(z[b][:, :], gt[gb][:, :], gate4[:, t, k:k + 1],
                                                                   z[b][:, :], ALU.mult, ALU.add),
                     r=("qgt%d" % gb, "qgate", "qcz%d" % b), w=("qcz%d" % b,))
            ln.apply(z[b][:, :], "qcz%d" % b, t)


def stream_ple(g, li):
    p = g.p
    W = g.W
    with ExitStack() as st:
        wg = p.sb(st, "pwg", [128, 8, D], BF16)
        for kc in range(8):
            p.dma("pool", wg[:, kc, :], W["ple_w_gate"][li, kc * 128:(kc + 1) * 128, :], w=("pwg",))
        wp = p.sb(st, "pwp", [128, 2, D], BF16)
        for kc in range(2):
            p.dma("pool", wp[:, kc, :], W["ple_w_proj"][li, kc * 128:(kc + 1) * 128, :], w=("pwp",))
        ln = LN(g, st, "ln3_g", "ln3_b", li, npst=1, tag="p")
        xt = [p.sb(st, "pxt", [128, 8, 128], BF16) for _ in range(2)]
        x32 = [p.sb(st, "px32", [128, D], F32) for _ in range(2)]
        pin = [p.sb(st, "ppin", [128, 256], F32) for _ in range(2)]
        pinb = [p.sb(st, "ppinb", [128, 256], BF16) for _ in range(2)]
        pT = [p.sb(st, "ppT", [128, 2, 128], BF16) for _ in range(2)]
        sg = [p.sb(st, "psg", [128, D], F32) for _ in range(2)]
        z = [p.sb(st, "pz", [128, D], F32) for _ in range(2)]
        psg = p.ps(st, "ppsg", [128, 2, 512], F32)
        psp = p.ps(st, "ppsp", [128, 2, 512], F32)
        pst = p.ps(st, "ppst", [128, 256], BF16)
        yield
        for t in range(NT):
            b = t % 2
            kb = str(b)
            p.dma("sp", xt[b][:, :, :], g.XT16[:, :, t * 128:(t + 1) * 128], r=(("XT16", t),), w=("pxt" + kb,))
            p.dma("sp", x32[b][:, :], g.X32[t * 128:(t + 1) * 128, :], r=(("X32", t),), w=("px32" + kb,))
            p.dma("sp", pin[b][:, :], g.pp[li, t * 128:(t + 1) * 128, :], w=("ppin" + kb,))
            p.op("act", lambda e: e.copy(pinb[b][:, :], pin[b][:, :]), r=("ppin" + kb,), w=("ppinb" + kb,))
            for c in range(2):
                p.op("pe", lambda e, c=c: e.transpose(pst[:, c * 128:(c + 1) * 128], pinb[b][:, c * 128:(c + 1) * 128],
                                                      g.identb[:, :]), r=("ppinb" + kb, "identb"), w=("ppst",))
            p.op("dve", lambda e: e.tensor_copy(pT[b][:, :, :], pst[:, :].rearrange("p (c t) -> p c t", c=2)),
                 r=("ppst",), w=("ppT" + kb,))
            for hf in range(2):
                for kc in range(8):
                    mm(p, psg[:, hf, :], xt[b][:, kc, :], wg[:, kc, hf * 512:(hf + 1) * 512], kc == 0, kc == 7,
                       r=("pxt" + kb, "pwg"), w=("ppsg",))
            for hf in range(2):
                for kc in range(2):
                    mm(p, psp[:, hf, :], pT[b][:, kc, :], wp[:, kc, hf * 512:(hf + 1) * 512], kc == 0, kc == 1,
                       r=("ppT" + kb, "pwp"), w=("ppsp",))
            p.op("act", lambda e: e.activation(sg[b][:, :], psg[:, :, :].rearrange("p a f -> p (a f)"), AF.Sigmoid),
                 r=("ppsg",), w=("psg" + kb,))
            p.op("dve", lambda e: e.tensor_tensor(sg[b][:, :], sg[b][:, :], psp[:, :, :].rearrange("p a f -> p (a f)"),
                                                  ALU.mult), r=("psg" + kb, "ppsp"), w=("psg" + kb,))
            p.op("dve", lambda e: e.scalar_tensor_tensor(z[b][:, :], x32[b][:, :], ALPHA, sg[b][:, :], ALU.mult,
                                                         ALU.add), r=("px32" + kb, "psg" + kb), w=("pz" + kb,))
            ln.apply(z[b][:, :], "pz" + kb, t)


def stage_ple(g, li):
    run_streams(g.p, [stream_ple(g, li)])


ENABLED_STAGES = ("init", "lru", "swa", "gdn", "merge", "moe", "ple")


def kernel(**inputs):
    n = 8
    nc, g = build_program(nlayers=DEPTH, stages=ENABLED_STAGES)
    consts = make_consts()
    in_maps = []
    for c in range(n):
        m = {"x": np.ascontiguousarray(inputs["x"][c], dtype=np.float32), "consts": consts}
        if "ple" in ENABLED_STAGES:
            m["p"] = np.ascontiguousarray(np.asarray(inputs["p"])[:, c], dtype=np.float32)
        for name in needed_weights(ENABLED_STAGES):
            m[name] = np.ascontiguousarray(inputs[name], dtype=np.float32)
        in_maps.append(m)
    res = run_bass_kernel_spmd(nc, in_maps, core_ids=list(range(n)))
    return np.stack([np.asarray(r["out"], dtype=np.float32) for r in res.results], axis=0)
```

```python
import math
from contextlib import ExitStack
import numpy as np
import concourse.bass as bass
import concourse.mybir as mybir
from concourse.bass_utils import run_bass_kernel_spmd

F32 = mybir.dt.float32
BF16 = mybir.dt.bfloat16
I32 = mybir.dt.int32
U32 = mybir.dt.uint32
AF = mybir.ActivationFunctionType
ALU = mybir.AluOpType
AX = mybir.AxisListType

S = 4096
D = 1024
NT = S // 128
DEPTH = 2
IN_COLS = 10768
ALPHA = (2.0 * DEPTH) ** 0.25
O_GQ, O_GK, O_GV, O_GZ, O_GA, O_GB = 0, 1024, 2048, 3072, 4096, 4104
O_LX, O_LG = 4112, 5136
O_SQ, O_SK, O_SV = 6160, 7184, 7440
O_MG = 7696


import os as _os
NOSELF = _os.environ.get("NOSELF", "0") == "1"


class _Rec:
    def __init__(self):
        self.call = None

    def __getattr__(self, name):
        def f(*a, **k):
            self.call = (name, a, k)
            return self
        return f


class Prog:
    def __init__(self, nc):
        self.nc = nc
        self.engs = {"pe": nc.tensor, "act": nc.scalar, "dve": nc.vector, "pool": nc.gpsimd, "sp": nc.sync}
        self.sem = {}
        self.cnt = {}
        self.es = ExitStack()
        for e in self.engs:
            self.sem[e] = self.es.enter_context(nc.semaphore("s_" + e))
            self.cnt[e] = 0
        self.ndsem = 8
        self.dsem = {}
        self.dcnt = {}
        self.dnext = {}
        for q in ("sp", "pool", "act"):
            self.dsem[q] = [self.es.enter_context(nc.semaphore("d_%s%d" % (q, i))) for i in range(self.ndsem)]
            self.dcnt[q] = [0] * self.ndsem
            self.dnext[q] = 0
        self.waited = {}
        self.last_w = {}
        self.readers = {}
        self.uid = 0
        self.all_tokens = {}
        self.recording = False
        self.recbuf = []

    def name(self, n):
        self.uid += 1
        return "%s_%d" % (n, self.uid)

    def sb(self, st, n, shape, dt):
        return st.enter_context(self.nc.sbuf_tensor(self.name(n), list(shape), dt))

    def ps(self, st, n, shape, dt):
        return st.enter_context(self.nc.psum_tensor(self.name(n), list(shape), dt))

    def dram(self, n, shape, dt):
        return self.nc.dram_tensor(n, list(shape), dt).ap()

    def _deps(self, r, w):
        deps = {}

        def add(tok):
            s, v = tok
            if deps.get(s, 0) < v:
                deps[s] = v

        for k in r:
            t = self.last_w.get(k)
            if t is not None:
                add(t)
        for k in w:
            t = self.last_w.get(k)
            if t is not None:
                add(t)
            for s, v in self.readers.get(k, {}).items():
                add((s, v))
        return deps

    def _wait(self, e, deps):
        eng = self.engs[e]
        for s, v in deps.items():
            if e == "pe" and s is self.sem["pe"]:
                continue
            if NOSELF and e in self.sem and s is self.sem[e]:
                continue
            key = (e, s.name)
            if self.waited.get(key, 0) < v:
                eng.wait_ge(s, v)
                self.waited[key] = v

    def _record(self, tok, r, w):
        s, v = tok
        self.all_tokens[s.name] = (s, v)
        for k in r:
            d = self.readers.setdefault(k, {})
            if d.get(s, 0) < v:
                d[s] = v
        for k in w:
            self.last_w[k] = tok
            self.readers[k] = {}

    def op(self, e, fn, r=(), w=()):
        if self.recording:
            rec = _Rec()
            fn(rec)
            self.recbuf.append(("op", e, rec.call, tuple(r), tuple(w)))
            return
        self._wait(e, self._deps(r, w))
        inst = fn(self.engs[e])
        self.cnt[e] += 1
        inst.then_inc(self.sem[e], 1)
        self._record((self.sem[e], self.cnt[e]), r, w)

    def emit(self, item):
        if item[0] == "op":
            _, e, (name, a, k), r, w = item
            self.op(e, lambda eng: getattr(eng, name)(*a, **k), r, w)
        elif item[0] == "dmac":
            _, q, (name, a, k), r, w = item
            self.dma_custom(q, lambda eng: getattr(eng, name)(*a, **k), r, w)
        else:
            _, q, out, in_, r, w, kw = item
            self.dma(q, out, in_, r, w, **kw)

    def dma(self, q, out, in_, r=(), w=(), **kw):
        if self.recording:
            self.recbuf.append(("dma", q, out, in_, tuple(r), tuple(w), kw))
            return
        deps = self._deps(r, w)
        j = self.dnext[q]
        self.dnext[q] = (j + 1) % self.ndsem
        s = self.dsem[q][j]
        if self.dcnt[q][j] > 0:
            deps[s] = max(deps.get(s, 0), 16 * self.dcnt[q][j])
        self._wait(q, deps)
        self.engs[q].dma_start(out=out, in_=in_, **kw).then_inc(s, 16)
        self.dcnt[q][j] += 1
        self._record((s, 16 * self.dcnt[q][j]), r, w)

    def dma_custom(self, q, fn, r=(), w=()):
        if self.recording:
            rec = _Rec()
            fn(rec)
            self.recbuf.append(("dmac", q, rec.call, tuple(r), tuple(w)))
            return
        deps = self._deps(r, w)
        j = self.dnext[q]
        self.dnext[q] = (j + 1) % self.ndsem
        s = self.dsem[q][j]
        if self.dcnt[q][j] > 0:
            deps[s] = max(deps.get(s, 0), 16 * self.dcnt[q][j])
        self._wait(q, deps)
        fn(self.engs[q]).then_inc(s, 16)
        self.dcnt[q][j] += 1
        self._record((s, 16 * self.dcnt[q][j]), r, w)

    def barrier(self):
        toks = {n: sv for n, sv in self.all_tokens.items()}
        for e in self.engs:
            d = {}
            for n, (s, v) in toks.items():
                d[s] = v
            self._wait(e, d)

    def finish(self):
        self.barrier()
        self.es.close()


def run_streams(p, gens, lags=None):
    lags = lags or [0.0] * len(gens)
    bufs = []
    for g_ in gens:
        p.recbuf = []
        p.recording = True
        next(g_)
        p.recording = False
        bufs.append(p.recbuf)
    for i_ in reversed(range(len(gens))):
        g_ = gens[i_]
        p.recbuf = bufs[i_]
        p.recording = True
        for _ in g_:
            pass
        p.recording = False
    items = []
    for si, b_ in enumerate(bufs):
        n_ = max(1, len(b_))
        for i_, it_ in enumerate(b_):
            items.append(((i_ + 0.5) / n_ + lags[si], si, i_, it_))
    items.sort(key=lambda t_: (t_[0], t_[1], t_[2]))
    for _, _, _, it_ in items:
        p.emit(it_)
    p.barrier()


def mm(p, out, lhsT, rhs, start, stop, r, w):
    p.op("pe", lambda e: e.matmul(out, lhsT, rhs, start=start, stop=stop), r=r, w=w)


class K:
    pass


def build_program(nlayers=DEPTH, stages=("init", "lru", "swa", "gdn", "merge", "moe", "ple"), dbg=None,
                  gather=False):
    nc = bass.Bass("TRN2", target_bir_lowering=False)
    p = Prog(nc)
    g = K()
    g.nc, g.p = nc, p
    ein = lambda n, shape, dt=F32: nc.dram_tensor(n, list(shape), dt, kind="ExternalInput").ap()
    g.x = ein("x", [S, D])
    if "ple" in stages:
        g.pp = ein("p", [DEPTH, S, 256])
    g.consts = ein("consts", [128, CONST_COLS])
    W = {}
    for n in needed_weights(stages):
        W[n] = ein(n, WEIGHT_SHAPES[n])
    g.W = W
    g.out = nc.dram_tensor("out", [S, D], F32, kind="ExternalOutput").ap()
    g.X32 = p.dram("X32", [S, D], F32)
    g.XT16 = p.dram("XT16", [128, 8, S], BF16)
    g.OAT = p.dram("OAT", [128, 8, S], BF16)
    g.OBT = p.dram("OBT", [128, 8, S], BF16)
    g.OCT = p.dram("OCT", [128, 8, S], BF16)
    g.EXT_h = nc.dram_tensor("EXT", [16, 383], F32)
    g.XBUF = p.dram("XBUF", [NE * CAP + S, D], BF16)
    g.OBUF = p.dram("OBUF", [NE * CAP + S, D], F32)
    g.dbg = {}
    if dbg:
        for n, (shape, dt) in dbg.items():
            g.dbg[n] = nc.dram_tensor("dbg_" + n, list(shape), dt, kind="ExternalOutput").ap()
    with nc.Block() as block, ExitStack() as gst:
        g.cst = p.sb(gst, "cst", [128, CONST_COLS], F32)
        p.dma("sp", g.cst[:, :], g.consts, r=(), w=("cst",))
        g.ident = g.cst[:, C_IDENT:C_IDENT + 128]
        g.identb = p.sb(gst, "identb", [128, 128], BF16)
        p.op("dve", lambda e: e.tensor_copy(g.identb[:, :], g.ident), r=("cst",), w=("identb",))
        g.onesb = p.sb(gst, "onesb", [128, 512], BF16)
        p.op("dve", lambda e: e.memset(g.onesb[:, :], 1.0), w=("onesb",))
        if "init" in stages:
            stage_init(g)
        for li in range(nlayers):
            if "lru" in stages:
                stage_lru(g, li)
            if "swa" in stages:
                stage_swa(g, li)
            if "gdn" in stages:
                stage_gdn(g, li)
            if "merge" in stages:
                stage_merge(g, li)
            if "moe" in stages:
                stage_moe(g, li, fuse_ple=("ple" in stages))
            elif "ple" in stages:
                stage_ple(g, li)
        for n in g.dbg:
            src = getattr(g, n)
            p.dma("sp", g.dbg[n], src, r=[(n, t) for t in range(NT)], w=("dbg_" + n,))
        for t in range(0, NT, 4):
            p.dma("sp", g.out[t * 128:(t + 4) * 128, :], g.X32[t * 128:(t + 4) * 128, :],
                  r=[("X32", t + j) for j in range(4)], w=(("out", t),))
        p.finish()
    return nc, g


C_IDENT = 0
C_OHX = 128
C_ANTI = 512
C_TRIU = 640
C_LOWS = 704
C_UPS = 768
C_UPI = 832
C_ONES = 896
C_TRI128 = 1024
C_EBASE = 1152
CONST_COLS = 1184
CAP = 768
NE = 32
BIGIDX = 1048576.0
NEG = -30000.0


def _t5_bucket(dist):
    max_exact = 16
    d = np.maximum(dist.astype(np.float32), np.float32(1.0))
    large = max_exact + (np.log(d / np.float32(max_exact)) / np.float32(math.log(128 / max_exact))
                         * np.float32(32 - max_exact)).astype(np.int32)
    large = np.minimum(large, 31)
    return np.where(dist < max_exact, dist, large)


def make_consts():
    c = np.zeros((128, CONST_COLS), np.float32)
    c[:, C_IDENT:C_IDENT + 128] = np.eye(128, dtype=np.float32)
    c[:, C_ANTI:C_ANTI + 128] = np.eye(128, dtype=np.float32)[::-1]
    r = np.arange(64)[:, None]
    cc = np.arange(64)[None, :]
    c[0:64, C_TRIU:C_TRIU + 64] = (r <= cc)
    c[0:64, C_LOWS:C_LOWS + 64] = (r > cc)
    c[0:64, C_UPS:C_UPS + 64] = (cc > r)
    c[0:64, C_UPI:C_UPI + 64] = (cc >= r)
    c[:, C_ONES:C_ONES + 128] = 1.0
    c[:, C_TRI128:C_TRI128 + 128] = (np.arange(128)[:, None] < np.arange(128)[None, :])
    c[:, C_EBASE:C_EBASE + 32] = (np.arange(32) * CAP)[None, :]
    for j in range(383):
        dist = j - 127
        if 0 <= dist < 128:
            c[int(_t5_bucket(np.array([dist]))[0]), C_OHX + j] = 1.0
        else:
            c[32, C_OHX + j] = NEG
    return c


WEIGHT_SHAPES = {
    "w_in": [DEPTH, D, IN_COLS],
    "rg_conv_w": [DEPTH, 4, D], "rg_conv_b": [DEPTH, D], "rg_w_a": [DEPTH, 8, 128, 128], "rg_b_a": [DEPTH, D],
    "rg_w_x": [DEPTH, 8, 128, 128], "rg_b_x": [DEPTH, D], "rg_lambda": [DEPTH, D],
    "attn_sinks": [DEPTH, 16], "rel_bias": [32, 16],
    "w_o_gdn": [DEPTH, D, D], "w_o_lru": [DEPTH, D, D], "w_o_swa": [DEPTH, D, D], "w_out": [DEPTH, D, D],
    "ln1_g": [DEPTH, D], "ln1_b": [DEPTH, D],
    "router_w": [DEPTH, D, NE], "router_b": [DEPTH, NE], "w_gu": [DEPTH, NE, D, 2 * D], "b_gu": [DEPTH, NE, 2 * D],
    "w_down": [DEPTH, NE, D, D], "b_down": [DEPTH, NE, D], "ln2_g": [DEPTH, D], "ln2_b": [DEPTH, D],
    "ple_w_gate": [DEPTH, D, D], "ple_w_proj": [DEPTH, 256, D], "ln3_g": [DEPTH, D], "ln3_b": [DEPTH, D],
    "conv_qkv_w": [DEPTH, 4, 3072], "gdn_a_log": [DEPTH, 8], "gdn_dt_bias": [DEPTH, 8], "gdn_norm_w": [DEPTH, 128],
}


def to_xt16(g, st, xt_sb_f32, tile_idx, keys_r, bufs, i, dst=None, dname="XT16", cast=True, tag=""):
    p = g.p
    xb, pst, xtb = bufs
    b = i % 2
    pb = b % len(pst)
    if dst is None:
        dst = g.XT16
    if cast:
        p.op("act", lambda e: e.copy(xb[b][:, :], xt_sb_f32), r=keys_r, w=("xb%s%d" % (tag, b),))
    else:
        p.op("pool", lambda e: e.tensor_copy(xb[b][:, :], xt_sb_f32), r=keys_r, w=("xb%s%d" % (tag, b),))
    for c in range(8):
        p.op("pe", lambda e: e.transpose(pst[pb][:, c * 128:(c + 1) * 128], xb[b][:, c * 128:(c + 1) * 128],
                                         g.identb[:, :]), r=("xb%s%d" % (tag, b), "identb"), w=("pst%s%d" % (tag, pb),))
    p.op("dve", lambda e: e.tensor_copy(xtb[b][:, :], pst[pb][:, :]), r=("pst%s%d" % (tag, pb),), w=("xtb%s%d" % (tag, b),))
    p.dma("sp", dst[:, :, tile_idx * 128:(tile_idx + 1) * 128],
          xtb[b][:, :].rearrange("p (c t) -> p c t", c=8), r=("xtb%s%d" % (tag, b),), w=((dname, tile_idx),))


def xt16_bufs(g, st, npst=2):
    p = g.p
    xb = [p.sb(st, "xb", [128, D], BF16) for _ in range(2)]
    pst = [p.ps(st, "pst", [128, D], BF16) for _ in range(npst)]
    xtb = [p.sb(st, "xtb", [128, D], BF16) for _ in range(2)]
    return xb, pst, xtb


def stage_init(g):
    p = g.p
    with ExitStack() as st:
        xin = [p.sb(st, "xin", [128, D], F32) for _ in range(2)]
        bufs = xt16_bufs(g, st)
        for t in range(NT):
            b = t % 2
            p.dma("sp", xin[b][:, :], g.x[t * 128:(t + 1) * 128, :], r=(), w=("xin%d" % b,))
            p.dma("sp", g.X32[t * 128:(t + 1) * 128, :], xin[b][:, :], r=("xin%d" % b,), w=(("X32", t),))
            to_xt16(g, st, xin[b][:, :], t, ("xin%d" % b,), bufs, t)
        p.barrier()


def stream_lru(g, li):
    p = g.p
    W = g.W
    TT = 512
    with ExitStack() as st:
        prm = p.sb(st, "lprm", [128, 8, 8], F32)
        for k in range(4):
            p.dma("sp", prm[:, :, k], W["rg_conv_w"][li, k, :].rearrange("(c p) -> p c", p=128),
                  w=("lprm",), allow_slow_non_contiguous=True)
        for k, n in ((4, "rg_conv_b"), (5, "rg_b_a"), (6, "rg_b_x"), (7, "rg_lambda")):
            p.dma("sp", prm[:, :, k], W[n][li, :].rearrange("(c p) -> p c", p=128), w=("lprm",),
                  allow_slow_non_contiguous=True)
        nsp = p.sb(st, "nsp", [128, 8], F32)
        p.op("act", lambda e: e.activation(nsp[:, :], prm[:, :, 7], AF.Exp, scale=-1.0), r=("lprm",), w=("nsp",))
        p.op("act", lambda e: e.activation(nsp[:, :], nsp[:, :], AF.Ln, bias=1.0), r=("nsp",), w=("nsp",))
        p.op("dve", lambda e: e.tensor_scalar(nsp[:, :], nsp[:, :], -8.0, None, ALU.mult), r=("nsp",), w=("nsp",))
        wx = p.sb(st, "lwx", [128, 8, 2048], BF16)
        for kc in range(8):
            p.dma("pool", wx[:, kc, :], W["w_in"][li, kc * 128:(kc + 1) * 128, O_LX:O_LX + 2048], w=("lwx",))
        wa = p.sb(st, "lwa", [128, 8, 128], BF16)
        wxx = p.sb(st, "lwxx", [128, 8, 128], BF16)
        p.dma("pool", wa[:, :, :], W["rg_w_a"][li].rearrange("h i j -> i h j"), w=("lwa",))
        p.dma("pool", wxx[:, :, :], W["rg_w_x"][li].rearrange("h i j -> i h j"), w=("lwxx",))
        xt = [p.sb(st, "lxt", [128, 8, TT], BF16) for _ in range(2)]
        xl = [p.sb(st, "lxl", [128, TT + 3], F32) for _ in range(2)]
        xc = [p.sb(st, "lxc", [128, TT], F32) for _ in range(2)]
        xcb = [p.sb(st, "lxcb", [128, TT], BF16) for _ in range(2)]
        ra = [p.sb(st, "lra", [128, TT], F32) for _ in range(2)]
        ri = [p.sb(st, "lri", [128, TT], F32) for _ in range(2)]
        gu = [p.sb(st, "lgu", [128, TT], F32) for _ in range(2)]
        hh = [p.sb(st, "lhh", [128, TT], F32) for _ in range(2)]
        ob = [p.sb(st, "lob", [128, 8, TT], BF16) for _ in range(2)]
        halo = p.sb(st, "lhalo", [128, 8, 3], F32)
        hst = p.sb(st, "lhst", [128, 8], F32)
        p.op("dve", lambda e: e.memset(halo[:, :, :], 0.0), w=("lhalo",))
        p.op("dve", lambda e: e.memset(hst[:, :], 0.0), w=("lhst",))
        ps1 = [p.ps(st, "lps1", [128, TT], F32) for _ in range(2)]
        ps2 = [p.ps(st, "lps2", [128, TT], F32) for _ in range(2)]
        ps3 = [p.ps(st, "lps3", [128, TT], F32) for _ in range(2)]
        it = 0
        yield
        for tt in range(S // TT):
            tb = tt % 2
            p.dma("sp", xt[tb][:, :, :], g.XT16[:, :, tt * TT:(tt + 1) * TT],
                  r=[("XT16", tt * 4 + j) for j in range(4)], w=("lxt%d" % tb,))
            for c in range(8):
                b = it % 2
                it += 1
                kx, kg = "lps1%d" % b, "lps2%d" % b
                for kc in range(8):
                    mm(p, ps1[b][:, :], wx[:, kc, c * 128:(c + 1) * 128], xt[tb][:, kc, :], kc == 0, kc == 7,
                       r=("lwx", "lxt%d" % tb), w=(kx,))
                for kc in range(8):
                    mm(p, ps2[b][:, :], wx[:, kc, 1024 + c * 128:1024 + (c + 1) * 128], xt[tb][:, kc, :], kc == 0,
                       kc == 7, r=("lwx", "lxt%d" % tb), w=(kg,))
                p.op("dve", lambda e: e.tensor_copy(xl[b][:, 0:3], halo[:, c, :]), r=("lhalo",), w=("lxl%d" % b,))
                p.op("act", lambda e: e.copy(xl[b][:, 3:], ps1[b][:, :]), r=(kx,), w=("lxl%d" % b,))
                p.op("dve", lambda e: e.tensor_copy(halo[:, c, :], xl[b][:, TT:TT + 3]), r=("lxl%d" % b,),
                     w=("lhalo",))
                p.op("dve", lambda e: e.tensor_scalar(xc[b][:, :], xl[b][:, 0:TT], prm[:, c, 0:1], prm[:, c, 4:5],
                                                      ALU.mult, ALU.add), r=("lxl%d" % b, "lprm"), w=("lxc%d" % b,))
                for j in range(1, 4):
                    p.op("dve", lambda e, j=j: e.scalar_tensor_tensor(xc[b][:, :], xl[b][:, j:j + TT],
                                                                       prm[:, c, j:j + 1], xc[b][:, :], ALU.mult,
                                                                       ALU.add),
                         r=("lxl%d" % b, "lprm", "lxc%d" % b), w=("lxc%d" % b,))
                p.op("act", lambda e: e.copy(xcb[b][:, :], xc[b][:, :]), r=("lxc%d" % b,), w=("lxcb%d" % b,))
                mm(p, ps3[b][:, :], wa[:, c, :], xcb[b][:, :], True, True, r=("lwa", "lxcb%d" % b), w=("lps3%d" % b,))
                p.op("act", lambda e: e.activation(ra[b][:, :], ps3[b][:, :], AF.Sigmoid, bias=prm[:, c, 5:6]),
                     r=("lps3%d" % b, "lprm"), w=("lra%d" % b,))
                mm(p, ps3[b][:, :], wxx[:, c, :], xcb[b][:, :], True, True, r=("lwxx", "lxcb%d" % b),
                   w=("lps3%d" % b,))
                p.op("act", lambda e: e.activation(ri[b][:, :], ps3[b][:, :], AF.Sigmoid, bias=prm[:, c, 6:7]),
                     r=("lps3%d" % b, "lprm"), w=("lri%d" % b,))
                p.op("act", lambda e: e.activation(ra[b][:, :], ra[b][:, :], AF.Exp, scale=nsp[:, c:c + 1]),
                     r=("lra%d" % b, "nsp"), w=("lra%d" % b,))
                p.op("dve", lambda e: e.tensor_tensor(gu[b][:, :], ra[b][:, :], ra[b][:, :], ALU.mult),
                     r=("lra%d" % b,), w=("lgu%d" % b,))
                p.op("act", lambda e: e.activation(gu[b][:, :], gu[b][:, :], AF.Sqrt, scale=-1.0, bias=1.0),
                     r=("lgu%d" % b,), w=("lgu%d" % b,))
                p.op("dve", lambda e: e.tensor_tensor(ri[b][:, :], ri[b][:, :], xc[b][:, :], ALU.mult),
                     r=("lri%d" % b, "lxc%d" % b), w=("lri%d" % b,))
                p.op("dve", lambda e: e.tensor_tensor(gu[b][:, :], gu[b][:, :], ri[b][:, :], ALU.mult),
                     r=("lgu%d" % b, "lri%d" % b), w=("lgu%d" % b,))
                p.op("dve", lambda e: e.tensor_tensor_scan(hh[b][:, :], ra[b][:, :], gu[b][:, :], hst[:, c:c + 1],
                                                           ALU.mult, ALU.add),
                     r=("lra%d" % b, "lgu%d" % b, "lhst"), w=("lhh%d" % b,))
                p.op("dve", lambda e: e.tensor_copy(hst[:, c:c + 1], hh[b][:, TT - 1:TT]), r=("lhh%d" % b,),
                     w=("lhst",))
                p.op("act", lambda e: e.activation(ri[b][:, :], ps2[b][:, :], AF.Gelu), r=(kg,), w=("lri%d" % b,))
                p.op("dve", lambda e: e.tensor_tensor(ob[tb][:, c, :], hh[b][:, :], ri[b][:, :], ALU.mult),
                     r=("lhh%d" % b, "lri%d" % b), w=("lob%d" % tb,))
            p.dma("sp", g.OBT[:, :, tt * TT:(tt + 1) * TT], ob[tb][:, :, :], r=("lob%d" % tb,),
                  w=[("OBT", tt * 4 + j) for j in range(4)])


def stream_swa(g, li):
    p = g.p
    W = g.W
    TT = 512
    with ExitStack() as st:
        rb = p.sb(st, "srb", [33, 16], F32)
        p.op("dve", lambda e: e.memset(rb[:, :], 1.0), w=("srb",))
        p.dma("sp", rb[0:32, :], W["rel_bias"], w=("srb",))
        sps = [p.ps(st, "ssps", [128, 512], F32) for _ in range(2)]
        pext = sps[1][0:16, 0:383]
        pext2 = sps[0]
        mm(p, pext, rb[:, :], g.cst[0:33, C_OHX:C_OHX + 383], True, True, r=("srb", "cst"), w=("ssps1",))
        ext = p.sb(st, "sext", [16, 383], F32)
        p.op("dve", lambda e: e.tensor_copy(ext[:, :], pext), r=("ssps1",), w=("sext",))
        p.dma("sp", g.EXT_h.ap(), ext[:, :], r=("sext",), w=("EXT",))
        bias = p.sb(st, "sbias", [128, 4, 2, 512], F32)
        hank = p.sb(st, "shank", [128, 512], F32)
        for hk in range(4):
            for blk, off in ((0, 128), (1, 0)):
                for gq in range(4):
                    h = 4 * hk + gq
                    src = bass.AP(g.EXT_h, h * 383 + off, [[1, 128], [1, 128]])
                    p.dma("sp", hank[:, gq * 128:(gq + 1) * 128], src, r=("EXT",), w=("shank",))
                mm(p, pext2[:, :], g.cst[:, C_ANTI:C_ANTI + 128], hank[:, :], True, True, r=("cst", "shank"),
                   w=("ssps0",))
                p.op("dve", lambda e: e.tensor_copy(bias[:, hk, blk, :], pext2[:, :]), r=("ssps0",), w=("sbias",))
        esink = p.sb(st, "sesink", [128, 16], F32)
        p.dma("sp", esink[:, :], bass.AP(W["attn_sinks"].tensor, li * 16, [[0, 128], [1, 16]]), w=("sesink",))
        p.op("act", lambda e: e.activation(esink[:, :], esink[:, :], AF.Exp), r=("sesink",), w=("sesink",))
        wq = p.sb(st, "swq", [128, 8, 1024], BF16)
        wk = p.sb(st, "swk", [128, 8, 256], BF16)
        wv = p.sb(st, "swv", [128, 8, 256], BF16)
        for kc in range(8):
            rows = W["w_in"][li, kc * 128:(kc + 1) * 128, :]
            p.dma("pool", wq[:, kc, :], rows[:, O_SQ:O_SQ + 1024], w=("swq",))
            p.dma("pool", wk[:, kc, :], rows[:, O_SK:O_SK + 256], w=("swk",))
            p.dma("pool", wv[:, kc, :], rows[:, O_SV:O_SV + 256], w=("swv",))
        xt = [p.sb(st, "sxt", [128, 8, TT], BF16) for _ in range(2)]
        qT = p.sb(st, "sqT", [64, 16, TT], BF16)
        kT = p.sb(st, "skT", [64, 4, 128 + TT], BF16)
        va = p.sb(st, "sva", [128, 5, 4, 65], BF16)
        p.op("dve", lambda e: e.memset(va[:, :, :, :], 1.0), w=("sva",))
        p.op("dve", lambda e: e.memset(kT[:, :, :], 0.0), w=("skT",))
        tmp = [p.sb(st, "stmp", [128, 512], F32) for _ in range(2)]
        pT = [p.sb(st, "spT", [128, 512], BF16) for _ in range(4)]
        den = p.sb(st, "sden", [128, 4], F32)
        oc = [p.sb(st, "soc", [128, 1024], BF16) for _ in range(2)]
        qps = [p.ps(st, "sqps", [64, TT], F32) for _ in range(2)]
        pvb = p.ps(st, "spvb", [128, 512], F32)
        pv = pvb[:, 0:260].rearrange("p (g d) -> p g d", g=4)
        vps = pvb[:, 384:512]
        xb = [oc[0], oc[1]]
        pst = [p.ps(st, "spst", [128, D], BF16)]
        xtb = [p.sb(st, "sxtb", [128, D], BF16) for _ in range(2)]
        iq = 0
        isc = 0
        yield
        for tt in range(S // TT):
            tb = tt % 2
            p.dma("sp", xt[tb][:, :, :], g.XT16[:, :, tt * TT:(tt + 1) * TT],
                  r=[("XT16", tt * 4 + j) for j in range(4)], w=("sxt%d" % tb,))
            xk = "sxt%d" % tb
            for h in range(16):
                b = iq % 2
                iq += 1
                for kc in range(8):
                    mm(p, qps[b][:, :], wq[:, kc, h * 64:(h + 1) * 64], xt[tb][:, kc, :], kc == 0, kc == 7,
                       r=("swq", xk), w=("sqps%d" % b,))
                p.op("act", lambda e: e.copy(qT[:, h, :], qps[b][:, :]), r=("sqps%d" % b,), w=("sqT",))
            for hk in range(4):
                b = iq % 2
                iq += 1
                for kc in range(8):
                    mm(p, qps[b][:, :], wk[:, kc, hk * 64:(hk + 1) * 64], xt[tb][:, kc, :], kc == 0, kc == 7,
                       r=("swk", xk), w=("sqps%d" % b,))
                p.op("act", lambda e: e.copy(kT[:, hk, 128:], qps[b][:, :]), r=("sqps%d" % b,), w=("skT",))
            for nb in range(4):
                for hv in range(2):
                    for kc in range(8):
                        mm(p, vps, xt[tb][:, kc, nb * 128:(nb + 1) * 128], wv[:, kc, hv * 128:(hv + 1) * 128], kc == 0,
                           kc == 7, r=("swv", xk), w=("spv",))
                    p.op("act", lambda e: e.copy(va[:, nb + 1, 2 * hv:2 * hv + 2, 0:64],
                                                 vps.rearrange("p (h d) -> p h d", h=2)), r=("spv",), w=("sva",))
            for nb in range(4):
                n = tt * 4 + nb
                ob = n % 2
                for hk in range(4):
                    blks = [1] if n == 0 else [0, 1]
                    pts = []
                    for blk in blks:
                        b = isc % 2
                        pb = isc % 4
                        isc += 1
                        koff = nb * 128 + (0 if blk == 0 else 128)
                        mm(p, sps[b][:, :], kT[:, hk, koff:koff + 128],
                           qT[:, 4 * hk:4 * hk + 4, nb * 128:(nb + 1) * 128], True, True,
                           r=("skT", "sqT"), w=("ssps%d" % b,))
                        p.op("dve", lambda e: e.scalar_tensor_tensor(tmp[b][:, :], sps[b][:, :], 0.125,
                                                                      bias[:, hk, blk, :], ALU.mult, ALU.add),
                             r=("ssps%d" % b, "sbias"), w=("stmp%d" % b,))
                        p.op("act", lambda e: e.activation(pT[pb][:, :], tmp[b][:, :], AF.Exp),
                             r=("stmp%d" % b,), w=("spT%d" % pb,))
                        pts.append((pb, nb + blk))
                    for gq in range(4):
                        for i, (pb, vb) in enumerate(pts):
                            mm(p, pv[:, gq, :], pT[pb][:, gq * 128:(gq + 1) * 128], va[:, vb, hk, :], i == 0,
                               i == len(pts) - 1, r=("spT%d" % pb, "sva"), w=("spv",))
                    p.op("dve", lambda e: e.tensor_tensor(den[:, :], pv[:, :, 64], esink[:, 4 * hk:4 * hk + 4],
                                                          ALU.add), r=("spv", "sesink"), w=("sden",))
                    p.op("dve", lambda e: e.reciprocal(den[:, :], den[:, :]), r=("sden",), w=("sden",))
                    p.op("dve", lambda e: e.tensor_tensor(
                        oc[ob][:, hk * 256:(hk + 1) * 256].rearrange("p (g d) -> p g d", g=4), pv[:, :, 0:64],
                        den[:, :].unsqueeze(2).to_broadcast([128, 4, 64]), ALU.mult),
                         r=("spv", "sden"), w=("soc%d" % ob,))
                for c in range(8):
                    p.op("pe", lambda e: e.transpose(pst[0][:, c * 128:(c + 1) * 128],
                                                     oc[ob][:, c * 128:(c + 1) * 128], g.identb[:, :]),
                         r=("soc%d" % ob, "identb"), w=("spst",))
                p.op("act", lambda e: e.copy(xtb[ob][:, :], pst[0][:, :]), r=("spst",), w=("sxtb%d" % ob,))
                p.dma("sp", g.OCT[:, :, n * 128:(n + 1) * 128], xtb[ob][:, :].rearrange("p (c t) -> p c t", c=8),
                      r=("sxtb%d" % ob,), w=(("OCT", n),))
            p.op("dve", lambda e: e.tensor_copy(kT[:, :, 0:128], kT[:, :, TT:TT + 128]), r=("skT",), w=("skT",))
            p.op("dve", lambda e: e.tensor_copy(va[:, 0, :, :], va[:, 4, :, :]), r=("sva",), w=("sva",))


def stage_lru(g, li):
    run_streams(g.p, [stream_lru(g, li)])


def stage_swa(g, li):
    run_streams(g.p, [stream_swa(g, li)])


def stage_lru_swa(g, li):
    run_streams(g.p, [stream_swa(g, li), stream_lru(g, li)])


import os
GDN_CUT = float(os.environ.get('GDN_CUT', '99'))


def bcast_rows(handle_ap, offset, n, parts):
    return bass.AP(handle_ap.tensor, offset, [[0, parts], [1, n]])


def stage_gdn(g, li, nchunks=S // 64):
    p = g.p
    with ExitStack() as st:
        PSall = p.ps(st, "gps", [128, 8, 512], F32)
        xt = [p.sb(st, "gxt", [128, 8, 512], BF16) for _ in range(2)]
        th = [gdn_thread(g, li, st, PSall[:, 4 * i:4 * i + 4, :], 4 * i, 4, "ab"[i], nchunks, xt) for i in range(2)]
        live = list(th)
        while live:
            bufs = []
            for t_ in list(live):
                p.recbuf = []
                p.recording = True
                try:
                    next(t_)
                except StopIteration:
                    live.remove(t_)
                p.recording = False
                bufs.append(p.recbuf)
            n_ = max(len(b_) for b_ in bufs) if bufs else 0
            for i_ in range(n_):
                for b_ in bufs:
                    if i_ < len(b_):
                        p.emit(b_[i_])
        p.barrier()


def gdn_thread(g, li, st, PS, h0, H, TG, nchunks, xt):
    p = g.p
    W = g.W
    TT = 512
    C = 64
    cst = g.cst
    HC = H * 64
    NBK = PS.shape[1]
    bank_i = [0]

    def bank():
        b = bank_i[0] % NBK
        bank_i[0] += 1
        return b

    def bk(b):
        return ["gbank%s%d" % (TG, b)]

    K = lambda n: n + TG
    sbt = lambda n, shape, dt: p.sb(st, n + TG, shape, dt)
    cw = sbt("gcw", [128, 3 * H, 4], F32)
    for j in range(3):
        for c in range(H):
            col = j * 1024 + (h0 + c) * 128
            p.dma("sp", cw[:, j * H + c, :], W["conv_qkv_w"][li, :, col:col + 128].rearrange("k p -> p k"),
                  w=(K("gcw"),), allow_slow_non_contiguous=True)
    hp = sbt("ghp", [64, 2 * H], F32)
    p.dma("sp", hp[:, 0:H], bcast_rows(W["gdn_dt_bias"], li * 8 + h0, H, 64), w=(K("ghp"),))
    p.dma("sp", hp[:, H:2 * H], bcast_rows(W["gdn_a_log"], li * 8 + h0, H, 64), w=(K("ghp"),))
    p.op("act", lambda e: e.activation(hp[:, H:2 * H], hp[:, H:2 * H], AF.Exp), r=(K("ghp"),), w=(K("ghp"),))
    p.op("dve", lambda e: e.tensor_scalar(hp[:, H:2 * H], hp[:, H:2 * H], -1.0, None, ALU.mult), r=(K("ghp"),),
         w=(K("ghp"),))
    nw = sbt("gnw", [64, 128], F32)
    p.dma("sp", nw[:, :], bcast_rows(W["gdn_norm_w"], li * 128, 128, 64), w=(K("gnw"),))
    wqkv = sbt("gwqkv", [128, 8, 3 * H * 128], BF16)
    wz = sbt("gwz", [128, 8, H * 128], BF16)
    wab = sbt("gwab", [128, 8, 2 * H], BF16)
    for kc in range(8):
        rows = W["w_in"][li, kc * 128:(kc + 1) * 128, :]
        for j in range(3):
            c0 = j * 1024 + h0 * 128
            p.dma("pool", wqkv[:, kc, j * H * 128:(j + 1) * H * 128], rows[:, c0:c0 + H * 128], w=(K("gwqkv"),))
        p.dma("pool", wz[:, kc, :], rows[:, O_GZ + h0 * 128:O_GZ + (h0 + H) * 128], w=(K("gwz"),))
        p.dma("pool", wab[:, kc, 0:H], rows[:, O_GA + h0:O_GA + h0 + H], w=(K("gwab"),))
        p.dma("pool", wab[:, kc, H:2 * H], rows[:, O_GB + h0:O_GB + h0 + H], w=(K("gwab"),))
    pc = sbt("gpc", [128, 3 + TT], F32)
    halo = sbt("ghalo", [128, 3 * H, 3], F32)
    p.op("dve", lambda e: e.memset(halo[:, :, :], 0.0), w=(K("ghalo"),))
    acc = sbt("gacc", [128, TT], F32)
    cv = sbt("gcv", [128, 3 * H, TT], BF16)
    Sst = sbt("gS", [128, H, 128], F32)
    Sb = sbt("gSb", [128, H, 128], BF16)
    p.op("dve", lambda e: e.memset(Sst[:, :, :], 0.0), w=(K("gS"),))
    p.op("dve", lambda e: e.memset(Sb[:, :, :], 0.0), w=(K("gSb"),))
    sm = sbt("gsm", [64, 12, H], F32)
    bcr = sbt("gbcr", [64, 4, H, 64], F32)
    bcrb = sbt("gbcrb", [64, 4, H, 64], BF16)
    ones64b = g.onesb[0:64, 0:128]
    sq = sbt("gsq", [128, 2 * H, 64], BF16)
    qkn = sbt("gqkn", [128, 2 * H, 64], BF16)
    qd = sbt("gqd", [128, H, 64], BF16)
    eg128 = sbt("geg128", [128, H, 64], F32)
    eg128b = sbt("geg128b", [128, H, 64], BF16)
    Dm = sbt("gDm", [64, H, 64], F32)
    Gsb = sbt("gGsb", [64, H, 64], F32)
    bqs = sbt("gbqs", [128, 2 * H, 64], F32)
    BBs = sbt("gBBs", [64, H, 64], F32)
    E = sbt("gE", [64, H, 64], F32)
    dls = sbt("gdls", [64, H, 64], F32)
    dus = sbt("gdus", [64, H, 64], F32)
    dui = sbt("gdui", [64, H, 64], F32)
    Am = [sbt("gA", [64, H, 64], BF16) for _ in range(2)]
    IA = sbt("gIA", [64, H, 64], BF16)
    Bm = [sbt("gB", [64, H, 64], BF16) for _ in range(2)]
    Pt = [sbt("gPt", [64, H, 64], BF16) for _ in range(2)]
    intraT = sbt("gintraT", [64, H, 64], BF16)
    ktok = sbt("gktok", [64, H, 128], F32)
    vtok = sbt("gvtok", [64, H, 128], F32)
    vb = sbt("gvb", [64, H, 128], BF16)
    kbg = sbt("gkbg", [64, H, 128], BF16)
    kdec = sbt("gkdec", [64, H, 128], BF16)
    usb = sbt("gusb", [64, H, 128], F32)
    vnew = sbt("gvnew", [64, H, 128], BF16)
    wT = sbt("gwT", [128, H, 64], BF16)
    osb = usb
    osq = sbt("gosq", [64, H, 128], F32)
    zs = sbt("gzs", [64, H, 128], F32)
    oa = sbt("goa", [64, H, 128], BF16)
    oaT = [sbt("goaT", [128, H, 64], BF16) for _ in range(2)]
    ident64 = cst[0:64, C_IDENT:C_IDENT + 64]
    ones64 = cst[0:64, C_ONES:C_ONES + 128]
    bc3 = lambda ap2, n: ap2.unsqueeze(2).to_broadcast([ap2.shape[0], ap2.shape[1], n])
    mk3 = lambda col: cst[0:64, col:col + 64].unsqueeze(1).to_broadcast([64, H, 64])
    hj = lambda ap2: ap2.rearrange("p (h j) -> p h j", h=H)
    hd = lambda ap2: ap2.rearrange("p (h d) -> p h d", h=H)
    yield

    for n in range(nchunks):
        tt, cc = divmod(n, TT // C)
        tb = tt % 2
        xk = "gxt%d" % tb
        if cc == 0:
            if TG == "a":
                p.dma("sp", xt[tb][:, :, :], g.XT16[:, :, tt * TT:(tt + 1) * TT],
                      r=[("XT16", tt * 4 + j) for j in range(4)], w=(xk,))
            for c in range(3 * H):
                b = bank()
                for kc in range(8):
                    mm(p, PS[:, b, :], wqkv[:, kc, c * 128:(c + 1) * 128], xt[tb][:, kc, :], kc == 0, kc == 7,
                       r=(K("gwqkv"), xk), w=bk(b))
                p.op("dve", lambda e: e.tensor_copy(pc[:, 0:3], halo[:, c, :]), r=(K("ghalo"),), w=(K("gpc"),))
                p.op("act", lambda e: e.copy(pc[:, 3:], PS[:, b, :]), r=bk(b), w=(K("gpc"),))
                p.op("dve", lambda e: e.tensor_copy(halo[:, c, :], pc[:, TT:TT + 3]), r=(K("gpc"),), w=(K("ghalo"),))
                p.op("dve", lambda e: e.tensor_scalar(acc[:, :], pc[:, 0:TT], cw[:, c, 0:1], None, ALU.mult),
                     r=(K("gpc"), K("gcw")), w=(K("gacc"),))
                for j in range(1, 4):
                    p.op("dve", lambda e, j=j: e.scalar_tensor_tensor(acc[:, :], pc[:, j:j + TT], cw[:, c, j:j + 1],
                                                                       acc[:, :], ALU.mult, ALU.add),
                         r=(K("gpc"), K("gcw"), K("gacc")), w=(K("gacc"),))
                p.op("act", lambda e: e.activation(cv[:, c, :], acc[:, :], AF.Silu), r=(K("gacc"),), w=(K("gcv"),))
                yield
        cs = slice(cc * C, (cc + 1) * C)
        b0 = bank()
        for kc in range(8):
            mm(p, PS[0:64, b0, 0:2 * H], xt[tb][:, kc, cs], wab[:, kc, :], kc == 0, kc == 7, r=(K("gwab"), xk),
               w=bk(b0))
        p.op("dve", lambda e: e.tensor_tensor(sm[:, 0, :], PS[0:64, b0, 0:H], hp[:, 0:H], ALU.add),
             r=bk(b0) + [K("ghp")], w=(K("gsm0"),))
        p.op("act", lambda e: e.activation(sm[:, 0, :], sm[:, 0, :], AF.Exp), r=(K("gsm0"),), w=(K("gsm0"),))
        p.op("act", lambda e: e.activation(sm[:, 0, :], sm[:, 0, :], AF.Ln, bias=1.0), r=(K("gsm0"),),
             w=(K("gsm0"),))
        p.op("dve", lambda e: e.tensor_tensor(sm[:, 0, :], sm[:, 0, :], hp[:, H:2 * H], ALU.mult),
             r=(K("gsm0"), K("ghp")), w=(K("gsm0"),))
        p.op("act", lambda e: e.activation(sm[:, 1, :], PS[0:64, b0, H:2 * H], AF.Sigmoid), r=bk(b0),
             w=(K("gsm1"),))
        p.op("dve", lambda e: e.tensor_scalar(sm[:, 7, :], sm[:, 1, :], -1.0, None, ALU.mult), r=(K("gsm1"),),
             w=(K("gsm7"),))
        yield
        bG = bank()
        p.op("pe", lambda e: e.matmul(PS[0:64, bG, 0:H], cst[0:64, C_TRIU:C_TRIU + 64], sm[:, 0, :], start=True,
                                      stop=True), r=("cst", K("gsm0")), w=bk(bG))
        p.op("dve", lambda e: e.tensor_copy(sm[:, 2, :], PS[0:64, bG, 0:H]), r=bk(bG), w=(K("gsm2"),))
        p.op("act", lambda e: e.activation(sm[:, 3, :], sm[:, 2, :], AF.Exp), r=(K("gsm2"),), w=(K("gsm3"),))
        p.op("dve", lambda e: e.tensor_tensor(bcr[:, 0, :, :], mk3(C_TRIU), bc3(sm[:, 0, :], 64), ALU.mult),
             r=("cst", K("gsm0")), w=(K("gbcr0"),))
        bGb = bank()
        p.op("pe", lambda e: e.matmul(PS[:, bGb, 0:HC], ones64, bcr[:, 0, :, :].rearrange("p h j -> p (h j)"),
                                      start=True, stop=True), r=("cst", K("gbcr0")), w=bk(bGb))
        Gbc = hj(PS[:, bGb, 0:HC])
        p.op("act", lambda e: e.activation(eg128[:, :, :], Gbc, AF.Exp), r=bk(bGb), w=(K("geg128"),))
        p.op("act", lambda e: e.activation(eg128b[:, :, :], Gbc, AF.Exp), r=bk(bGb), w=(K("geg128b"),))
        p.op("act", lambda e: e.copy(Gsb[:, :, :], Gbc[0:64]), r=bk(bGb), w=(K("gGsb"),))
        yield
        p.op("dve", lambda e: e.tensor_tensor(Dm[:, :, :], Gsb[:, :, :], bc3(sm[:, 2, :], 64), ALU.subtract),
             r=(K("gGsb"), K("gsm2")), w=(K("gDm"),))
        p.op("dve", lambda e: e.tensor_tensor(sm[:, 4, :], Gsb[:, :, 63], sm[:, 2, :], ALU.subtract),
             r=(K("gGsb"), K("gsm2")), w=(K("gsm4"),))
        p.op("act", lambda e: e.activation(sm[:, 4, :], sm[:, 4, :], AF.Exp), r=(K("gsm4"),), w=(K("gsm4"),))
        p.op("act", lambda e: e.activation(Dm[:, :, :], Dm[:, :, :], AF.Abs), r=(K("gDm"),), w=(K("gDm"),))
        p.op("act", lambda e: e.activation(E[:, :, :], Dm[:, :, :], AF.Exp, scale=-1.0), r=(K("gDm"),), w=(K("gE"),))
        p.op("dve", lambda e: e.tensor_tensor(dls[:, :, :], E[:, :, :], mk3(C_LOWS), ALU.mult), r=(K("gE"), "cst"),
             w=(K("gdls"),))
        p.op("dve", lambda e: e.tensor_tensor(dus[:, :, :], E[:, :, :], mk3(C_UPS), ALU.mult), r=(K("gE"), "cst"),
             w=(K("gdus"),))
        p.op("dve", lambda e: e.tensor_tensor(dui[:, :, :], E[:, :, :], mk3(C_UPI), ALU.mult), r=(K("gE"), "cst"),
             w=(K("gdui"),))
        yield
        qk_raw = cv[:, 0:2 * H, cs]
        p.op("dve", lambda e: e.tensor_tensor(sq[:, :, :], qk_raw, qk_raw, ALU.mult), r=(K("gcv"),), w=(K("gsq"),))
        bs = bank()
        for h in range(2 * H):
            p.op("pe", lambda e, h=h: e.matmul(PS[0:64, bs, h:h + 1], sq[:, h, :], g.onesb[:, 0:1], start=True,
                                               stop=True), r=(K("gsq"), "onesb"), w=bk(bs))
        p.op("act", lambda e: e.activation(sm[:, 5:7, :], PS[0:64, bs, 0:2 * H].rearrange("p (a h) -> p a h", a=2),
                                           AF.Sqrt, bias=1e-6), r=bk(bs), w=(K("gsm5"), K("gsm6")))
        p.op("dve", lambda e: e.reciprocal(sm[:, 5:7, :], sm[:, 5:7, :]), r=(K("gsm5"), K("gsm6")),
             w=(K("gsm5"), K("gsm6")))
        p.op("dve", lambda e: e.tensor_scalar(sm[:, 5, :], sm[:, 5, :], 128.0 ** -0.5, None, ALU.mult),
             r=(K("gsm5"),), w=(K("gsm5"),))
        for i, src in ((1, 5), (2, 6), (3, 1)):
            p.op("dve", lambda e, i=i, src=src: e.tensor_tensor(
                bcrb[:, i, :, :], ident64.unsqueeze(1).to_broadcast([64, H, 64]), bc3(sm[:, src, :], 64), ALU.mult),
                 r=("cst", K("gsm%d" % src)), w=(K("gbcr%d" % i),))
        yield
        bq = bank()
        for i in (1, 2):
            p.op("pe", lambda e, i=i: e.matmul(PS[:, bq, (i - 1) * HC:i * HC], ones64b,
                                               bcrb[:, i, :, :].rearrange("p h j -> p (h j)"), start=True, stop=True),
                 r=("onesb", K("gbcr%d" % i)), w=bk(bq))
        p.op("act", lambda e: e.copy(bqs[:, :, :], PS[:, bq, 0:2 * HC].rearrange("p (a j) -> p a j", j=64)),
             r=bk(bq), w=(K("gbqs"),))
        p.op("dve", lambda e: e.tensor_tensor(qkn[:, :, :], qk_raw, bqs[:, :, :], ALU.mult),
             r=(K("gbqs"), K("gcv")), w=(K("gqkn"),))
        bB = bank()
        p.op("pe", lambda e: e.matmul(PS[0:64, bB, 0:HC], ones64b[:, 0:64],
                                      bcrb[:, 3, :, :].rearrange("p h j -> p (h j)"), start=True, stop=True),
             r=("onesb", K("gbcr3")), w=bk(bB))
        p.op("act", lambda e: e.copy(BBs[:, :, :], hj(PS[0:64, bB, 0:HC])), r=bk(bB), w=(K("gBBs"),))
        p.op("dve", lambda e: e.tensor_tensor(qd[:, :, :], qkn[:, 0:H, :], eg128b[:, :, :], ALU.mult),
             r=(K("gqkn"), K("geg128b")), w=(K("gqd"),))
        yield
        bt = bank()
        PSb = PS[0:64, bt, :].bitcast(BF16)
        for h in range(H):
            p.op("pe", lambda e, h=h: e.transpose(PSb[:, h * 128:(h + 1) * 128], qkn[:, H + h, :], g.identb[:, :]),
                 r=(K("gqkn"), "identb"), w=bk(bt))
        for h in range(H):
            p.op("pe", lambda e, h=h: e.transpose(PSb[:, (H + h) * 128:(H + h + 1) * 128], cv[:, 2 * H + h, cs],
                                                  g.identb[:, :]), r=(K("gcv"), "identb"), w=bk(bt))
        p.op("act", lambda e: e.copy(ktok[:, :, :], hd(PSb[:, 0:H * 128])), r=bk(bt), w=(K("gktok"),))
        p.op("act", lambda e: e.copy(vtok[:, :, :], hd(PSb[:, H * 128:2 * H * 128])), r=bk(bt), w=(K("gvtok"),))
        p.op("dve", lambda e: e.tensor_tensor(vb[:, :, :], vtok[:, :, :], bc3(sm[:, 1, :], 128), ALU.mult),
             r=(K("gvtok"), K("gsm1")), w=(K("gvb"),))
        p.op("dve", lambda e: e.tensor_tensor(sm[:, 8, :], sm[:, 1, :], sm[:, 3, :], ALU.mult),
             r=(K("gsm1"), K("gsm3")), w=(K("gsm8"),))
        p.op("dve", lambda e: e.tensor_tensor(kbg[:, :, :], ktok[:, :, :], bc3(sm[:, 8, :], 128), ALU.mult),
             r=(K("gktok"), K("gsm8")), w=(K("gkbg"),))
        p.op("dve", lambda e: e.tensor_tensor(kdec[:, :, :], ktok[:, :, :], bc3(sm[:, 4, :], 128), ALU.mult),
             r=(K("gktok"), K("gsm4")), w=(K("gkdec"),))
        yield
        bKK = bank()
        bQK = bank()
        for h in range(H):
            mm(p, PS[0:64, bKK, h * 64:(h + 1) * 64], qkn[:, H + h, :], qkn[:, H + h, :], True, True,
               r=(K("gqkn"),), w=bk(bKK))
        for h in range(H):
            mm(p, PS[0:64, bQK, h * 64:(h + 1) * 64], qkn[:, H + h, :], qkn[:, h, :], True, True,
               r=(K("gqkn"),), w=bk(bQK))
        KKp = hj(PS[0:64, bKK, 0:HC])
        QKp = hj(PS[0:64, bQK, 0:HC])
        p.op("dve", lambda e: e.tensor_tensor(Dm[:, :, :], KKp, dls[:, :, :], ALU.mult), r=bk(bKK) + [K("gdls")],
             w=(K("gDm"),))
        p.op("dve", lambda e: e.tensor_tensor(Am[0][:, :, :], Dm[:, :, :], bc3(sm[:, 7, :], 64), ALU.mult),
             r=(K("gDm"), K("gsm7")), w=(K("gA0"),))
        p.op("dve", lambda e: e.tensor_tensor(Gsb[:, :, :], KKp, dus[:, :, :], ALU.mult), r=bk(bKK) + [K("gdus")],
             w=(K("gGsb"),))
        p.op("dve", lambda e: e.scalar_tensor_tensor(Bm[0][:, :, :], Gsb[:, :, :], -1.0, BBs[:, :, :], ALU.mult,
                                                     ALU.mult), r=(K("gGsb"), K("gBBs")), w=(K("gB0"),))
        p.op("dve", lambda e: e.tensor_tensor(intraT[:, :, :], QKp, dui[:, :, :], ALU.mult), r=bk(bQK) + [K("gdui")],
             w=(K("gintraT"),))
        p.op("dve", lambda e: e.tensor_tensor(Pt[0][:, :, :], Bm[0][:, :, :],
                                              ident64.unsqueeze(1).to_broadcast([64, H, 64]), ALU.add),
             r=(K("gB0"), "cst"), w=(K("gPt0"),))
        yield
        for k in range(5):
            ka, kb = k % 2, (k + 1) % 2
            bA = bank()
            for h in range(H):
                mm(p, PS[0:64, bA, h * 64:(h + 1) * 64], Bm[ka][:, h, :], Am[ka][:, h, :], True, True,
                   r=(K("gB%d" % ka), K("gA%d" % ka)), w=bk(bA))
            if k < 4:
                bBk = bank()
                for h in range(H):
                    mm(p, PS[0:64, bBk, h * 64:(h + 1) * 64], Am[ka][:, h, :], Bm[ka][:, h, :], True, True,
                       r=(K("gB%d" % ka), K("gA%d" % ka)), w=bk(bBk))
            p.op("act", lambda e: e.copy(Am[kb][:, :, :], hj(PS[0:64, bA, 0:HC])), r=bk(bA), w=(K("gA%d" % kb),))
            p.op("dve", lambda e: e.tensor_tensor(IA[:, :, :], Am[kb][:, :, :],
                                                  ident64.unsqueeze(1).to_broadcast([64, H, 64]), ALU.add),
                 r=(K("gA%d" % kb), "cst"), w=(K("gIA"),))
            if k < 4:
                p.op("act", lambda e: e.copy(Bm[kb][:, :, :], hj(PS[0:64, bBk, 0:HC])), r=bk(bBk),
                     w=(K("gB%d" % kb),))
            bP = bank()
            for h in range(H):
                mm(p, PS[0:64, bP, h * 64:(h + 1) * 64], IA[:, h, :], Pt[ka][:, h, :], True, True,
                   r=(K("gIA"), K("gPt%d" % ka)), w=bk(bP))
            p.op("dve", lambda e: e.tensor_copy(Pt[kb][:, :, :], hj(PS[0:64, bP, 0:HC])), r=bk(bP),
                 w=(K("gPt%d" % kb),))
            yield
        TT_ = Pt[1]
        tk = K("gPt1")
        bu = bank()
        for h in range(H):
            mm(p, PS[0:64, bu, h * 128:(h + 1) * 128], TT_[:, h, :], vb[:, h, :], True, True, r=(tk, K("gvb")),
               w=bk(bu))
        bw = bank()
        for h in range(H):
            mm(p, PS[:, bw, h * 64:(h + 1) * 64], kbg[:, h, :], TT_[:, h, :], True, True, r=(tk, K("gkbg")), w=bk(bw))
        p.op("act", lambda e: e.copy(usb[:, :, :], hd(PS[0:64, bu, :])), r=bk(bu), w=(K("gusb"),))
        p.op("act", lambda e: e.copy(wT[:, :, :], hj(PS[:, bw, 0:HC])), r=bk(bw), w=(K("gwT"),))
        yield
        bws = bank()
        for h in range(H):
            mm(p, PS[0:64, bws, h * 128:(h + 1) * 128], wT[:, h, :], Sb[:, h, :], True, True,
               r=(K("gwT"), K("gSb")), w=bk(bws))
        p.op("dve", lambda e: e.tensor_tensor(vnew[:, :, :], usb[:, :, :], hd(PS[0:64, bws, :]), ALU.subtract),
             r=bk(bws) + [K("gusb")], w=(K("gvnew"),))
        bo = bank()
        for h in range(H):
            oo = PS[0:64, bo, h * 128:(h + 1) * 128]
            mm(p, oo, qd[:, h, :], Sb[:, h, :], True, False, r=(K("gqd"), K("gSb")), w=bk(bo))
            mm(p, oo, intraT[:, h, :], vnew[:, h, :], False, True, r=(K("gintraT"), K("gvnew")), w=bk(bo))
        bd = bank()
        for h in range(H):
            mm(p, PS[:, bd, h * 128:(h + 1) * 128], kdec[:, h, :], vnew[:, h, :], True, True,
               r=(K("gkdec"), K("gvnew")), w=bk(bd))
        p.op("dve", lambda e: e.tensor_tensor(Sst[:, :, :], Sst[:, :, :],
                                              eg128[:, :, 63:64].to_broadcast([128, H, 128]), ALU.mult),
             r=(K("gS"), K("geg128")), w=(K("gS"),))
        p.op("dve", lambda e: e.tensor_tensor(Sst[:, :, :], Sst[:, :, :], hd(PS[:, bd, :]), ALU.add),
             r=bk(bd) + [K("gS")], w=(K("gS"),))
        p.op("act", lambda e: e.copy(Sb[:, :, :], Sst[:, :, :]), r=(K("gS"),), w=(K("gSb"),))
        p.op("act", lambda e: e.copy(osb[:, :, :], hd(PS[0:64, bo, :])), r=bk(bo), w=(K("gusb"),))
        yield
        bz = bank()
        for kc in range(8):
            mm(p, PS[0:64, bz, :], xt[tb][:, kc, cs], wz[:, kc, :], kc == 0, kc == 7, r=(K("gwz"), xk), w=bk(bz))
        p.op("act", lambda e: e.activation(zs[:, :, :], hd(PS[0:64, bz, :]), AF.Silu), r=bk(bz), w=(K("gzs"),))
        p.op("dve", lambda e: e.tensor_tensor(osq[:, :, :], osb[:, :, :], osb[:, :, :], ALU.mult), r=(K("gusb"),),
             w=(K("gosq"),))
        p.op("dve", lambda e: e.tensor_reduce(sm[:, 9, :], osq[:, :, :], AX.X, ALU.add), r=(K("gosq"),),
             w=(K("gsm9"),))
        p.op("dve", lambda e: e.tensor_scalar(sm[:, 9, :], sm[:, 9, :], 1.0 / 128.0, 1e-6, ALU.mult, ALU.add),
             r=(K("gsm9"),), w=(K("gsm9"),))
        p.op("act", lambda e: e.activation(sm[:, 9, :], sm[:, 9, :], AF.Sqrt), r=(K("gsm9"),), w=(K("gsm9"),))
        p.op("dve", lambda e: e.reciprocal(sm[:, 9, :], sm[:, 9, :]), r=(K("gsm9"),), w=(K("gsm9"),))
        p.op("dve", lambda e: e.tensor_tensor(osb[:, :, :], osb[:, :, :], bc3(sm[:, 9, :], 128), ALU.mult),
             r=(K("gusb"), K("gsm9")), w=(K("gusb"),))
        p.op("dve", lambda e: e.tensor_tensor(zs[:, :, :], zs[:, :, :],
                                              nw[:, :].unsqueeze(1).to_broadcast([64, H, 128]), ALU.mult),
             r=(K("gzs"), K("gnw")), w=(K("gzs"),))
        p.op("dve", lambda e: e.tensor_tensor(oa[:, :, :], osb[:, :, :], zs[:, :, :], ALU.mult),
             r=(K("gusb"), K("gzs")), w=(K("goa"),))
        bT = bank()
        PT = PS[:, bT, :].bitcast(BF16)
        ob = n % 2
        for h in range(H):
            p.op("pe", lambda e, h=h: e.transpose(PT[:, h * 64:(h + 1) * 64], oa[:, h, :], g.identb[0:64, 0:64]),
                 r=(K("goa"), "identb"), w=bk(bT))
        p.op("act", lambda e: e.copy(oaT[ob][:, :, :], hj(PT[:, 0:HC])), r=bk(bT), w=(K("goaT%d" % ob),))
        p.dma("sp", g.OAT[:, h0:h0 + H, n * C:(n + 1) * C], oaT[ob][:, :, :], r=(K("goaT%d" % ob),),
              w=((K("OAT"), n // 2),))
        yield


STAGE_W = {
    "init": [],
    "lru": ["w_in", "rg_conv_w", "rg_conv_b", "rg_w_a", "rg_b_a", "rg_w_x", "rg_b_x", "rg_lambda"],
    "swa": ["w_in", "attn_sinks", "rel_bias"],
    "gdn": ["w_in", "conv_qkv_w", "gdn_a_log", "gdn_dt_bias", "gdn_norm_w"],
    "merge": ["w_in", "w_o_gdn", "w_o_lru", "w_o_swa", "w_out", "ln1_g", "ln1_b"],
    "moe": ["router_w", "router_b", "w_gu", "b_gu", "w_down", "b_down", "ln2_g", "ln2_b"],
    "ple": ["ple_w_gate", "ple_w_proj", "ln3_g", "ln3_b"],
}


def needed_weights(stages):
    out = []
    for n in WEIGHT_SHAPES:
        if any(n in STAGE_W[s_] for s_ in stages):
            out.append(n)
    return out


class LN:
    def __init__(self, g, st, gname, bname, li, npst=1, tag=""):
        p = g.p
        self.g = g
        self.T = tag
        self.lg = p.sb(st, "lng", [128, D], F32)
        self.lb = p.sb(st, "lnb", [128, D], F32)
        p.dma("sp", self.lg[:, :], bcast_rows(g.W[gname], li * D, D, 128), w=("lng" + tag,))
        p.dma("sp", self.lb[:, :], bcast_rows(g.W[bname], li * D, D, 128), w=("lnb" + tag,))
        self.stats = p.sb(st, "lnst", [128, 2, 6], F32)
        self.mv = p.sb(st, "lnmv", [128, 2], F32)
        self.xo = [p.sb(st, "lnxo", [128, D], F32) for _ in range(2)]
        self.bufs = xt16_bufs(g, st, npst)
        self.i = 0

    def apply(self, z, zkey, t):
        g, p, T = self.g, self.g.p, self.T
        b = self.i % 2
        self.i += 1
        kst, kmv, klg, klb = "lnst" + T, "lnmv" + T, "lng" + T, "lnb" + T
        for hf in range(2):
            p.op("dve", lambda e: e.bn_stats(self.stats[:, hf, :], z[:, hf * 512:(hf + 1) * 512]), r=(zkey,),
                 w=(kst,))
        p.op("dve", lambda e: e.bn_aggr(self.mv[:, :], self.stats[:, :, :].rearrange("p a s -> p (a s)")),
             r=(kst,), w=(kmv,))
        p.op("act", lambda e: e.activation(self.mv[:, 1:2], self.mv[:, 1:2], AF.Sqrt, bias=1e-5), r=(kmv,), w=(kmv,))
        p.op("dve", lambda e: e.reciprocal(self.mv[:, 1:2], self.mv[:, 1:2]), r=(kmv,), w=(kmv,))
        xo = self.xo[b]
        xk = "lnxo%s%d" % (T, b)
        p.op("dve", lambda e: e.tensor_scalar(xo[:, :], z, self.mv[:, 0:1], self.mv[:, 1:2], ALU.subtract,
                                              ALU.mult), r=(zkey, kmv), w=(xk,))
        p.op("dve", lambda e: e.tensor_tensor(xo[:, :], xo[:, :], self.lg[:, :], ALU.mult), r=(xk, klg), w=(xk,))
        p.op("dve", lambda e: e.tensor_tensor(xo[:, :], xo[:, :], self.lb[:, :], ALU.add), r=(xk, klb), w=(xk,))
        p.dma("sp", g.X32[t * 128:(t + 1) * 128, :], xo[:, :], r=(xk,), w=(("X32", t),))
        to_xt16(g, None, xo[:, :], t, (xk,), self.bufs, self.i, tag=T)


def stage_merge(g, li):
    p = g.p
    W = g.W
    TT = 512
    with ExitStack() as st:
        wo = []
        for bi, n in enumerate(("w_o_gdn", "w_o_lru", "w_o_swa")):
            w_ = p.sb(st, "mwo", [128, 8, D], BF16)
            for kc in range(8):
                p.dma("pool", w_[:, kc, :], W[n][li, kc * 128:(kc + 1) * 128, :], w=("mwo%d" % bi,))
            wo.append(w_)
        wout = p.sb(st, "mwout", [128, 8, D], BF16)
        for kc in range(8):
            p.dma("pool", wout[:, kc, :], W["w_out"][li, kc * 128:(kc + 1) * 128, :], w=("mwout",))
        wg = [p.sb(st, "mwg", [128, 8, 128], BF16) for _ in range(2)]
        ln = LN(g, st, "ln1_g", "ln1_b", li, npst=1)
        xt = p.sb(st, "mxt", [128, 8, TT], BF16)
        ot = [p.sb(st, "mot", [128, 8, TT], BF16) for _ in range(3)]
        gs = [p.sb(st, "mgs", [128, TT], F32) for _ in range(2)]
        tmp = p.sb(st, "mtmp", [128, TT], F32)
        macc = p.sb(st, "macc", [128, TT], F32)
        mT = p.sb(st, "mmT", [128, 8, TT], BF16)
        x32 = [p.sb(st, "mx32", [128, D], F32) for _ in range(2)]
        z = [p.sb(st, "mz", [128, D], F32) for _ in range(2)]
        psg = [p.ps(st, "mpsg", [128, TT], F32) for _ in range(2)]
        psy = [p.ps(st, "mpsy", [128, TT], F32) for _ in range(2)]
        psm = p.ps(st, "mpsm", [128, 2, 512], F32)
        srcs = (("OAT", g.OAT), ("OBT", g.OBT), ("OCT", g.OCT))
        ig = 0
        for tt in range(S // TT):
            tk = [("XT16", tt * 4 + j) for j in range(4)]
            p.dma("sp", xt[:, :, :], g.XT16[:, :, tt * TT:(tt + 1) * TT], r=tk, w=("mxt",))
            for bi, (nm, src) in enumerate(srcs):
                p.dma("sp", ot[bi][:, :, :], src[:, :, tt * TT:(tt + 1) * TT],
                      r=[(nm, tt * 4 + j) for j in range(4)], w=("mot%d" % bi,))
            for c in range(8):
                for bi in range(3):
                    b = ig % 2
                    ig += 1
                    col = O_MG + bi * 1024 + c * 128
                    p.dma("pool", wg[b][:, :, :],
                          W["w_in"][li, :, col:col + 128].rearrange("(kc p) m -> p kc m", p=128), w=("mwg%d" % b,))
                    for kc in range(8):
                        mm(p, psg[b][:, :], wg[b][:, kc, :], xt[:, kc, :], kc == 0, kc == 7,
                           r=("mwg%d" % b, "mxt"), w=("mpsg%d" % b,))
                    for kc in range(8):
                        mm(p, psy[b][:, :], wo[bi][:, kc, c * 128:(c + 1) * 128], ot[bi][:, kc, :], kc == 0, kc == 7,
                           r=("mwo%d" % bi, "mot%d" % bi), w=("mpsy%d" % b,))
                    p.op("act", lambda e: e.activation(gs[b][:, :], psg[b][:, :], AF.Sigmoid), r=("mpsg%d" % b,),
                         w=("mgs%d" % b,))
                    if bi == 0:
                        p.op("dve", lambda e: e.tensor_tensor(macc[:, :], gs[b][:, :], psy[b][:, :], ALU.mult),
                             r=("mgs%d" % b, "mpsy%d" % b), w=("macc",))
                    else:
                        p.op("dve", lambda e: e.tensor_tensor(tmp[:, :], gs[b][:, :], psy[b][:, :], ALU.mult),
                             r=("mgs%d" % b, "mpsy%d" % b), w=("mtmp",))
                        if bi == 1:
                            p.op("dve", lambda e: e.tensor_tensor(macc[:, :], macc[:, :], tmp[:, :], ALU.add),
                                 r=("macc", "mtmp"), w=("macc",))
                        else:
                            p.op("dve", lambda e: e.tensor_tensor(mT[:, c, :], macc[:, :], tmp[:, :], ALU.add),
                                 r=("macc", "mtmp"), w=("mmT",))
            for nb in range(4):
                t = tt * 4 + nb
                b = t % 2
                p.dma("sp", x32[b][:, :], g.X32[t * 128:(t + 1) * 128, :], r=(("X32", t),), w=("mx32%d" % b,))
                for hf in range(2):
                    for kc in range(8):
                        mm(p, psm[:, hf, :], mT[:, kc, nb * 128:(nb + 1) * 128], wout[:, kc, hf * 512:(hf + 1) * 512],
                           kc == 0, kc == 7, r=("mmT", "mwout"), w=("mpsm",))
                p.op("dve", lambda e: e.scalar_tensor_tensor(z[b][:, :], x32[b][:, :], ALPHA,
                                                             psm[:, :, :].rearrange("p a f -> p (a f)"), ALU.mult,
                                                             ALU.add), r=("mx32%d" % b, "mpsm"), w=("mz%d" % b,))
                ln.apply(z[b][:, :], "mz%d" % b, t)
        p.barrier()


def stage_moe(g, li, nexp=NE, fuse_ple=False):
    p = g.p
    W = g.W
    cst = g.cst
    NG = 2
    GS = CAP // NG
    NB = CAP // 128
    with ExitStack() as st0:
        dest = st0.enter_context(g.nc.sbuf_tensor(p.name("qdest"), [128, NT, 4], I32))
        gate4 = st0.enter_context(g.nc.sbuf_tensor(p.name("qgate"), [128, NT, 4], F32))
        with ExitStack() as st:
            zt = p.sb(st, "qz", [128, D], BF16)
            p.op("dve", lambda e: e.memset(zt[:, :], 0.0), w=("qz",))
            for r0 in range(0, NE * CAP, 1024):
                p.dma("sp", g.XBUF[r0:r0 + 1024, :].rearrange("(a p) d -> p a d", p=128),
                      zt[:, :].unsqueeze(1).to_broadcast([128, 8, D]), r=("qz",), w=("XBUF",))
            rw = p.sb(st, "qrw", [128, 8, NE], F32)
            p.dma("sp", rw[:, :, :], W["router_w"][li].rearrange("(kc p) e -> p kc e", p=128), w=("qrw",))
            rb = p.sb(st, "qrb", [1, NE], F32)
            p.dma("sp", rb[:, :], W["router_b"][li:li + 1, :], w=("qrb",))
            cnt = p.sb(st, "qcnt", [1, NE], F32)
            p.op("dve", lambda e: e.memset(cnt[:, :], 0.0), w=("qcnt",))
            x32 = [p.sb(st, "qx32", [128, D], F32) for _ in range(2)]
            xb = [p.sb(st, "qxb", [128, D], BF16) for _ in range(2)]
            xTf = p.sb(st, "qxTf", [128, 8, 128], F32)
            lg = p.sb(st, "qlg", [128, NE], F32)
            top8 = p.sb(st, "qtop8", [128, 8], F32)
            mask = p.sb(st, "qmask", [128, NE], F32)
            vv = p.sb(st, "qvv", [128, NE], F32)
            junk = p.sb(st, "qjunk", [128, NE], F32)
            d4f = p.sb(st, "qd4f", [128, 4], F32)
            sm = p.sb(st, "qsm", [128, 4], F32)
            pT = p.ps(st, "qpT", [128, 2, 512], F32)
            pl = p.ps(st, "qpl", [128, NE], F32)
            pp = p.ps(st, "qpp", [128, NE], F32)
            pc = p.ps(st, "qpc", [1, NE], F32)
            ones1 = cst[0:1, C_ONES:C_ONES + 128]
            for t in range(NT):
                b = t % 2
                xk = "qx32%d" % b
                p.dma("sp", x32[b][:, :], g.X32[t * 128:(t + 1) * 128, :], r=(("X32", t),), w=(xk,))
                p.op("act", lambda e: e.copy(xb[b][:, :], x32[b][:, :]), r=(xk,), w=("qxb%d" % b,))
                for c in range(8):
                    p.op("pe", lambda e, c=c: e.transpose(pT[:, c // 4, (c % 4) * 128:(c % 4 + 1) * 128],
                                                          x32[b][:, c * 128:(c + 1) * 128], g.ident),
                         r=(xk, "cst"), w=("qpT",))
                p.op("dve", lambda e: e.tensor_copy(xTf[:, :, :], pT[:, :, :].rearrange("p a (c t) -> p (a c) t", c=4)),
                     r=("qpT",), w=("qxTf",))
                for kc in range(8):
                    mm(p, pl[:, :], xTf[:, kc, :], rw[:, kc, :], kc == 0, False, r=("qxTf", "qrw"), w=("qpl",))
                mm(p, pl[:, :], ones1, rb[:, :], False, True, r=("cst", "qrb"), w=("qpl",))
                p.op("act", lambda e: e.copy(lg[:, :], pl[:, :]), r=("qpl",), w=("qlg",))
                p.op("dve", lambda e: e.max(top8[:, :], lg[:, :]), r=("qlg",), w=("qtop8",))
                p.op("dve", lambda e: e.tensor_scalar(mask[:, :], lg[:, :], top8[:, 3:4], None, ALU.is_ge),
                     r=("qlg", "qtop8"), w=("qmask",))
                p.op("dve", lambda e: e.tensor_scalar(sm[:, :], top8[:, 0:4], top8[:, 0:1], None, ALU.subtract),
                     r=("qtop8",), w=("qsm",))
                p.op("act", lambda e: e.activation(sm[:, :], sm[:, :], AF.Exp), r=("qsm",), w=("qsm",))
                p.op("dve", lambda e: e.tensor_reduce(d4f[:, 0:1], sm[:, :], AX.X, ALU.add), r=("qsm",), w=("qd4f",))
                p.op("dve", lambda e: e.reciprocal(d4f[:, 0:1], d4f[:, 0:1]), r=("qd4f",), w=("qd4f",))
                p.op("dve", lambda e: e.tensor_scalar(gate4[:, t, :], sm[:, :], d4f[:, 0:1], None, ALU.mult),
                     r=("qsm", "qd4f"), w=("qgate",))
                mm(p, pp[:, :], cst[:, C_TRI128:C_TRI128 + 128], mask[:, :], True, False, r=("cst", "qmask"), w=("qpp",))
                mm(p, pp[:, :], ones1, cnt[:, :], False, True, r=("cst", "qcnt"), w=("qpp",))
                mm(p, pc[:, :], cst[:, C_ONES:C_ONES + 1], mask[:, :], True, True, r=("cst", "qmask"), w=("qpc",))
                p.op("dve", lambda e: e.tensor_tensor(vv[:, :], pp[:, :], cst[:, C_EBASE:C_EBASE + NE], ALU.add),
                     r=("qpp", "cst"), w=("qvv",))
                p.op("dve", lambda e: e.tensor_tensor(cnt[:, :], cnt[:, :], pc[:, :], ALU.add), r=("qcnt", "qpc"),
                     w=("qcnt",))
                for k in range(4):
                    p.op("dve", lambda e, k=k: e.scalar_tensor_tensor(junk[:, :], lg[:, :], top8[:, k:k + 1], vv[:, :],
                                                                       ALU.is_equal, ALU.mult,
                                                                       accum_out=d4f[:, k:k + 1]),
                         r=("qlg", "qtop8", "qvv", "qd4f"), w=("qjunk", "qd4f"))
                p.op("dve", lambda e: e.tensor_copy(dest[:, t, :], d4f[:, :]), r=("qd4f",), w=("qdest",))
                for k in range(4):
                    p.dma_custom("pool", lambda e, k=k: e.indirect_dma_start(
                        out=g.XBUF[:, :], out_offset=bass.IndirectOffsetOnAxis(ap=dest[:, t, k:k + 1], axis=0),
                        in_=xb[b][:, :], in_offset=None),
                        r=("qxb%d" % b, "qdest"), w=("XBUF",))
            p.barrier()
        with ExitStack() as st:
            wgu = [p.sb(st, "qwgu", [128, 8, 2 * D], BF16) for _ in range(2)]
            wdn = [p.sb(st, "qwdn", [128, 8, D], BF16) for _ in range(2)]
            bgu = [p.sb(st, "qbgu", [1, 2 * D], BF16) for _ in range(2)]
            bdn = [p.sb(st, "qbdn", [1, D], BF16) for _ in range(2)]
            xin = [p.sb(st, "qxin", [128, D], BF16) for _ in range(2)]
            xbT = p.sb(st, "qxbT", [128, 8, CAP], BF16)
            aT = p.sb(st, "qaT", [128, 8, CAP], BF16)
            ta = [p.sb(st, "qta", [128, GS], F32) for _ in range(2)]
            tsg = [p.sb(st, "qtsg", [128, GS], F32) for _ in range(2)]
            tl = [p.sb(st, "qtl", [128, GS], F32) for _ in range(2)]
            osb = [p.sb(st, "qosb", [128, D], F32) for _ in range(2)]
            ptr = [p.ps(st, "qptr", [128, D], BF16) for _ in range(2)]
            pg = [p.ps(st, "qpg", [128, GS], F32) for _ in range(2)]
            pln = [p.ps(st, "qpln", [128, GS], F32) for _ in range(2)]
            pd = p.ps(st, "qpd", [128, 2, 512], F32)
            onesb1 = g.onesb[0:1, :]
            ix = 0
            ia = 0
            io = 0
            for ei in range(nexp):
                wb = ei % 2
                kw = ("qwgu%d" % wb, "qwdn%d" % wb, "qbgu%d" % wb, "qbdn%d" % wb)
                for kc in range(8):
                    p.dma("pool", wgu[wb][:, kc, :], W["w_gu"][li, ei, kc * 128:(kc + 1) * 128, :], w=(kw[0],))
                    p.dma("pool", wdn[wb][:, kc, :], W["w_down"][li, ei, kc * 128:(kc + 1) * 128, :], w=(kw[1],))
                p.dma("pool", bgu[wb][:, :], W["b_gu"][li, ei:ei + 1, :], w=(kw[2],))
                p.dma("pool", bdn[wb][:, :], W["b_down"][li, ei:ei + 1, :], w=(kw[3],))
                for blk in range(NB):
                    b = ix % 2
                    ix += 1
                    r0 = ei * CAP + blk * 128
                    p.dma("sp", xin[b][:, :], g.XBUF[r0:r0 + 128, :], r=("XBUF",), w=("qxin%d" % b,))
                    for c in range(8):
                        p.op("pe", lambda e, c=c: e.transpose(ptr[b][:, c * 128:(c + 1) * 128],
                                                              xin[b][:, c * 128:(c + 1) * 128], g.identb[:, :]),
                             r=("qxin%d" % b, "identb"), w=("qptr%d" % b,))
                    p.op("act", lambda e: e.copy(xbT[:, :, blk * 128:(blk + 1) * 128],
                                                 ptr[b][:, :].rearrange("p (c t) -> p c t", c=8)),
                         r=("qptr%d" % b,), w=("qxbT",))
                for j in range(8):
                    for gi in range(NG):
                        b = ia % 2
                        ia += 1
                        sl = slice(gi * GS, (gi + 1) * GS)
                        for two, pst_, key in ((0, pg[b], "qpg%d" % b), (1, pln[b], "qpln%d" % b)):
                            c0 = 256 * j + two
                            for kc in range(8):
                                mm(p, pst_[:, :], wgu[wb][:, kc, c0:c0 + 255:2], xbT[:, kc, sl], kc == 0, False,
                                   r=(kw[0], "qxbT"), w=(key,))
                            mm(p, pst_[:, :], bgu[wb][0:1, c0:c0 + 255:2], g.onesb[0:1, 0:GS], False, True,
                               r=(kw[2], "onesb"), w=(key,))
                        p.op("dve", lambda e: e.tensor_scalar(ta[b][:, :], pg[b][:, :], 7.0, None, ALU.min),
                             r=("qpg%d" % b,), w=("qta%d" % b,))
                        p.op("act", lambda e: e.activation(tsg[b][:, :], ta[b][:, :], AF.Sigmoid, scale=1.702),
                             r=("qta%d" % b,), w=("qtsg%d" % b,))
                        p.op("dve", lambda e: e.tensor_scalar(tl[b][:, :], pln[b][:, :], -7.0, 7.0, ALU.max, ALU.min),
                             r=("qpln%d" % b,), w=("qtl%d" % b,))
                        p.op("dve", lambda e: e.tensor_tensor(ta[b][:, :], ta[b][:, :], tsg[b][:, :], ALU.mult),
                             r=("qta%d" % b, "qtsg%d" % b), w=("qta%d" % b,))
                        p.op("dve", lambda e: e.scalar_tensor_tensor(aT[:, j, sl], tl[b][:, :], 1.0, ta[b][:, :],
                                                                      ALU.add, ALU.mult),
                             r=("qtl%d" % b, "qta%d" % b), w=("qaT",))
                for blk in range(NB):
                    b = io % 2
                    io += 1
                    for hf in range(2):
                        for kc in range(8):
                            mm(p, pd[:, hf, :], aT[:, kc, blk * 128:(blk + 1) * 128],
                               wdn[wb][:, kc, hf * 512:(hf + 1) * 512], kc == 0, False, r=("qaT", kw[1]), w=("qpd",))
                        mm(p, pd[:, hf, :], g.onesb[0:1, 0:128], bdn[wb][0:1, hf * 512:(hf + 1) * 512], False, True,
                           r=("onesb", kw[3]), w=("qpd",))
                    p.op("act", lambda e: e.copy(osb[b][:, :], pd[:, :, :].rearrange("p a f -> p (a f)")),
                         r=("qpd",), w=("qosb%d" % b,))
                    r0 = ei * CAP + blk * 128
                    p.dma("sp", g.OBUF[r0:r0 + 128, :], osb[b][:, :], r=("qosb%d" % b,), w=("OBUF",))
            p.barrier()
        if fuse_ple:
            run_streams(p, [stream_combine(g, li, dest, gate4), stream_ple(g, li)], lags=[0.0, 2.5 / NT])
        else:
            run_streams(p, [stream_combine(g, li, dest, gate4)])


def stream_combine(g, li, dest, gate4):
    p = g.p
    with ExitStack() as st:
        ln = LN(g, st, "ln2_g", "ln2_b", li, npst=1, tag="c")
        x32 = [p.sb(st, "qcx", [128, D], F32) for _ in range(2)]
        gt = [p.sb(st, "qgt", [128, D], F32) for _ in range(4)]
        z = [p.sb(st, "qcz", [128, D], F32) for _ in range(2)]
        ig = 0
        yield
        for t in range(NT):
            b = t % 2
            p.dma("sp", x32[b][:, :], g.X32[t * 128:(t + 1) * 128, :], r=(("X32", t),), w=("qcx%d" % b,))
            p.op("act", lambda e: e.activation(z[b][:, :], x32[b][:, :], AF.Copy, scale=ALPHA),
                 r=("qcx%d" % b,), w=("qcz%d" % b,))
            for k in range(4):
                gb = ig % 4
                ig += 1
                p.dma_custom("pool", lambda e, k=k: e.indirect_dma_start(
                    out=gt[gb][:, :], out_offset=None, in_=g.OBUF[:, :],
                    in_offset=bass.IndirectOffsetOnAxis(ap=dest[:, t, k:k + 1], axis=0)),
                    r=("OBUF", "qdest"), w=("qgt%d" % gb,))
                p.op("dve", lambda e, k=k: e.scalar_tensor_tensor(z[b][:, :], gt[gb][:, :], gate4[:, t, k:k + 1],
                                                                   z[b][:, :], ALU.mult, ALU.add),
                     r=("qgt%d" % gb, "qgate", "qcz%d" % b), w=("qcz%d" % b,))
            ln.apply(z[b][:, :], "qcz%d" % b, t)


def stream_ple(g, li):
    p = g.p
    W = g.W
    with ExitStack() as st:
        wg = p.sb(st, "pwg", [128, 8, D], BF16)
        for kc in range(8):
            p.dma("pool", wg[:, kc, :], W["ple_w_gate"][li, kc * 128:(kc + 1) * 128, :], w=("pwg",))
        wp = p.sb(st, "pwp", [128, 2, D], BF16)
        for kc in range(2):
            p.dma("pool", wp[:, kc, :], W["ple_w_proj"][li, kc * 128:(kc + 1) * 128, :], w=("pwp",))
        ln = LN(g, st, "ln3_g", "ln3_b", li, npst=1, tag="p")
        xt = [p.sb(st, "pxt", [128, 8, 128], BF16) for _ in range(2)]
        x32 = [p.sb(st, "px32", [128, D], F32) for _ in range(2)]
        pin = [p.sb(st, "ppin", [128, 256], F32) for _ in range(2)]
        pinb = [p.sb(st, "ppinb", [128, 256], BF16) for _ in range(2)]
        pT = [p.sb(st, "ppT", [128, 2, 128], BF16) for _ in range(2)]
        sg = [p.sb(st, "psg", [128, D], F32) for _ in range(2)]
        z = [p.sb(st, "pz", [128, D], F32) for _ in range(2)]
        psg = p.ps(st, "ppsg", [128, 2, 512], F32)
        psp = p.ps(st, "ppsp", [128, 2, 512], F32)
        pst = p.ps(st, "ppst", [128, 256], BF16)
        yield
        for t in range(NT):
            b = t % 2
            kb = str(b)
            p.dma("sp", xt[b][:, :, :], g.XT16[:, :, t * 128:(t + 1) * 128], r=(("XT16", t),), w=("pxt" + kb,))
            p.dma("sp", x32[b][:, :], g.X32[t * 128:(t + 1) * 128, :], r=(("X32", t),), w=("px32" + kb,))
            p.dma("sp", pin[b][:, :], g.pp[li, t * 128:(t + 1) * 128, :], w=("ppin" + kb,))
            p.op("act", lambda e: e.copy(pinb[b][:, :], pin[b][:, :]), r=("ppin" + kb,), w=("ppinb" + kb,))
            for c in range(2):
                p.op("pe", lambda e, c=c: e.transpose(pst[:, c * 128:(c + 1) * 128], pinb[b][:, c * 128:(c + 1) * 128],
                                                      g.identb[:, :]), r=("ppinb" + kb, "identb"), w=("ppst",))
            p.op("dve", lambda e: e.tensor_copy(pT[b][:, :, :], pst[:, :].rearrange("p (c t) -> p c t", c=2)),
                 r=("ppst",), w=("ppT" + kb,))
            for hf in range(2):
                for kc in range(8):
                    mm(p, psg[:, hf, :], xt[b][:, kc, :], wg[:, kc, hf * 512:(hf + 1) * 512], kc == 0, kc == 7,
                       r=("pxt" + kb, "pwg"), w=("ppsg",))
            for hf in range(2):
                for kc in range(2):
                    mm(p, psp[:, hf, :], pT[b][:, kc, :], wp[:, kc, hf * 512:(hf + 1) * 512], kc == 0, kc == 1,
                       r=("ppT" + kb, "pwp"), w=("ppsp",))
            p.op("act", lambda e: e.activation(sg[b][:, :], psg[:, :, :].rearrange("p a f -> p (a f)"), AF.Sigmoid),
                 r=("ppsg",), w=("psg" + kb,))
            p.op("dve", lambda e: e.tensor_tensor(sg[b][:, :], sg[b][:, :], psp[:, :, :].rearrange("p a f -> p (a f)"),
                                                  ALU.mult), r=("psg" + kb, "ppsp"), w=("psg" + kb,))
            p.op("dve", lambda e: e.scalar_tensor_tensor(z[b][:, :], x32[b][:, :], ALPHA, sg[b][:, :], ALU.mult,
                                                         ALU.add), r=("px32" + kb, "psg" + kb), w=("pz" + kb,))
            ln.apply(z[b][:, :], "pz" + kb, t)


def stage_ple(g, li):
    run_streams(g.p, [stream_ple(g, li)])


ENABLED_STAGES = ("init", "lru", "swa", "gdn", "merge", "moe", "ple")


def kernel(**inputs):
    n = 8
    nc, g = build_program(nlayers=DEPTH, stages=ENABLED_STAGES)
    consts = make_consts()
    in_maps = []
    for c in range(n):
        m = {"x": np.ascontiguousarray(inputs["x"][c], dtype=np.float32), "consts": consts}
        if "ple" in ENABLED_STAGES:
            m["p"] = np.ascontiguousarray(np.asarray(inputs["p"])[:, c], dtype=np.float32)
        for name in needed_weights(ENABLED_STAGES):
            m[name] = np.ascontiguousarray(inputs[name], dtype=np.float32)
        in_maps.append(m)
    res = run_bass_kernel_spmd(nc, in_maps, core_ids=list(range(n)))
    return np.stack([np.asarray(r["out"], dtype=np.float32) for r in res.results], axis=0)
```

```python
import math
from contextlib import ExitStack
import numpy as np
import concourse.bass as bass
import concourse.mybir as mybir
from concourse.bass_utils import run_bass_kernel_spmd

F32 = mybir.dt.float32
BF16 = mybir.dt.bfloat16
I32 = mybir.dt.int32
U32 = mybir.dt.uint32
AF = mybir.ActivationFunctionType
ALU = mybir.AluOpType
AX = mybir.AxisListType

S = 4096
D = 1024
NT = S // 128
DEPTH = 2
IN_COLS = 10768
ALPHA = (2.0 * DEPTH) ** 0.25
O_GQ, O_GK, O_GV, O_GZ, O_GA, O_GB = 0, 1024, 2048, 3072, 4096, 4104
O_LX, O_LG = 4112, 5136
O_SQ, O_SK, O_SV = 6160, 7184, 7440
O_MG = 7696


import os as _os
NOSELF = _os.environ.get("NOSELF", "0") == "1"


class _Rec:
    def __init__(self):
        self.call = None

    def __getattr__(self, name):
        def f(*a, **k):
            self.call = (name, a, k)
            return self
        return f


class Prog:
    def __init__(self, nc):
        self.nc = nc
        self.engs = {"pe": nc.tensor, "act": nc.scalar, "dve": nc.vector, "pool": nc.gpsimd, "sp": nc.sync}
        self.sem = {}
        self.cnt = {}
        self.es = ExitStack()
        for e in self.engs:
            self.sem[e] = self.es.enter_context(nc.semaphore("s_" + e))
            self.cnt[e] = 0
        self.ndsem = 8
        self.dsem = {}
        self.dcnt = {}
        self.dnext = {}
        for q in ("sp", "pool", "act"):
            self.dsem[q] = [self.es.enter_context(nc.semaphore("d_%s%d" % (q, i))) for i in range(self.ndsem)]
            self.dcnt[q] = [0] * self.ndsem
            self.dnext[q] = 0
        self.waited = {}
        self.last_w = {}
        self.readers = {}
        self.uid = 0
        self.all_tokens = {}
        self.recording = False
        self.recbuf = []

    def name(self, n):
        self.uid += 1
        return "%s_%d" % (n, self.uid)

    def sb(self, st, n, shape, dt):
        return st.enter_context(self.nc.sbuf_tensor(self.name(n), list(shape), dt))

    def ps(self, st, n, shape, dt):
        return st.enter_context(self.nc.psum_tensor(self.name(n), list(shape), dt))

    def dram(self, n, shape, dt):
        return self.nc.dram_tensor(n, list(shape), dt).ap()

    def _deps(self, r, w):
        deps = {}

        def add(tok):
            s, v = tok
            if deps.get(s, 0) < v:
                deps[s] = v

        for k in r:
            t = self.last_w.get(k)
            if t is not None:
                add(t)
        for k in w:
            t = self.last_w.get(k)
            if t is not None:
                add(t)
            for s, v in self.readers.get(k, {}).items():
                add((s, v))
        return deps

    def _wait(self, e, deps):
        eng = self.engs[e]
        for s, v in deps.items():
            if e == "pe" and s is self.sem["pe"]:
                continue
            if NOSELF and e in self.sem and s is self.sem[e]:
                continue
            key = (e, s.name)
            if self.waited.get(key, 0) < v:
                eng.wait_ge(s, v)
                self.waited[key] = v

    def _record(self, tok, r, w):
        s, v = tok
        self.all_tokens[s.name] = (s, v)
        for k in r:
            d = self.readers.setdefault(k, {})
            if d.get(s, 0) < v:
                d[s] = v
        for k in w:
            self.last_w[k] = tok
            self.readers[k] = {}

    def op(self, e, fn, r=(), w=()):
        if self.recording:
            rec = _Rec()
            fn(rec)
            self.recbuf.append(("op", e, rec.call, tuple(r), tuple(w)))
            return
        self._wait(e, self._deps(r, w))
        inst = fn(self.engs[e])
        self.cnt[e] += 1
        inst.then_inc(self.sem[e], 1)
        self._record((self.sem[e], self.cnt[e]), r, w)

    def emit(self, item):
        if item[0] == "op":
            _, e, (name, a, k), r, w = item
            self.op(e, lambda eng: getattr(eng, name)(*a, **k), r, w)
        elif item[0] == "dmac":
            _, q, (name, a, k), r, w = item
            self.dma_custom(q, lambda eng: getattr(eng, name)(*a, **k), r, w)
        else:
            _, q, out, in_, r, w, kw = item
            self.dma(q, out, in_, r, w, **kw)

    def dma(self, q, out, in_, r=(), w=(), **kw):
        if self.recording:
            self.recbuf.append(("dma", q, out, in_, tuple(r), tuple(w), kw))
            return
        deps = self._deps(r, w)
        j = self.dnext[q]
        self.dnext[q] = (j + 1) % self.ndsem
        s = self.dsem[q][j]
        if self.dcnt[q][j] > 0:
            deps[s] = max(deps.get(s, 0), 16 * self.dcnt[q][j])
        self._wait(q, deps)
        self.engs[q].dma_start(out=out, in_=in_, **kw).then_inc(s, 16)
        self.dcnt[q][j] += 1
        self._record((s, 16 * self.dcnt[q][j]), r, w)

    def dma_custom(self, q, fn, r=(), w=()):
        if self.recording:
            rec = _Rec()
            fn(rec)
            self.recbuf.append(("dmac", q, rec.call, tuple(r), tuple(w)))
            return
        deps = self._deps(r, w)
        j = self.dnext[q]
        self.dnext[q] = (j + 1) % self.ndsem
        s = self.dsem[q][j]
        if self.dcnt[q][j] > 0:
            deps[s] = max(deps.get(s, 0), 16 * self.dcnt[q][j])
        self._wait(q, deps)
        fn(self.engs[q]).then_inc(s, 16)
        self.dcnt[q][j] += 1
        self._record((s, 16 * self.dcnt[q][j]), r, w)

    def barrier(self):
        toks = {n: sv for n, sv in self.all_tokens.items()}
        for e in self.engs:
            d = {}
            for n, (s, v) in toks.items():
                d[s] = v
            self._wait(e, d)

    def finish(self):
        self.barrier()
        self.es.close()


def run_streams(p, gens, lags=None):
    lags = lags or [0.0] * len(gens)
    bufs = []
    for g_ in gens:
        p.recbuf = []
        p.recording = True
        next(g_)
        p.recording = False
        bufs.append(p.recbuf)
    for i_ in reversed(range(len(gens))):
        g_ = gens[i_]
        p.recbuf = bufs[i_]
        p.recording = True
        for _ in g_:
            pass
        p.recording = False
    items = []
    for si, b_ in enumerate(bufs):
        n_ = max(1, len(b_))
        for i_, it_ in enumerate(b_):
            items.append(((i_ + 0.5) / n_ + lags[si], si, i_, it_))
    items.sort(key=lambda t_: (t_[0], t_[1], t_[2]))
    for _, _, _, it_ in items:
        p.emit(it_)
    p.barrier()


def mm(p, out, lhsT, rhs, start, stop, r, w):
    p.op("pe", lambda e: e.matmul(out, lhsT, rhs, start=start, stop=stop), r=r, w=w)


class K:
    pass


def build_program(nlayers=DEPTH, stages=("init", "lru", "swa", "gdn", "merge", "moe", "ple"), dbg=None,
                  gather=False):
    nc = bass.Bass("TRN2", target_bir_lowering=False)
    p = Prog(nc)
    g = K()
    g.nc, g.p = nc, p
    ein = lambda n, shape, dt=F32: nc.dram_tensor(n, list(shape), dt, kind="ExternalInput").ap()
    g.x = ein("x", [S, D])
    if "ple" in stages:
        g.pp = ein("p", [DEPTH, S, 256])
    g.consts = ein("consts", [128, CONST_COLS])
    W = {}
    for n in needed_weights(stages):
        W[n] = ein(n, WEIGHT_SHAPES[n])
    g.W = W
    g.out = nc.dram_tensor("out", [S, D], F32, kind="ExternalOutput").ap()
    g.X32 = p.dram("X32", [S, D], F32)
    g.XT16 = p.dram("XT16", [128, 8, S], BF16)
    g.OAT = p.dram("OAT", [128, 8, S], BF16)
    g.OBT = p.dram("OBT", [128, 8, S], BF16)
    g.OCT = p.dram("OCT", [128, 8, S], BF16)
    g.EXT_h = nc.dram_tensor("EXT", [16, 383], F32)
    g.XBUF = p.dram("XBUF", [NE * CAP + S, D], BF16)
    g.OBUF = p.dram("OBUF", [NE * CAP + S, D], F32)
    g.dbg = {}
    if dbg:
        for n, (shape, dt) in dbg.items():
            g.dbg[n] = nc.dram_tensor("dbg_" + n, list(shape), dt, kind="ExternalOutput").ap()
    with nc.Block() as block, ExitStack() as gst:
        g.cst = p.sb(gst, "cst", [128, CONST_COLS], F32)
        p.dma("sp", g.cst[:, :], g.consts, r=(), w=("cst",))
        g.ident = g.cst[:, C_IDENT:C_IDENT + 128]
        g.identb = p.sb(gst, "identb", [128, 128], BF16)
        p.op("dve", lambda e: e.tensor_copy(g.identb[:, :], g.ident), r=("cst",), w=("identb",))
        g.onesb = p.sb(gst, "onesb", [128, 512], BF16)
        p.op("dve", lambda e: e.memset(g.onesb[:, :], 1.0), w=("onesb",))
        if "init" in stages:
            stage_init(g)
        for li in range(nlayers):
            if "lru" in stages:
                stage_lru(g, li)
            if "swa" in stages:
                stage_swa(g, li)
            if "gdn" in stages:
                stage_gdn(g, li)
            if "merge" in stages:
                stage_merge(g, li)
            if "moe" in stages:
                stage_moe(g, li, fuse_ple=("ple" in stages))
            elif "ple" in stages:
                stage_ple(g, li)
        for n in g.dbg:
            src = getattr(g, n)
            p.dma("sp", g.dbg[n], src, r=[(n, t) for t in range(NT)], w=("dbg_" + n,))
        for t in range(0, NT, 4):
            p.dma("sp", g.out[t * 128:(t + 4) * 128, :], g.X32[t * 128:(t + 4) * 128, :],
                  r=[("X32", t + j) for j in range(4)], w=(("out", t),))
        p.finish()
    return nc, g


C_IDENT = 0
C_OHX = 128
C_ANTI = 512
C_TRIU = 640
C_LOWS = 704
C_UPS = 768
C_UPI = 832
C_ONES = 896
C_TRI128 = 1024
C_EBASE = 1152
CONST_COLS = 1184
CAP = 768
NE = 32
BIGIDX = 1048576.0
NEG = -30000.0


def _t5_bucket(dist):
    max_exact = 16
    d = np.maximum(dist.astype(np.float32), np.float32(1.0))
    large = max_exact + (np.log(d / np.float32(max_exact)) / np.float32(math.log(128 / max_exact))
                         * np.float32(32 - max_exact)).astype(np.int32)
    large = np.minimum(large, 31)
    return np.where(dist < max_exact, dist, large)


def make_consts():
    c = np.zeros((128, CONST_COLS), np.float32)
    c[:, C_IDENT:C_IDENT + 128] = np.eye(128, dtype=np.float32)
    c[:, C_ANTI:C_ANTI + 128] = np.eye(128, dtype=np.float32)[::-1]
    r = np.arange(64)[:, None]
    cc = np.arange(64)[None, :]
    c[0:64, C_TRIU:C_TRIU + 64] = (r <= cc)
    c[0:64, C_LOWS:C_LOWS + 64] = (r > cc)
    c[0:64, C_UPS:C_UPS + 64] = (cc > r)
    c[0:64, C_UPI:C_UPI + 64] = (cc >= r)
    c[:, C_ONES:C_ONES + 128] = 1.0
    c[:, C_TRI128:C_TRI128 + 128] = (np.arange(128)[:, None] < np.arange(128)[None, :])
    c[:, C_EBASE:C_EBASE + 32] = (np.arange(32) * CAP)[None, :]
    for j in range(383):
        dist = j - 127
        if 0 <= dist < 128:
            c[int(_t5_bucket(np.array([dist]))[0]), C_OHX + j] = 1.0
        else:
            c[32, C_OHX + j] = NEG
    return c


WEIGHT_SHAPES = {
    "w_in": [DEPTH, D, IN_COLS],
    "rg_conv_w": [DEPTH, 4, D], "rg_conv_b": [DEPTH, D], "rg_w_a": [DEPTH, 8, 128, 128], "rg_b_a": [DEPTH, D],
    "rg_w_x": [DEPTH, 8, 128, 128], "rg_b_x": [DEPTH, D], "rg_lambda": [DEPTH, D],
    "attn_sinks": [DEPTH, 16], "rel_bias": [32, 16],
    "w_o_gdn": [DEPTH, D, D], "w_o_lru": [DEPTH, D, D], "w_o_swa": [DEPTH, D, D], "w_out": [DEPTH, D, D],
    "ln1_g": [DEPTH, D], "ln1_b": [DEPTH, D],
    "router_w": [DEPTH, D, NE], "router_b": [DEPTH, NE], "w_gu": [DEPTH, NE, D, 2 * D], "b_gu": [DEPTH, NE, 2 * D],
    "w_down": [DEPTH, NE, D, D], "b_down": [DEPTH, NE, D], "ln2_g": [DEPTH, D], "ln2_b": [DEPTH, D],
    "ple_w_gate": [DEPTH, D, D], "ple_w_proj": [DEPTH, 256, D], "ln3_g": [DEPTH, D], "ln3_b": [DEPTH, D],
    "conv_qkv_w": [DEPTH, 4, 3072], "gdn_a_log": [DEPTH, 8], "gdn_dt_bias": [DEPTH, 8], "gdn_norm_w": [DEPTH, 128],
}


def to_xt16(g, st, xt_sb_f32, tile_idx, keys_r, bufs, i, dst=None, dname="XT16", cast=True, tag=""):
    p = g.p
    xb, pst, xtb = bufs
    b = i % 2
    pb = b % len(pst)
    if dst is None:
        dst = g.XT16
    if cast:
        p.op("act", lambda e: e.copy(xb[b][:, :], xt_sb_f32), r=keys_r, w=("xb%s%d" % (tag, b),))
    else:
        p.op("pool", lambda e: e.tensor_copy(xb[b][:, :], xt_sb_f32), r=keys_r, w=("xb%s%d" % (tag, b),))
    for c in range(8):
        p.op("pe", lambda e: e.transpose(pst[pb][:, c * 128:(c + 1) * 128], xb[b][:, c * 128:(c + 1) * 128],
                                         g.identb[:, :]), r=("xb%s%d" % (tag, b), "identb"), w=("pst%s%d" % (tag, pb),))
    p.op("dve", lambda e: e.tensor_copy(xtb[b][:, :], pst[pb][:, :]), r=("pst%s%d" % (tag, pb),), w=("xtb%s%d" % (tag, b),))
    p.dma("sp", dst[:, :, tile_idx * 128:(tile_idx + 1) * 128],
          xtb[b][:, :].rearrange("p (c t) -> p c t", c=8), r=("xtb%s%d" % (tag, b),), w=((dname, tile_idx),))


def xt16_bufs(g, st, npst=2):
    p = g.p
    xb = [p.sb(st, "xb", [128, D], BF16) for _ in range(2)]
    pst = [p.ps(st, "pst", [128, D], BF16) for _ in range(npst)]
    xtb = [p.sb(st, "xtb", [128, D], BF16) for _ in range(2)]
    return xb, pst, xtb


def stage_init(g):
    p = g.p
    with ExitStack() as st:
        xin = [p.sb(st, "xin", [128, D], F32) for _ in range(2)]
        bufs = xt16_bufs(g, st)
        for t in range(NT):
            b = t % 2
            p.dma("sp", xin[b][:, :], g.x[t * 128:(t + 1) * 128, :], r=(), w=("xin%d" % b,))
            p.dma("sp", g.X32[t * 128:(t + 1) * 128, :], xin[b][:, :], r=("xin%d" % b,), w=(("X32", t),))
            to_xt16(g, st, xin[b][:, :], t, ("xin%d" % b,), bufs, t)
        p.barrier()


def stream_lru(g, li):
    p = g.p
    W = g.W
    TT = 512
    with ExitStack() as st:
        prm = p.sb(st, "lprm", [128, 8, 8], F32)
        for k in range(4):
            p.dma("sp", prm[:, :, k], W["rg_conv_w"][li, k, :].rearrange("(c p) -> p c", p=128),
                  w=("lprm",), allow_slow_non_contiguous=True)
        for k, n in ((4, "rg_conv_b"), (5, "rg_b_a"), (6, "rg_b_x"), (7, "rg_lambda")):
            p.dma("sp", prm[:, :, k], W[n][li, :].rearrange("(c p) -> p c", p=128), w=("lprm",),
                  allow_slow_non_contiguous=True)
        nsp = p.sb(st, "nsp", [128, 8], F32)
        p.op("act", lambda e: e.activation(nsp[:, :], prm[:, :, 7], AF.Exp, scale=-1.0), r=("lprm",), w=("nsp",))
        p.op("act", lambda e: e.activation(nsp[:, :], nsp[:, :], AF.Ln, bias=1.0), r=("nsp",), w=("nsp",))
        p.op("dve", lambda e: e.tensor_scalar(nsp[:, :], nsp[:, :], -8.0, None, ALU.mult), r=("nsp",), w=("nsp",))
        wx = p.sb(st, "lwx", [128, 8, 2048], BF16)
        for kc in range(8):
            p.dma("pool", wx[:, kc, :], W["w_in"][li, kc * 128:(kc + 1) * 128, O_LX:O_LX + 2048], w=("lwx",))
        wa = p.sb(st, "lwa", [128, 8, 128], BF16)
        wxx = p.sb(st, "lwxx", [128, 8, 128], BF16)
        p.dma("pool", wa[:, :, :], W["rg_w_a"][li].rearrange("h i j -> i h j"), w=("lwa",))
        p.dma("pool", wxx[:, :, :], W["rg_w_x"][li].rearrange("h i j -> i h j"), w=("lwxx",))
        xt = [p.sb(st, "lxt", [128, 8, TT], BF16) for _ in range(2)]
        xl = [p.sb(st, "lxl", [128, TT + 3], F32) for _ in range(2)]
        xc = [p.sb(st, "lxc", [128, TT], F32) for _ in range(2)]
        xcb = [p.sb(st, "lxcb", [128, TT], BF16) for _ in range(2)]
        ra = [p.sb(st, "lra", [128, TT], F32) for _ in range(2)]
        ri = [p.sb(st, "lri", [128, TT], F32) for _ in range(2)]
        gu = [p.sb(st, "lgu", [128, TT], F32) for _ in range(2)]
        hh = [p.sb(st, "lhh", [128, TT], F32) for _ in range(2)]
        ob = [p.sb(st, "lob", [128, 8, TT], BF16) for _ in range(2)]
        halo = p.sb(st, "lhalo", [128, 8, 3], F32)
        hst = p.sb(st, "lhst", [128, 8], F32)
        p.op("dve", lambda e: e.memset(halo[:, :, :], 0.0), w=("lhalo",))
        p.op("dve", lambda e: e.memset(hst[:, :], 0.0), w=("lhst",))
        ps1 = [p.ps(st, "lps1", [128, TT], F32) for _ in range(2)]
        ps2 = [p.ps(st, "lps2", [128, TT], F32) for _ in range(2)]
        ps3 = [p.ps(st, "lps3", [128, TT], F32) for _ in range(2)]
        it = 0
        yield
        def proj(tt, c, b):
            tb = tt % 2
            if c == 0:
                p.dma("sp", xt[tb][:, :, :], g.XT16[:, :, tt * TT:(tt + 1) * TT],
                      r=[("XT16", tt * 4 + j) for j in range(4)], w=("lxt%d" % tb,))
            for kc in range(8):
                mm(p, ps1[b][:, :], wx[:, kc, c * 128:(c + 1) * 128], xt[tb][:, kc, :], kc == 0, kc == 7,
                   r=("lwx", "lxt%d" % tb), w=("lps1%d" % b,))
            for kc in range(8):
                mm(p, ps2[b][:, :], wx[:, kc, 1024 + c * 128:1024 + (c + 1) * 128], xt[tb][:, kc, :], kc == 0,
                   kc == 7, r=("lwx", "lxt%d" % tb), w=("lps2%d" % b,))

        iters = [(tt, c) for tt in range(S // TT) for c in range(8)]
        proj(0, 0, 0)
        for tt in range(S // TT):
            tb = tt % 2
            for c in range(8):
                b = it % 2
                it += 1
                kx, kg = "lps1%d" % b, "lps2%d" % b
                if it < len(iters):
                    proj(iters[it][0], iters[it][1], it % 2)
                p.op("dve", lambda e: e.tensor_copy(xl[b][:, 0:3], halo[:, c, :]), r=("lhalo",), w=("lxl%d" % b,))
                p.op("act", lambda e: e.copy(xl[b][:, 3:], ps1[b][:, :]), r=(kx,), w=("lxl%d" % b,))
                p.op("dve", lambda e: e.tensor_copy(halo[:, c, :], xl[b][:, TT:TT + 3]), r=("lxl%d" % b,),
                     w=("lhalo",))
                p.op("dve", lambda e: e.tensor_scalar(xc[b][:, :], xl[b][:, 0:TT], prm[:, c, 0:1], prm[:, c, 4:5],
                                                      ALU.mult, ALU.add), r=("lxl%d" % b, "lprm"), w=("lxc%d" % b,))
                for j in range(1, 4):
                    p.op("dve", lambda e, j=j: e.scalar_tensor_tensor(xc[b][:, :], xl[b][:, j:j + TT],
                                                                       prm[:, c, j:j + 1], xc[b][:, :], ALU.mult,
                                                                       ALU.add),
                         r=("lxl%d" % b, "lprm", "lxc%d" % b), w=("lxc%d" % b,))
                p.op("act", lambda e: e.copy(xcb[b][:, :], xc[b][:, :]), r=("lxc%d" % b,), w=("lxcb%d" % b,))
                mm(p, ps3[b][:, :], wa[:, c, :], xcb[b][:, :], True, True, r=("lwa", "lxcb%d" % b), w=("lps3%d" % b,))
                p.op("act", lambda e: e.activation(ra[b][:, :], ps3[b][:, :], AF.Sigmoid, bias=prm[:, c, 5:6]),
                     r=("lps3%d" % b, "lprm"), w=("lra%d" % b,))
                mm(p, ps3[b][:, :], wxx[:, c, :], xcb[b][:, :], True, True, r=("lwxx", "lxcb%d" % b),
                   w=("lps3%d" % b,))
                p.op("act", lambda e: e.activation(ri[b][:, :], ps3[b][:, :], AF.Sigmoid, bias=prm[:, c, 6:7]),
                     r=("lps3%d" % b, "lprm"), w=("lri%d" % b,))
                p.op("act", lambda e: e.activation(ra[b][:, :], ra[b][:, :], AF.Exp, scale=nsp[:, c:c + 1]),
                     r=("lra%d" % b, "nsp"), w=("lra%d" % b,))
                p.op("dve", lambda e: e.tensor_tensor(gu[b][:, :], ra[b][:, :], ra[b][:, :], ALU.mult),
                     r=("lra%d" % b,), w=("lgu%d" % b,))
                p.op("act", lambda e: e.activation(gu[b][:, :], gu[b][:, :], AF.Sqrt, scale=-1.0, bias=1.0),
                     r=("lgu%d" % b,), w=("lgu%d" % b,))
                p.op("dve", lambda e: e.tensor_tensor(ri[b][:, :], ri[b][:, :], xc[b][:, :], ALU.mult),
                     r=("lri%d" % b, "lxc%d" % b), w=("lri%d" % b,))
                p.op("dve", lambda e: e.tensor_tensor(gu[b][:, :], gu[b][:, :], ri[b][:, :], ALU.mult),
                     r=("lgu%d" % b, "lri%d" % b), w=("lgu%d" % b,))
                p.op("dve", lambda e: e.tensor_tensor_scan(hh[b][:, :], ra[b][:, :], gu[b][:, :], hst[:, c:c + 1],
                                                           ALU.mult, ALU.add),
                     r=("lra%d" % b, "lgu%d" % b, "lhst"), w=("lhh%d" % b,))
                p.op("dve", lambda e: e.tensor_copy(hst[:, c:c + 1], hh[b][:, TT - 1:TT]), r=("lhh%d" % b,),
                     w=("lhst",))
                p.op("act", lambda e: e.activation(ri[b][:, :], ps2[b][:, :], AF.Gelu), r=(kg,), w=("lri%d" % b,))
                p.op("dve", lambda e: e.tensor_tensor(ob[tb][:, c, :], hh[b][:, :], ri[b][:, :], ALU.mult),
                     r=("lhh%d" % b, "lri%d" % b), w=("lob%d" % tb,))
            p.dma("sp", g.OBT[:, :, tt * TT:(tt + 1) * TT], ob[tb][:, :, :], r=("lob%d" % tb,),
                  w=[("OBT", tt * 4 + j) for j in range(4)])


def stream_swa(g, li):
    p = g.p
    W = g.W
    TT = 512
    with ExitStack() as st:
        rb = p.sb(st, "srb", [33, 16], F32)
        p.op("dve", lambda e: e.memset(rb[:, :], 1.0), w=("srb",))
        p.dma("sp", rb[0:32, :], W["rel_bias"], w=("srb",))
        sps = [p.ps(st, "ssps", [128, 512], F32) for _ in range(2)]
        pext = sps[1][0:16, 0:383]
        pext2 = sps[0]
        mm(p, pext, rb[:, :], g.cst[0:33, C_OHX:C_OHX + 383], True, True, r=("srb", "cst"), w=("ssps1",))
        ext = p.sb(st, "sext", [16, 383], F32)
        p.op("dve", lambda e: e.tensor_copy(ext[:, :], pext), r=("ssps1",), w=("sext",))
        p.dma("sp", g.EXT_h.ap(), ext[:, :], r=("sext",), w=("EXT",))
        bias = p.sb(st, "sbias", [128, 4, 2, 512], F32)
        hank = p.sb(st, "shank", [128, 512], F32)
        for hk in range(4):
            for blk, off in ((0, 128), (1, 0)):
                for gq in range(4):
                    h = 4 * hk + gq
                    src = bass.AP(g.EXT_h, h * 383 + off, [[1, 128], [1, 128]])
                    p.dma("sp", hank[:, gq * 128:(gq + 1) * 128], src, r=("EXT",), w=("shank",))
                mm(p, pext2[:, :], g.cst[:, C_ANTI:C_ANTI + 128], hank[:, :], True, True, r=("cst", "shank"),
                   w=("ssps0",))
                p.op("dve", lambda e: e.tensor_copy(bias[:, hk, blk, :], pext2[:, :]), r=("ssps0",), w=("sbias",))
        esink = p.sb(st, "sesink", [128, 16], F32)
        p.dma("sp", esink[:, :], bass.AP(W["attn_sinks"].tensor, li * 16, [[0, 128], [1, 16]]), w=("sesink",))
        p.op("act", lambda e: e.activation(esink[:, :], esink[:, :], AF.Exp), r=("sesink",), w=("sesink",))
        wq = p.sb(st, "swq", [128, 8, 1024], BF16)
        wk = p.sb(st, "swk", [128, 8, 256], BF16)
        wv = p.sb(st, "swv", [128, 8, 256], BF16)
        for kc in range(8):
            rows = W["w_in"][li, kc * 128:(kc + 1) * 128, :]
            p.dma("pool", wq[:, kc, :], rows[:, O_SQ:O_SQ + 1024], w=("swq",))
            p.dma("pool", wk[:, kc, :], rows[:, O_SK:O_SK + 256], w=("swk",))
            p.dma("pool", wv[:, kc, :], rows[:, O_SV:O_SV + 256], w=("swv",))
        xt = [p.sb(st, "sxt", [128, 8, TT], BF16) for _ in range(2)]
        qT = p.sb(st, "sqT", [64, 16, TT], BF16)
        kT = p.sb(st, "skT", [64, 4, 128 + TT], BF16)
        va = p.sb(st, "sva", [128, 5, 4, 65], BF16)
        p.op("dve", lambda e: e.memset(va[:, :, :, :], 1.0), w=("sva",))
        p.op("dve", lambda e: e.memset(kT[:, :, :], 0.0), w=("skT",))
        tmp = [p.sb(st, "stmp", [128, 512], F32) for _ in range(2)]
        pT = [p.sb(st, "spT", [128, 512], BF16) for _ in range(4)]
        den = p.sb(st, "sden", [128, 4], F32)
        oc = [p.sb(st, "soc", [128, 1024], BF16) for _ in range(2)]
        qps = [p.ps(st, "sqps", [64, TT], F32) for _ in range(2)]
        pvb = p.ps(st, "spvb", [128, 512], F32)
        pv = pvb[:, 0:260].rearrange("p (g d) -> p g d", g=4)
        vps = pvb[:, 384:512]
        xb = [oc[0], oc[1]]
        pst = [p.ps(st, "spst", [128, D], BF16)]
        xtb = [p.sb(st, "sxtb", [128, D], BF16) for _ in range(2)]
        iq = 0
        isc = 0
        yield
        for tt in range(S // TT):
            tb = tt % 2
            p.dma("sp", xt[tb][:, :, :], g.XT16[:, :, tt * TT:(tt + 1) * TT],
                  r=[("XT16", tt * 4 + j) for j in range(4)], w=("sxt%d" % tb,))
            xk = "sxt%d" % tb
            for h in range(16):
                b = iq % 2
                iq += 1
                for kc in range(8):
                    mm(p, qps[b][:, :], wq[:, kc, h * 64:(h + 1) * 64], xt[tb][:, kc, :], kc == 0, kc == 7,
                       r=("swq", xk), w=("sqps%d" % b,))
                p.op("act", lambda e: e.copy(qT[:, h, :], qps[b][:, :]), r=("sqps%d" % b,), w=("sqT",))
            for hk in range(4):
                b = iq % 2
                iq += 1
                for kc in range(8):
                    mm(p, qps[b][:, :], wk[:, kc, hk * 64:(hk + 1) * 64], xt[tb][:, kc, :], kc == 0, kc == 7,
                       r=("swk", xk), w=("sqps%d" % b,))
                p.op("act", lambda e: e.copy(kT[:, hk, 128:], qps[b][:, :]), r=("sqps%d" % b,), w=("skT",))
            for nb in range(4):
                for hv in range(2):
                    for kc in range(8):
                        mm(p, vps, xt[tb][:, kc, nb * 128:(nb + 1) * 128], wv[:, kc, hv * 128:(hv + 1) * 128], kc == 0,
                           kc == 7, r=("swv", xk), w=("spv",))
                    p.op("act", lambda e: e.copy(va[:, nb + 1, 2 * hv:2 * hv + 2, 0:64],
                                                 vps.rearrange("p (h d) -> p h d", h=2)), r=("spv",), w=("sva",))
            for nb in range(4):
                n = tt * 4 + nb
                ob = n % 2
                for hk in range(4):
                    blks = [1] if n == 0 else [0, 1]
                    pts = []
                    for blk in blks:
                        b = isc % 2
                        pb = isc % 4
                        isc += 1
                        koff = nb * 128 + (0 if blk == 0 else 128)
                        mm(p, sps[b][:, :], kT[:, hk, koff:koff + 128],
                           qT[:, 4 * hk:4 * hk + 4, nb * 128:(nb + 1) * 128], True, True,
                           r=("skT", "sqT"), w=("ssps%d" % b,))
                        p.op("dve", lambda e: e.scalar_tensor_tensor(tmp[b][:, :], sps[b][:, :], 0.125,
                                                                      bias[:, hk, blk, :], ALU.mult, ALU.add),
                             r=("ssps%d" % b, "sbias"), w=("stmp%d" % b,))
                        p.op("act", lambda e: e.activation(pT[pb][:, :], tmp[b][:, :], AF.Exp),
                             r=("stmp%d" % b,), w=("spT%d" % pb,))
                        pts.append((pb, nb + blk))
                    for gq in range(4):
                        for i, (pb, vb) in enumerate(pts):
                            mm(p, pv[:, gq, :], pT[pb][:, gq * 128:(gq + 1) * 128], va[:, vb, hk, :], i == 0,
                               i == len(pts) - 1, r=("spT%d" % pb, "sva"), w=("spv",))
                    p.op("dve", lambda e: e.tensor_tensor(den[:, :], pv[:, :, 64], esink[:, 4 * hk:4 * hk + 4],
                                                          ALU.add), r=("spv", "sesink"), w=("sden",))
                    p.op("dve", lambda e: e.reciprocal(den[:, :], den[:, :]), r=("sden",), w=("sden",))
                    p.op("dve", lambda e: e.tensor_tensor(
                        oc[ob][:, hk * 256:(hk + 1) * 256].rearrange("p (g d) -> p g d", g=4), pv[:, :, 0:64],
                        den[:, :].unsqueeze(2).to_broadcast([128, 4, 64]), ALU.mult),
                         r=("spv", "sden"), w=("soc%d" % ob,))
                for c in range(8):
                    p.op("pe", lambda e: e.transpose(pst[0][:, c * 128:(c + 1) * 128],
                                                     oc[ob][:, c * 128:(c + 1) * 128], g.identb[:, :]),
                         r=("soc%d" % ob, "identb"), w=("spst",))
                p.op("act", lambda e: e.copy(xtb[ob][:, :], pst[0][:, :]), r=("spst",), w=("sxtb%d" % ob,))
                p.dma("sp", g.OCT[:, :, n * 128:(n + 1) * 128], xtb[ob][:, :].rearrange("p (c t) -> p c t", c=8),
                      r=("sxtb%d" % ob,), w=(("OCT", n),))
            p.op("dve", lambda e: e.tensor_copy(kT[:, :, 0:128], kT[:, :, TT:TT + 128]), r=("skT",), w=("skT",))
            p.op("dve", lambda e: e.tensor_copy(va[:, 0, :, :], va[:, 4, :, :]), r=("sva",), w=("sva",))


def stage_lru(g, li):
    run_streams(g.p, [stream_lru(g, li)])


def stage_swa(g, li):
    run_streams(g.p, [stream_swa(g, li)])


def stage_lru_swa(g, li):
    run_streams(g.p, [stream_swa(g, li), stream_lru(g, li)])


import os
GDN_CUT = float(os.environ.get('GDN_CUT', '99'))


def bcast_rows(handle_ap, offset, n, parts):
    return bass.AP(handle_ap.tensor, offset, [[0, parts], [1, n]])


def stage_gdn(g, li, nchunks=S // 64):
    p = g.p
    with ExitStack() as st:
        PSall = p.ps(st, "gps", [128, 8, 512], F32)
        xt = [p.sb(st, "gxt", [128, 8, 512], BF16) for _ in range(2)]
        th = [gdn_thread(g, li, st, PSall[:, 4 * i:4 * i + 4, :], 4 * i, 4, "ab"[i], nchunks, xt) for i in range(2)]
        live = list(th)
        while live:
            bufs = []
            for t_ in list(live):
                p.recbuf = []
                p.recording = True
                try:
                    next(t_)
                except StopIteration:
                    live.remove(t_)
                p.recording = False
                bufs.append(p.recbuf)
            n_ = max(len(b_) for b_ in bufs) if bufs else 0
            for i_ in range(n_):
                for b_ in bufs:
                    if i_ < len(b_):
                        p.emit(b_[i_])
        p.barrier()


def gdn_thread(g, li, st, PS, h0, H, TG, nchunks, xt):
    p = g.p
    W = g.W
    TT = 512
    C = 64
    cst = g.cst
    HC = H * 64
    NBK = PS.shape[1]
    bank_i = [0]

    def bank():
        b = bank_i[0] % NBK
        bank_i[0] += 1
        return b

    def bk(b):
        return ["gbank%s%d" % (TG, b)]

    K = lambda n: n + TG
    sbt = lambda n, shape, dt: p.sb(st, n + TG, shape, dt)
    cw = sbt("gcw", [128, 3 * H, 4], F32)
    for j in range(3):
        for c in range(H):
            col = j * 1024 + (h0 + c) * 128
            p.dma("sp", cw[:, j * H + c, :], W["conv_qkv_w"][li, :, col:col + 128].rearrange("k p -> p k"),
                  w=(K("gcw"),), allow_slow_non_contiguous=True)
    hp = sbt("ghp", [64, 2 * H], F32)
    p.dma("sp", hp[:, 0:H], bcast_rows(W["gdn_dt_bias"], li * 8 + h0, H, 64), w=(K("ghp"),))
    p.dma("sp", hp[:, H:2 * H], bcast_rows(W["gdn_a_log"], li * 8 + h0, H, 64), w=(K("ghp"),))
    p.op("act", lambda e: e.activation(hp[:, H:2 * H], hp[:, H:2 * H], AF.Exp), r=(K("ghp"),), w=(K("ghp"),))
    p.op("dve", lambda e: e.tensor_scalar(hp[:, H:2 * H], hp[:, H:2 * H], -1.0, None, ALU.mult), r=(K("ghp"),),
         w=(K("ghp"),))
    nw = sbt("gnw", [64, 128], F32)
    p.dma("sp", nw[:, :], bcast_rows(W["gdn_norm_w"], li * 128, 128, 64), w=(K("gnw"),))
    wqkv = sbt("gwqkv", [128, 8, 3 * H * 128], BF16)
    wz = sbt("gwz", [128, 8, H * 128], BF16)
    wab = sbt("gwab", [128, 8, 2 * H], BF16)
    for kc in range(8):
        rows = W["w_in"][li, kc * 128:(kc + 1) * 128, :]
        for j in range(3):
            c0 = j * 1024 + h0 * 128
            p.dma("pool", wqkv[:, kc, j * H * 128:(j + 1) * H * 128], rows[:, c0:c0 + H * 128], w=(K("gwqkv"),))
        p.dma("pool", wz[:, kc, :], rows[:, O_GZ + h0 * 128:O_GZ + (h0 + H) * 128], w=(K("gwz"),))
        p.dma("pool", wab[:, kc, 0:H], rows[:, O_GA + h0:O_GA + h0 + H], w=(K("gwab"),))
        p.dma("pool", wab[:, kc, H:2 * H], rows[:, O_GB + h0:O_GB + h0 + H], w=(K("gwab"),))
    pc = sbt("gpc", [128, 3 + TT], F32)
    halo = sbt("ghalo", [128, 3 * H, 3], F32)
    p.op("dve", lambda e: e.memset(halo[:, :, :], 0.0), w=(K("ghalo"),))
    acc = sbt("gacc", [128, TT], F32)
    cv = sbt("gcv", [128, 3 * H, TT], BF16)
    Sst = sbt("gS", [128, H, 128], F32)
    Sb = sbt("gSb", [128, H, 128], BF16)
    p.op("dve", lambda e: e.memset(Sst[:, :, :], 0.0), w=(K("gS"),))
    p.op("dve", lambda e: e.memset(Sb[:, :, :], 0.0), w=(K("gSb"),))
    sm = sbt("gsm", [64, 12, H], F32)
    bcr = sbt("gbcr", [64, 4, H, 64], F32)
    bcrb = sbt("gbcrb", [64, 4, H, 64], BF16)
    ones64b = g.onesb[0:64, 0:128]
    sq = sbt("gsq", [128, 2 * H, 64], BF16)
    qkn = sbt("gqkn", [128, 2 * H, 64], BF16)
    qd = sbt("gqd", [128, H, 64], BF16)
    eg128 = sbt("geg128", [128, H, 64], F32)
    eg128b = sbt("geg128b", [128, H, 64], BF16)
    Dm = sbt("gDm", [64, H, 64], F32)
    Gsb = sbt("gGsb", [64, H, 64], F32)
    bqs = sbt("gbqs", [128, 2 * H, 64], F32)
    BBs = sbt("gBBs", [64, H, 64], F32)
    E = sbt("gE", [64, H, 64], F32)
    dls = sbt("gdls", [64, H, 64], F32)
    dus = sbt("gdus", [64, H, 64], F32)
    dui = sbt("gdui", [64, H, 64], F32)
    Am = [sbt("gA", [64, H, 64], BF16) for _ in range(2)]
    IA = sbt("gIA", [64, H, 64], BF16)
    Bm = [sbt("gB", [64, H, 64], BF16) for _ in range(2)]
    Pt = [sbt("gPt", [64, H, 64], BF16) for _ in range(2)]
    intraT = sbt("gintraT", [64, H, 64], BF16)
    ktok = sbt("gktok", [64, H, 128], F32)
    vtok = sbt("gvtok", [64, H, 128], F32)
    vb = sbt("gvb", [64, H, 128], BF16)
    kbg = sbt("gkbg", [64, H, 128], BF16)
    kdec = sbt("gkdec", [64, H, 128], BF16)
    usb = sbt("gusb", [64, H, 128], F32)
    vnew = sbt("gvnew", [64, H, 128], BF16)
    wT = sbt("gwT", [128, H, 64], BF16)
    osb = usb
    osq = sbt("gosq", [64, H, 128], F32)
    zs = sbt("gzs", [64, H, 128], F32)
    oa = sbt("goa", [64, H, 128], BF16)
    oaT = [sbt("goaT", [128, H, 64], BF16) for _ in range(2)]
    ident64 = cst[0:64, C_IDENT:C_IDENT + 64]
    ones64 = cst[0:64, C_ONES:C_ONES + 128]
    bc3 = lambda ap2, n: ap2.unsqueeze(2).to_broadcast([ap2.shape[0], ap2.shape[1], n])
    mk3 = lambda col: cst[0:64, col:col + 64].unsqueeze(1).to_broadcast([64, H, 64])
    hj = lambda ap2: ap2.rearrange("p (h j) -> p h j", h=H)
    hd = lambda ap2: ap2.rearrange("p (h d) -> p h d", h=H)
    yield

    for n in range(nchunks):
        tt, cc = divmod(n, TT // C)
        tb = tt % 2
        xk = "gxt%d" % tb
        if cc == 0:
            if TG == "a":
                p.dma("sp", xt[tb][:, :, :], g.XT16[:, :, tt * TT:(tt + 1) * TT],
                      r=[("XT16", tt * 4 + j) for j in range(4)], w=(xk,))
            for c in range(3 * H):
                b = bank()
                for kc in range(8):
                    mm(p, PS[:, b, :], wqkv[:, kc, c * 128:(c + 1) * 128], xt[tb][:, kc, :], kc == 0, kc == 7,
                       r=(K("gwqkv"), xk), w=bk(b))
                p.op("dve", lambda e: e.tensor_copy(pc[:, 0:3], halo[:, c, :]), r=(K("ghalo"),), w=(K("gpc"),))
                p.op("act", lambda e: e.copy(pc[:, 3:], PS[:, b, :]), r=bk(b), w=(K("gpc"),))
                p.op("dve", lambda e: e.tensor_copy(halo[:, c, :], pc[:, TT:TT + 3]), r=(K("gpc"),), w=(K("ghalo"),))
                p.op("dve", lambda e: e.tensor_scalar(acc[:, :], pc[:, 0:TT], cw[:, c, 0:1], None, ALU.mult),
                     r=(K("gpc"), K("gcw")), w=(K("gacc"),))
                for j in range(1, 4):
                    p.op("dve", lambda e, j=j: e.scalar_tensor_tensor(acc[:, :], pc[:, j:j + TT], cw[:, c, j:j + 1],
                                                                       acc[:, :], ALU.mult, ALU.add),
                         r=(K("gpc"), K("gcw"), K("gacc")), w=(K("gacc"),))
                p.op("act", lambda e: e.activation(cv[:, c, :], acc[:, :], AF.Silu), r=(K("gacc"),), w=(K("gcv"),))
                yield
        cs = slice(cc * C, (cc + 1) * C)
        b0 = bank()
        for kc in range(8):
            mm(p, PS[0:64, b0, 0:2 * H], xt[tb][:, kc, cs], wab[:, kc, :], kc == 0, kc == 7, r=(K("gwab"), xk),
               w=bk(b0))
        p.op("dve", lambda e: e.tensor_tensor(sm[:, 0, :], PS[0:64, b0, 0:H], hp[:, 0:H], ALU.add),
             r=bk(b0) + [K("ghp")], w=(K("gsm0"),))
        p.op("act", lambda e: e.activation(sm[:, 0, :], sm[:, 0, :], AF.Exp), r=(K("gsm0"),), w=(K("gsm0"),))
        p.op("act", lambda e: e.activation(sm[:, 0, :], sm[:, 0, :], AF.Ln, bias=1.0), r=(K("gsm0"),),
             w=(K("gsm0"),))
        p.op("dve", lambda e: e.tensor_tensor(sm[:, 0, :], sm[:, 0, :], hp[:, H:2 * H], ALU.mult),
             r=(K("gsm0"), K("ghp")), w=(K("gsm0"),))
        p.op("act", lambda e: e.activation(sm[:, 1, :], PS[0:64, b0, H:2 * H], AF.Sigmoid), r=bk(b0),
             w=(K("gsm1"),))
        p.op("dve", lambda e: e.tensor_scalar(sm[:, 7, :], sm[:, 1, :], -1.0, None, ALU.mult), r=(K("gsm1"),),
             w=(K("gsm7"),))
        yield
        bG = bank()
        p.op("pe", lambda e: e.matmul(PS[0:64, bG, 0:H], cst[0:64, C_TRIU:C_TRIU + 64], sm[:, 0, :], start=True,
                                      stop=True), r=("cst", K("gsm0")), w=bk(bG))
        p.op("dve", lambda e: e.tensor_copy(sm[:, 2, :], PS[0:64, bG, 0:H]), r=bk(bG), w=(K("gsm2"),))
        p.op("act", lambda e: e.activation(sm[:, 3, :], sm[:, 2, :], AF.Exp), r=(K("gsm2"),), w=(K("gsm3"),))
        p.op("dve", lambda e: e.tensor_tensor(bcr[:, 0, :, :], mk3(C_TRIU), bc3(sm[:, 0, :], 64), ALU.mult),
             r=("cst", K("gsm0")), w=(K("gbcr0"),))
        bGb = bank()
        p.op("pe", lambda e: e.matmul(PS[:, bGb, 0:HC], ones64, bcr[:, 0, :, :].rearrange("p h j -> p (h j)"),
                                      start=True, stop=True), r=("cst", K("gbcr0")), w=bk(bGb))
        Gbc = hj(PS[:, bGb, 0:HC])
        p.op("act", lambda e: e.activation(eg128[:, :, :], Gbc, AF.Exp), r=bk(bGb), w=(K("geg128"),))
        p.op("act", lambda e: e.activation(eg128b[:, :, :], Gbc, AF.Exp), r=bk(bGb), w=(K("geg128b"),))
        p.op("act", lambda e: e.copy(Gsb[:, :, :], Gbc[0:64]), r=bk(bGb), w=(K("gGsb"),))
        yield
        p.op("dve", lambda e: e.tensor_tensor(Dm[:, :, :], Gsb[:, :, :], bc3(sm[:, 2, :], 64), ALU.subtract),
             r=(K("gGsb"), K("gsm2")), w=(K("gDm"),))
        p.op("dve", lambda e: e.tensor_tensor(sm[:, 4, :], Gsb[:, :, 63], sm[:, 2, :], ALU.subtract),
             r=(K("gGsb"), K("gsm2")), w=(K("gsm4"),))
        p.op("act", lambda e: e.activation(sm[:, 4, :], sm[:, 4, :], AF.Exp), r=(K("gsm4"),), w=(K("gsm4"),))
        p.op("act", lambda e: e.activation(Dm[:, :, :], Dm[:, :, :], AF.Abs), r=(K("gDm"),), w=(K("gDm"),))
        p.op("act", lambda e: e.activation(E[:, :, :], Dm[:, :, :], AF.Exp, scale=-1.0), r=(K("gDm"),), w=(K("gE"),))
        p.op("dve", lambda e: e.tensor_tensor(dls[:, :, :], E[:, :, :], mk3(C_LOWS), ALU.mult), r=(K("gE"), "cst"),
             w=(K("gdls"),))
        p.op("dve", lambda e: e.tensor_tensor(dus[:, :, :], E[:, :, :], mk3(C_UPS), ALU.mult), r=(K("gE"), "cst"),
             w=(K("gdus"),))
        p.op("dve", lambda e: e.tensor_tensor(dui[:, :, :], E[:, :, :], mk3(C_UPI), ALU.mult), r=(K("gE"), "cst"),
             w=(K("gdui"),))
        yield
        qk_raw = cv[:, 0:2 * H, cs]
        p.op("dve", lambda e: e.tensor_tensor(sq[:, :, :], qk_raw, qk_raw, ALU.mult), r=(K("gcv"),), w=(K("gsq"),))
        bs = bank()
        for h in range(2 * H):
            p.op("pe", lambda e, h=h: e.matmul(PS[0:64, bs, h:h + 1], sq[:, h, :], g.onesb[:, 0:1], start=True,
                                               stop=True), r=(K("gsq"), "onesb"), w=bk(bs))
        p.op("act", lambda e: e.activation(sm[:, 5:7, :], PS[0:64, bs, 0:2 * H].rearrange("p (a h) -> p a h", a=2),
                                           AF.Sqrt, bias=1e-6), r=bk(bs), w=(K("gsm5"), K("gsm6")))
        p.op("dve", lambda e: e.reciprocal(sm[:, 5:7, :], sm[:, 5:7, :]), r=(K("gsm5"), K("gsm6")),
             w=(K("gsm5"), K("gsm6")))
        p.op("dve", lambda e: e.tensor_scalar(sm[:, 5, :], sm[:, 5, :], 128.0 ** -0.5, None, ALU.mult),
             r=(K("gsm5"),), w=(K("gsm5"),))
        for i, src in ((1, 5), (2, 6), (3, 1)):
            p.op("dve", lambda e, i=i, src=src: e.tensor_tensor(
                bcrb[:, i, :, :], ident64.unsqueeze(1).to_broadcast([64, H, 64]), bc3(sm[:, src, :], 64), ALU.mult),
                 r=("cst", K("gsm%d" % src)), w=(K("gbcr%d" % i),))
        yield
        bq = bank()
        for i in (1, 2):
            p.op("pe", lambda e, i=i: e.matmul(PS[:, bq, (i - 1) * HC:i * HC], ones64b,
                                               bcrb[:, i, :, :].rearrange("p h j -> p (h j)"), start=True, stop=True),
                 r=("onesb", K("gbcr%d" % i)), w=bk(bq))
        p.op("act", lambda e: e.copy(bqs[:, :, :], PS[:, bq, 0:2 * HC].rearrange("p (a j) -> p a j", j=64)),
             r=bk(bq), w=(K("gbqs"),))
        p.op("dve", lambda e: e.tensor_tensor(qkn[:, :, :], qk_raw, bqs[:, :, :], ALU.mult),
             r=(K("gbqs"), K("gcv")), w=(K("gqkn"),))
        bB = bank()
        p.op("pe", lambda e: e.matmul(PS[0:64, bB, 0:HC], ones64b[:, 0:64],
                                      bcrb[:, 3, :, :].rearrange("p h j -> p (h j)"), start=True, stop=True),
             r=("onesb", K("gbcr3")), w=bk(bB))
        p.op("act", lambda e: e.copy(BBs[:, :, :], hj(PS[0:64, bB, 0:HC])), r=bk(bB), w=(K("gBBs"),))
        p.op("dve", lambda e: e.tensor_tensor(qd[:, :, :], qkn[:, 0:H, :], eg128b[:, :, :], ALU.mult),
             r=(K("gqkn"), K("geg128b")), w=(K("gqd"),))
        yield
        bt = bank()
        PSb = PS[0:64, bt, :].bitcast(BF16)
        for h in range(H):
            p.op("pe", lambda e, h=h: e.transpose(PSb[:, h * 128:(h + 1) * 128], qkn[:, H + h, :], g.identb[:, :]),
                 r=(K("gqkn"), "identb"), w=bk(bt))
        for h in range(H):
            p.op("pe", lambda e, h=h: e.transpose(PSb[:, (H + h) * 128:(H + h + 1) * 128], cv[:, 2 * H + h, cs],
                                                  g.identb[:, :]), r=(K("gcv"), "identb"), w=bk(bt))
        p.op("act", lambda e: e.copy(ktok[:, :, :], hd(PSb[:, 0:H * 128])), r=bk(bt), w=(K("gktok"),))
        p.op("act", lambda e: e.copy(vtok[:, :, :], hd(PSb[:, H * 128:2 * H * 128])), r=bk(bt), w=(K("gvtok"),))
        p.op("dve", lambda e: e.tensor_tensor(vb[:, :, :], vtok[:, :, :], bc3(sm[:, 1, :], 128), ALU.mult),
             r=(K("gvtok"), K("gsm1")), w=(K("gvb"),))
        p.op("dve", lambda e: e.tensor_tensor(sm[:, 8, :], sm[:, 1, :], sm[:, 3, :], ALU.mult),
             r=(K("gsm1"), K("gsm3")), w=(K("gsm8"),))
        p.op("dve", lambda e: e.tensor_tensor(kbg[:, :, :], ktok[:, :, :], bc3(sm[:, 8, :], 128), ALU.mult),
             r=(K("gktok"), K("gsm8")), w=(K("gkbg"),))
        p.op("dve", lambda e: e.tensor_tensor(kdec[:, :, :], ktok[:, :, :], bc3(sm[:, 4, :], 128), ALU.mult),
             r=(K("gktok"), K("gsm4")), w=(K("gkdec"),))
        yield
        bKK = bank()
        bQK = bank()
        for h in range(H):
            mm(p, PS[0:64, bKK, h * 64:(h + 1) * 64], qkn[:, H + h, :], qkn[:, H + h, :], True, True,
               r=(K("gqkn"),), w=bk(bKK))
        for h in range(H):
            mm(p, PS[0:64, bQK, h * 64:(h + 1) * 64], qkn[:, H + h, :], qkn[:, h, :], True, True,
               r=(K("gqkn"),), w=bk(bQK))
        KKp = hj(PS[0:64, bKK, 0:HC])
        QKp = hj(PS[0:64, bQK, 0:HC])
        p.op("dve", lambda e: e.tensor_tensor(Dm[:, :, :], KKp, dls[:, :, :], ALU.mult), r=bk(bKK) + [K("gdls")],
             w=(K("gDm"),))
        p.op("dve", lambda e: e.tensor_tensor(Am[0][:, :, :], Dm[:, :, :], bc3(sm[:, 7, :], 64), ALU.mult),
             r=(K("gDm"), K("gsm7")), w=(K("gA0"),))
        p.op("dve", lambda e: e.tensor_tensor(Gsb[:, :, :], KKp, dus[:, :, :], ALU.mult), r=bk(bKK) + [K("gdus")],
             w=(K("gGsb"),))
        p.op("dve", lambda e: e.scalar_tensor_tensor(Bm[0][:, :, :], Gsb[:, :, :], -1.0, BBs[:, :, :], ALU.mult,
                                                     ALU.mult), r=(K("gGsb"), K("gBBs")), w=(K("gB0"),))
        p.op("dve", lambda e: e.tensor_tensor(intraT[:, :, :], QKp, dui[:, :, :], ALU.mult), r=bk(bQK) + [K("gdui")],
             w=(K("gintraT"),))
        p.op("dve", lambda e: e.tensor_tensor(Pt[0][:, :, :], Bm[0][:, :, :],
                                              ident64.unsqueeze(1).to_broadcast([64, H, 64]), ALU.add),
             r=(K("gB0"), "cst"), w=(K("gPt0"),))
        yield
        for k in range(5):
            ka, kb = k % 2, (k + 1) % 2
            bA = bank()
            for h in range(H):
                mm(p, PS[0:64, bA, h * 64:(h + 1) * 64], Bm[ka][:, h, :], Am[ka][:, h, :], True, True,
                   r=(K("gB%d" % ka), K("gA%d" % ka)), w=bk(bA))
            if k < 4:
                bBk = bank()
                for h in range(H):
                    mm(p, PS[0:64, bBk, h * 64:(h + 1) * 64], Am[ka][:, h, :], Bm[ka][:, h, :], True, True,
                       r=(K("gB%d" % ka), K("gA%d" % ka)), w=bk(bBk))
            p.op("act", lambda e: e.copy(Am[kb][:, :, :], hj(PS[0:64, bA, 0:HC])), r=bk(bA), w=(K("gA%d" % kb),))
            p.op("dve", lambda e: e.tensor_tensor(IA[:, :, :], Am[kb][:, :, :],
                                                  ident64.unsqueeze(1).to_broadcast([64, H, 64]), ALU.add),
                 r=(K("gA%d" % kb), "cst"), w=(K("gIA"),))
            if k < 4:
                p.op("act", lambda e: e.copy(Bm[kb][:, :, :], hj(PS[0:64, bBk, 0:HC])), r=bk(bBk),
                     w=(K("gB%d" % kb),))
            bP = bank()
            for h in range(H):
                mm(p, PS[0:64, bP, h * 64:(h + 1) * 64], IA[:, h, :], Pt[ka][:, h, :], True, True,
                   r=(K("gIA"), K("gPt%d" % ka)), w=bk(bP))
            p.op("dve", lambda e: e.tensor_copy(Pt[kb][:, :, :], hj(PS[0:64, bP, 0:HC])), r=bk(bP),
                 w=(K("gPt%d" % kb),))
            yield
        TT_ = Pt[1]
        tk = K("gPt1")
        bu = bank()
        for h in range(H):
            mm(p, PS[0:64, bu, h * 128:(h + 1) * 128], TT_[:, h, :], vb[:, h, :], True, True, r=(tk, K("gvb")),
               w=bk(bu))
        bw = bank()
        for h in range(H):
            mm(p, PS[:, bw, h * 64:(h + 1) * 64], kbg[:, h, :], TT_[:, h, :], True, True, r=(tk, K("gkbg")), w=bk(bw))
        p.op("act", lambda e: e.copy(usb[:, :, :], hd(PS[0:64, bu, :])), r=bk(bu), w=(K("gusb"),))
        p.op("act", lambda e: e.copy(wT[:, :, :], hj(PS[:, bw, 0:HC])), r=bk(bw), w=(K("gwT"),))
        yield
        bws = bank()
        for h in range(H):
            mm(p, PS[0:64, bws, h * 128:(h + 1) * 128], wT[:, h, :], Sb[:, h, :], True, True,
               r=(K("gwT"), K("gSb")), w=bk(bws))
        p.op("dve", lambda e: e.tensor_tensor(vnew[:, :, :], usb[:, :, :], hd(PS[0:64, bws, :]), ALU.subtract),
             r=bk(bws) + [K("gusb")], w=(K("gvnew"),))
        bo = bank()
        for h in range(H):
            oo = PS[0:64, bo, h * 128:(h + 1) * 128]
            mm(p, oo, qd[:, h, :], Sb[:, h, :], True, False, r=(K("gqd"), K("gSb")), w=bk(bo))
            mm(p, oo, intraT[:, h, :], vnew[:, h, :], False, True, r=(K("gintraT"), K("gvnew")), w=bk(bo))
        bd = bank()
        for h in range(H):
            mm(p, PS[:, bd, h * 128:(h + 1) * 128], kdec[:, h, :], vnew[:, h, :], True, True,
               r=(K("gkdec"), K("gvnew")), w=bk(bd))
        p.op("dve", lambda e: e.tensor_tensor(Sst[:, :, :], Sst[:, :, :],
                                              eg128[:, :, 63:64].to_broadcast([128, H, 128]), ALU.mult),
             r=(K("gS"), K("geg128")), w=(K("gS"),))
        p.op("dve", lambda e: e.tensor_tensor(Sst[:, :, :], Sst[:, :, :], hd(PS[:, bd, :]), ALU.add),
             r=bk(bd) + [K("gS")], w=(K("gS"),))
        p.op("act", lambda e: e.copy(Sb[:, :, :], Sst[:, :, :]), r=(K("gS"),), w=(K("gSb"),))
        p.op("act", lambda e: e.copy(osb[:, :, :], hd(PS[0:64, bo, :])), r=bk(bo), w=(K("gusb"),))
        yield
        bz = bank()
        for kc in range(8):
            mm(p, PS[0:64, bz, :], xt[tb][:, kc, cs], wz[:, kc, :], kc == 0, kc == 7, r=(K("gwz"), xk), w=bk(bz))
        p.op("act", lambda e: e.activation(zs[:, :, :], hd(PS[0:64, bz, :]), AF.Silu), r=bk(bz), w=(K("gzs"),))
        p.op("dve", lambda e: e.tensor_tensor(osq[:, :, :], osb[:, :, :], osb[:, :, :], ALU.mult), r=(K("gusb"),),
             w=(K("gosq"),))
        p.op("dve", lambda e: e.tensor_reduce(sm[:, 9, :], osq[:, :, :], AX.X, ALU.add), r=(K("gosq"),),
             w=(K("gsm9"),))
        p.op("dve", lambda e: e.tensor_scalar(sm[:, 9, :], sm[:, 9, :], 1.0 / 128.0, 1e-6, ALU.mult, ALU.add),
             r=(K("gsm9"),), w=(K("gsm9"),))
        p.op("act", lambda e: e.activation(sm[:, 9, :], sm[:, 9, :], AF.Sqrt), r=(K("gsm9"),), w=(K("gsm9"),))
        p.op("dve", lambda e: e.reciprocal(sm[:, 9, :], sm[:, 9, :]), r=(K("gsm9"),), w=(K("gsm9"),))
        p.op("dve", lambda e: e.tensor_tensor(osb[:, :, :], osb[:, :, :], bc3(sm[:, 9, :], 128), ALU.mult),
             r=(K("gusb"), K("gsm9")), w=(K("gusb"),))
        p.op("dve", lambda e: e.tensor_tensor(zs[:, :, :], zs[:, :, :],
                                              nw[:, :].unsqueeze(1).to_broadcast([64, H, 128]), ALU.mult),
             r=(K("gzs"), K("gnw")), w=(K("gzs"),))
        p.op("dve", lambda e: e.tensor_tensor(oa[:, :, :], osb[:, :, :], zs[:, :, :], ALU.mult),
             r=(K("gusb"), K("gzs")), w=(K("goa"),))
        bT = bank()
        PT = PS[:, bT, :].bitcast(BF16)
        ob = n % 2
        for h in range(H):
            p.op("pe", lambda e, h=h: e.transpose(PT[:, h * 64:(h + 1) * 64], oa[:, h, :], g.identb[0:64, 0:64]),
                 r=(K("goa"), "identb"), w=bk(bT))
        p.op("act", lambda e: e.copy(oaT[ob][:, :, :], hj(PT[:, 0:HC])), r=bk(bT), w=(K("goaT%d" % ob),))
        p.dma("sp", g.OAT[:, h0:h0 + H, n * C:(n + 1) * C], oaT[ob][:, :, :], r=(K("goaT%d" % ob),),
              w=((K("OAT"), n // 2),))
        yield


STAGE_W = {
    "init": [],
    "lru": ["w_in", "rg_conv_w", "rg_conv_b", "rg_w_a", "rg_b_a", "rg_w_x", "rg_b_x", "rg_lambda"],
    "swa": ["w_in", "attn_sinks", "rel_bias"],
    "gdn": ["w_in", "conv_qkv_w", "gdn_a_log", "gdn_dt_bias", "gdn_norm_w"],
    "merge": ["w_in", "w_o_gdn", "w_o_lru", "w_o_swa", "w_out", "ln1_g", "ln1_b"],
    "moe": ["router_w", "router_b", "w_gu", "b_gu", "w_down", "b_down", "ln2_g", "ln2_b"],
    "ple": ["ple_w_gate", "ple_w_proj", "ln3_g", "ln3_b"],
}


def needed_weights(stages):
    out = []
    for n in WEIGHT_SHAPES:
        if any(n in STAGE_W[s_] for s_ in stages):
            out.append(n)
    return out


class LN:
    def __init__(self, g, st, gname, bname, li, npst=1, tag=""):
        p = g.p
        self.g = g
        self.T = tag
        self.lg = p.sb(st, "lng", [128, D], F32)
        self.lb = p.sb(st, "lnb", [128, D], F32)
        p.dma("sp", self.lg[:, :], bcast_rows(g.W[gname], li * D, D, 128), w=("lng" + tag,))
        p.dma("sp", self.lb[:, :], bcast_rows(g.W[bname], li * D, D, 128), w=("lnb" + tag,))
        self.stats = p.sb(st, "lnst", [128, 2, 6], F32)
        self.mv = p.sb(st, "lnmv", [128, 2], F32)
        self.xo = [p.sb(st, "lnxo", [128, D], F32) for _ in range(2)]
        self.bufs = xt16_bufs(g, st, npst)
        self.i = 0

    def apply(self, z, zkey, t):
        g, p, T = self.g, self.g.p, self.T
        b = self.i % 2
        self.i += 1
        kst, kmv, klg, klb = "lnst" + T, "lnmv" + T, "lng" + T, "lnb" + T
        for hf in range(2):
            p.op("dve", lambda e: e.bn_stats(self.stats[:, hf, :], z[:, hf * 512:(hf + 1) * 512]), r=(zkey,),
                 w=(kst,))
        p.op("dve", lambda e: e.bn_aggr(self.mv[:, :], self.stats[:, :, :].rearrange("p a s -> p (a s)")),
             r=(kst,), w=(kmv,))
        p.op("act", lambda e: e.activation(self.mv[:, 1:2], self.mv[:, 1:2], AF.Sqrt, bias=1e-5), r=(kmv,), w=(kmv,))
        p.op("dve", lambda e: e.reciprocal(self.mv[:, 1:2], self.mv[:, 1:2]), r=(kmv,), w=(kmv,))
        xo = self.xo[b]
        xk = "lnxo%s%d" % (T, b)
        p.op("dve", lambda e: e.tensor_scalar(xo[:, :], z, self.mv[:, 0:1], self.mv[:, 1:2], ALU.subtract,
                                              ALU.mult), r=(zkey, kmv), w=(xk,))
        p.op("dve", lambda e: e.tensor_tensor(xo[:, :], xo[:, :], self.lg[:, :], ALU.mult), r=(xk, klg), w=(xk,))
        p.op("dve", lambda e: e.tensor_tensor(xo[:, :], xo[:, :], self.lb[:, :], ALU.add), r=(xk, klb), w=(xk,))
        p.dma("sp", g.X32[t * 128:(t + 1) * 128, :], xo[:, :], r=(xk,), w=(("X32", t),))
        to_xt16(g, None, xo[:, :], t, (xk,), self.bufs, self.i, tag=T)


def stage_merge(g, li):
    p = g.p
    W = g.W
    TT = 512
    with ExitStack() as st:
        wo = []
        for bi, n in enumerate(("w_o_gdn", "w_o_lru", "w_o_swa")):
            w_ = p.sb(st, "mwo", [128, 8, D], BF16)
            for kc in range(8):
                p.dma("pool", w_[:, kc, :], W[n][li, kc * 128:(kc + 1) * 128, :], w=("mwo%d" % bi,))
            wo.append(w_)
        wout = p.sb(st, "mwout", [128, 8, D], BF16)
        for kc in range(8):
            p.dma("pool", wout[:, kc, :], W["w_out"][li, kc * 128:(kc + 1) * 128, :], w=("mwout",))
        wg = [p.sb(st, "mwg", [128, 8, 128], BF16) for _ in range(2)]
        ln = LN(g, st, "ln1_g", "ln1_b", li, npst=1)
        xt = p.sb(st, "mxt", [128, 8, TT], BF16)
        ot = [p.sb(st, "mot", [128, 8, TT], BF16) for _ in range(3)]
        gs = [p.sb(st, "mgs", [128, TT], F32) for _ in range(2)]
        tmp = p.sb(st, "mtmp", [128, TT], F32)
        macc = p.sb(st, "macc", [128, TT], F32)
        mT = p.sb(st, "mmT", [128, 8, TT], BF16)
        x32 = [p.sb(st, "mx32", [128, D], F32) for _ in range(2)]
        z = [p.sb(st, "mz", [128, D], F32) for _ in range(2)]
        psg = [p.ps(st, "mpsg", [128, TT], F32) for _ in range(2)]
        psy = [p.ps(st, "mpsy", [128, TT], F32) for _ in range(2)]
        psm = p.ps(st, "mpsm", [128, 2, 512], F32)
        srcs = (("OAT", g.OAT), ("OBT", g.OBT), ("OCT", g.OCT))
        ig = 0
        for tt in range(S // TT):
            tk = [("XT16", tt * 4 + j) for j in range(4)]
            p.dma("sp", xt[:, :, :], g.XT16[:, :, tt * TT:(tt + 1) * TT], r=tk, w=("mxt",))
            for bi, (nm, src) in enumerate(srcs):
                p.dma("sp", ot[bi][:, :, :], src[:, :, tt * TT:(tt + 1) * TT],
                      r=[(nm, tt * 4 + j) for j in range(4)], w=("mot%d" % bi,))
            for c in range(8):
                for bi in range(3):
                    b = ig % 2
                    ig += 1
                    col = O_MG + bi * 1024 + c * 128
                    p.dma("pool", wg[b][:, :, :],
                          W["w_in"][li, :, col:col + 128].rearrange("(kc p) m -> p kc m", p=128), w=("mwg%d" % b,))
                    for kc in range(8):
                        mm(p, psg[b][:, :], wg[b][:, kc, :], xt[:, kc, :], kc == 0, kc == 7,
                           r=("mwg%d" % b, "mxt"), w=("mpsg%d" % b,))
                    for kc in range(8):
                        mm(p, psy[b][:, :], wo[bi][:, kc, c * 128:(c + 1) * 128], ot[bi][:, kc, :], kc == 0, kc == 7,
                           r=("mwo%d" % bi, "mot%d" % bi), w=("mpsy%d" % b,))
                    p.op("act", lambda e: e.activation(gs[b][:, :], psg[b][:, :], AF.Sigmoid), r=("mpsg%d" % b,),
                         w=("mgs%d" % b,))
                    if bi == 0:
                        p.op("dve", lambda e: e.tensor_tensor(macc[:, :], gs[b][:, :], psy[b][:, :], ALU.mult),
                             r=("mgs%d" % b, "mpsy%d" % b), w=("macc",))
                    else:
                        p.op("dve", lambda e: e.tensor_tensor(tmp[:, :], gs[b][:, :], psy[b][:, :], ALU.mult),
                             r=("mgs%d" % b, "mpsy%d" % b), w=("mtmp",))
                        if bi == 1:
                            p.op("dve", lambda e: e.tensor_tensor(macc[:, :], macc[:, :], tmp[:, :], ALU.add),
                                 r=("macc", "mtmp"), w=("macc",))
                        else:
                            p.op("dve", lambda e: e.tensor_tensor(mT[:, c, :], macc[:, :], tmp[:, :], ALU.add),
                                 r=("macc", "mtmp"), w=("mmT",))
            for nb in range(4):
                t = tt * 4 + nb
                b = t % 2
                p.dma("sp", x32[b][:, :], g.X32[t * 128:(t + 1) * 128, :], r=(("X32", t),), w=("mx32%d" % b,))
                for hf in range(2):
                    for kc in range(8):
                        mm(p, psm[:, hf, :], mT[:, kc, nb * 128:(nb + 1) * 128], wout[:, kc, hf * 512:(hf + 1) * 512],
                           kc == 0, kc == 7, r=("mmT", "mwout"), w=("mpsm",))
                p.op("dve", lambda e: e.scalar_tensor_tensor(z[b][:, :], x32[b][:, :], ALPHA,
                                                             psm[:, :, :].rearrange("p a f -> p (a f)"), ALU.mult,
                                                             ALU.add), r=("mx32%d" % b, "mpsm"), w=("mz%d" % b,))
                ln.apply(z[b][:, :], "mz%d" % b, t)
        p.barrier()


def stage_moe(g, li, nexp=NE, fuse_ple=False):
    p = g.p
    W = g.W
    cst = g.cst
    NG = 2
    GS = CAP // NG
    NB = CAP // 128
    with ExitStack() as st0:
        dest = st0.enter_context(g.nc.sbuf_tensor(p.name("qdest"), [128, NT, 4], I32))
        gate4 = st0.enter_context(g.nc.sbuf_tensor(p.name("qgate"), [128, NT, 4], F32))
        with ExitStack() as st:
            zt = p.sb(st, "qz", [128, D], BF16)
            p.op("dve", lambda e: e.memset(zt[:, :], 0.0), w=("qz",))
            for r0 in range(0, NE * CAP, 1024):
                p.dma("sp", g.XBUF[r0:r0 + 1024, :].rearrange("(a p) d -> p a d", p=128),
                      zt[:, :].unsqueeze(1).to_broadcast([128, 8, D]), r=("qz",), w=("XBUF",))
            rw = p.sb(st, "qrw", [128, 8, NE], F32)
            p.dma("sp", rw[:, :, :], W["router_w"][li].rearrange("(kc p) e -> p kc e", p=128), w=("qrw",))
            rb = p.sb(st, "qrb", [1, NE], F32)
            p.dma("sp", rb[:, :], W["router_b"][li:li + 1, :], w=("qrb",))
            cnt = p.sb(st, "qcnt", [1, NE], F32)
            p.op("dve", lambda e: e.memset(cnt[:, :], 0.0), w=("qcnt",))
            x32 = [p.sb(st, "qx32", [128, D], F32) for _ in range(2)]
            xb = [p.sb(st, "qxb", [128, D], BF16) for _ in range(2)]
            xTf = p.sb(st, "qxTf", [128, 8, 128], F32)
            lg = p.sb(st, "qlg", [128, NE], F32)
            top8 = p.sb(st, "qtop8", [128, 8], F32)
            mask = p.sb(st, "qmask", [128, NE], F32)
            vv = p.sb(st, "qvv", [128, NE], F32)
            junk = p.sb(st, "qjunk", [128, NE], F32)
            d4f = p.sb(st, "qd4f", [128, 4], F32)
            sm = p.sb(st, "qsm", [128, 4], F32)
            pT = p.ps(st, "qpT", [128, 2, 512], F32)
            pl = p.ps(st, "qpl", [128, NE], F32)
            pp = p.ps(st, "qpp", [128, NE], F32)
            pc = p.ps(st, "qpc", [1, NE], F32)
            ones1 = cst[0:1, C_ONES:C_ONES + 128]
            for t in range(NT):
                b = t % 2
                xk = "qx32%d" % b
                p.dma("sp", x32[b][:, :], g.X32[t * 128:(t + 1) * 128, :], r=(("X32", t),), w=(xk,))
                p.op("act", lambda e: e.copy(xb[b][:, :], x32[b][:, :]), r=(xk,), w=("qxb%d" % b,))
                for c in range(8):
                    p.op("pe", lambda e, c=c: e.transpose(pT[:, c // 4, (c % 4) * 128:(c % 4 + 1) * 128],
                                                          x32[b][:, c * 128:(c + 1) * 128], g.ident),
                         r=(xk, "cst"), w=("qpT",))
                p.op("dve", lambda e: e.tensor_copy(xTf[:, :, :], pT[:, :, :].rearrange("p a (c t) -> p (a c) t", c=4)),
                     r=("qpT",), w=("qxTf",))
                for kc in range(8):
                    mm(p, pl[:, :], xTf[:, kc, :], rw[:, kc, :], kc == 0, False, r=("qxTf", "qrw"), w=("qpl",))
                mm(p, pl[:, :], ones1, rb[:, :], False, True, r=("cst", "qrb"), w=("qpl",))
                p.op("act", lambda e: e.copy(lg[:, :], pl[:, :]), r=("qpl",), w=("qlg",))
                p.op("dve", lambda e: e.max(top8[:, :], lg[:, :]), r=("qlg",), w=("qtop8",))
                p.op("dve", lambda e: e.tensor_scalar(mask[:, :], lg[:, :], top8[:, 3:4], None, ALU.is_ge),
                     r=("qlg", "qtop8"), w=("qmask",))
                p.op("dve", lambda e: e.tensor_scalar(sm[:, :], top8[:, 0:4], top8[:, 0:1], None, ALU.subtract),
                     r=("qtop8",), w=("qsm",))
                p.op("act", lambda e: e.activation(sm[:, :], sm[:, :], AF.Exp), r=("qsm",), w=("qsm",))
                p.op("dve", lambda e: e.tensor_reduce(d4f[:, 0:1], sm[:, :], AX.X, ALU.add), r=("qsm",), w=("qd4f",))
                p.op("dve", lambda e: e.reciprocal(d4f[:, 0:1], d4f[:, 0:1]), r=("qd4f",), w=("qd4f",))
                p.op("dve", lambda e: e.tensor_scalar(gate4[:, t, :], sm[:, :], d4f[:, 0:1], None, ALU.mult),
                     r=("qsm", "qd4f"), w=("qgate",))
                mm(p, pp[:, :], cst[:, C_TRI128:C_TRI128 + 128], mask[:, :], True, False, r=("cst", "qmask"), w=("qpp",))
                mm(p, pp[:, :], ones1, cnt[:, :], False, True, r=("cst", "qcnt"), w=("qpp",))
                mm(p, pc[:, :], cst[:, C_ONES:C_ONES + 1], mask[:, :], True, True, r=("cst", "qmask"), w=("qpc",))
                p.op("dve", lambda e: e.tensor_tensor(vv[:, :], pp[:, :], cst[:, C_EBASE:C_EBASE + NE], ALU.add),
                     r=("qpp", "cst"), w=("qvv",))
                p.op("dve", lambda e: e.tensor_tensor(cnt[:, :], cnt[:, :], pc[:, :], ALU.add), r=("qcnt", "qpc"),
                     w=("qcnt",))
                for k in range(4):
                    p.op("dve", lambda e, k=k: e.scalar_tensor_tensor(junk[:, :], lg[:, :], top8[:, k:k + 1], vv[:, :],
                                                                       ALU.is_equal, ALU.mult,
                                                                       accum_out=d4f[:, k:k + 1]),
                         r=("qlg", "qtop8", "qvv", "qd4f"), w=("qjunk", "qd4f"))
                p.op("dve", lambda e: e.tensor_copy(dest[:, t, :], d4f[:, :]), r=("qd4f",), w=("qdest",))
                for k in range(4):
                    p.dma_custom("pool", lambda e, k=k: e.indirect_dma_start(
                        out=g.XBUF[:, :], out_offset=bass.IndirectOffsetOnAxis(ap=dest[:, t, k:k + 1], axis=0),
                        in_=xb[b][:, :], in_offset=None),
                        r=("qxb%d" % b, "qdest"), w=("XBUF",))
            p.barrier()
        with ExitStack() as st:
            wgu = [p.sb(st, "qwgu", [128, 8, 2 * D], BF16) for _ in range(2)]
            wdn = [p.sb(st, "qwdn", [128, 8, D], BF16) for _ in range(2)]
            bgu = [p.sb(st, "qbgu", [1, 2 * D], BF16) for _ in range(2)]
            bdn = [p.sb(st, "qbdn", [1, D], BF16) for _ in range(2)]
            xin = [p.sb(st, "qxin", [128, D], BF16) for _ in range(2)]
            xbT = p.sb(st, "qxbT", [128, 8, CAP], BF16)
            aT = p.sb(st, "qaT", [128, 8, CAP], BF16)
            ta = [p.sb(st, "qta", [128, GS], F32) for _ in range(2)]
            tsg = [p.sb(st, "qtsg", [128, GS], F32) for _ in range(2)]
            tl = [p.sb(st, "qtl", [128, GS], F32) for _ in range(2)]
            osb = [p.sb(st, "qosb", [128, D], F32) for _ in range(2)]
            ptr = [p.ps(st, "qptr", [128, D], BF16) for _ in range(2)]
            pg = [p.ps(st, "qpg", [128, GS], F32) for _ in range(2)]
            pln = [p.ps(st, "qpln", [128, GS], F32) for _ in range(2)]
            pd = p.ps(st, "qpd", [128, 2, 512], F32)
            onesb1 = g.onesb[0:1, :]
            ix = 0
            ia = 0
            io = 0
            for ei in range(nexp):
                wb = ei % 2
                kw = ("qwgu%d" % wb, "qwdn%d" % wb, "qbgu%d" % wb, "qbdn%d" % wb)
                for kc in range(8):
                    p.dma("pool", wgu[wb][:, kc, :], W["w_gu"][li, ei, kc * 128:(kc + 1) * 128, :], w=(kw[0],))
                    p.dma("pool", wdn[wb][:, kc, :], W["w_down"][li, ei, kc * 128:(kc + 1) * 128, :], w=(kw[1],))
                p.dma("pool", bgu[wb][:, :], W["b_gu"][li, ei:ei + 1, :], w=(kw[2],))
                p.dma("pool", bdn[wb][:, :], W["b_down"][li, ei:ei + 1, :], w=(kw[3],))
                for blk in range(NB):
                    b = ix % 2
                    ix += 1
                    r0 = ei * CAP + blk * 128
                    p.dma("sp", xin[b][:, :], g.XBUF[r0:r0 + 128, :], r=("XBUF",), w=("qxin%d" % b,))
                    for c in range(8):
                        p.op("pe", lambda e, c=c: e.transpose(ptr[b][:, c * 128:(c + 1) * 128],
                                                              xin[b][:, c * 128:(c + 1) * 128], g.identb[:, :]),
                             r=("qxin%d" % b, "identb"), w=("qptr%d" % b,))
                    p.op("act", lambda e: e.copy(xbT[:, :, blk * 128:(blk + 1) * 128],
                                                 ptr[b][:, :].rearrange("p (c t) -> p c t", c=8)),
                         r=("qptr%d" % b,), w=("qxbT",))
                for j in range(8):
                    for gi in range(NG):
                        b = ia % 2
                        ia += 1
                        sl = slice(gi * GS, (gi + 1) * GS)
                        for two, pst_, key in ((0, pg[b], "qpg%d" % b), (1, pln[b], "qpln%d" % b)):
                            c0 = 256 * j + two
                            for kc in range(8):
                                mm(p, pst_[:, :], wgu[wb][:, kc, c0:c0 + 255:2], xbT[:, kc, sl], kc == 0, False,
                                   r=(kw[0], "qxbT"), w=(key,))
                            mm(p, pst_[:, :], bgu[wb][0:1, c0:c0 + 255:2], g.onesb[0:1, 0:GS], False, True,
                               r=(kw[2], "onesb"), w=(key,))
                        p.op("dve", lambda e: e.tensor_scalar(ta[b][:, :], pg[b][:, :], 7.0, None, ALU.min),
                             r=("qpg%d" % b,), w=("qta%d" % b,))
                        p.op("act", lambda e: e.activation(tsg[b][:, :], ta[b][:, :], AF.Sigmoid, scale=1.702),
                             r=("qta%d" % b,), w=("qtsg%d" % b,))
                        p.op("dve", lambda e: e.tensor_scalar(tl[b][:, :], pln[b][:, :], -7.0, 7.0, ALU.max, ALU.min),
                             r=("qpln%d" % b,), w=("qtl%d" % b,))
                        p.op("dve", lambda e: e.tensor_tensor(ta[b][:, :], ta[b][:, :], tsg[b][:, :], ALU.mult),
                             r=("qta%d" % b, "qtsg%d" % b), w=("qta%d" % b,))
                        p.op("dve", lambda e: e.scalar_tensor_tensor(aT[:, j, sl], tl[b][:, :], 1.0, ta[b][:, :],
                                                                      ALU.add, ALU.mult),
                             r=("qtl%d" % b, "qta%d" % b), w=("qaT",))
                for blk in range(NB):
                    b = io % 2
                    io += 1
                    for hf in range(2):
                        for kc in range(8):
                            mm(p, pd[:, hf, :], aT[:, kc, blk * 128:(blk + 1) * 128],
                               wdn[wb][:, kc, hf * 512:(hf + 1) * 512], kc == 0, False, r=("qaT", kw[1]), w=("qpd",))
                        mm(p, pd[:, hf, :], g.onesb[0:1, 0:128], bdn[wb][0:1, hf * 512:(hf + 1) * 512], False, True,
                           r=("onesb", kw[3]), w=("qpd",))
                    p.op("act", lambda e: e.copy(osb[b][:, :], pd[:, :, :].rearrange("p a f -> p (a f)")),
                         r=("qpd",), w=("qosb%d" % b,))
                    r0 = ei * CAP + blk * 128
                    p.dma("sp", g.OBUF[r0:r0 + 128, :], osb[b][:, :], r=("qosb%d" % b,), w=("OBUF",))
            p.barrier()
        if fuse_ple:
            run_streams(p, [stream_combine(g, li, dest, gate4), stream_ple(g, li)], lags=[0.0, 2.5 / NT])
        else:
            run_streams(p, [stream_combine(g, li, dest, gate4)])


def stream_combine(g, li, dest, gate4):
    p = g.p
    with ExitStack() as st:
        ln = LN(g, st, "ln2_g", "ln2_b", li, npst=1, tag="c")
        x32 = [p.sb(st, "qcx", [128, D], F32) for _ in range(2)]
        gt = [p.sb(st, "qgt", [128, D], F32) for _ in range(4)]
        z = [p.sb(st, "qcz", [128, D], F32) for _ in range(2)]
        ig = 0
        yield
        for t in range(NT):
            b = t % 2
            p.dma("sp", x32[b][:, :], g.X32[t * 128:(t + 1) * 128, :], r=(("X32", t),), w=("qcx%d" % b,))
            p.op("act", lambda e: e.activation(z[b][:, :], x32[b][:, :], AF.Copy, scale=ALPHA),
                 r=("qcx%d" % b,), w=("qcz%d" % b,))
            for k in range(4):
                gb = ig % 4
                ig += 1
                p.dma_custom("pool", lambda e, k=k: e.indirect_dma_start(
                    out=gt[gb][:, :], out_offset=None, in_=g.OBUF[:, :],
                    in_offset=bass.IndirectOffsetOnAxis(ap=dest[:, t, k:k + 1], axis=0)),
                    r=("OBUF", "qdest"), w=("qgt%d" % gb,))
                p.op("dve", lambda e, k=k: e.scalar_tensor_tensor(z[b][:, :], gt[gb][:, :], gate4[:, t, k:k + 1],
                                                                   z[b][:, :], ALU.mult, ALU.add),
                     r=("qgt%d" % gb, "qgate", "qcz%d" % b), w=("qcz%d" % b,))
            ln.apply(z[b][:, :], "qcz%d" % b, t)


def stream_ple(g, li):
    p = g.p
    W = g.W
    with ExitStack() as st:
        wg = p.sb(st, "pwg", [128, 8, D], BF16)
        for kc in range(8):
            p.dma("pool", wg[:, kc, :], W["ple_w_gate"][li, kc * 128:(kc + 1) * 128, :], w=("pwg",))
        wp = p.sb(st, "pwp", [128, 2, D], BF16)
        for kc in range(2):
            p.dma("pool", wp[:, kc, :], W["ple_w_proj"][li, kc * 128:(kc + 1) * 128, :], w=("pwp",))
        ln = LN(g, st, "ln3_g", "ln3_b", li, npst=1, tag="p")
        xt = [p.sb(st, "pxt", [128, 8, 128], BF16) for _ in range(2)]
        x32 = [p.sb(st, "px32", [128, D], F32) for _ in range(2)]
        pin = [p.sb(st, "ppin", [128, 256], F32) for _ in range(2)]
        pinb = [p.sb(st, "ppinb", [128, 256], BF16) for _ in range(2)]
        pT = [p.sb(st, "ppT", [128, 2, 128], BF16) for _ in range(2)]
        sg = [p.sb(st, "psg", [128, D], F32) for _ in range(2)]
        z = [p.sb(st, "pz", [128, D], F32) for _ in range(2)]
        psg = p.ps(st, "ppsg", [128, 2, 512], F32)
        psp = p.ps(st, "ppsp", [128, 2, 512], F32)
        pst = p.ps(st, "ppst", [128, 256], BF16)
        yield
        for t in range(NT):
            b = t % 2
            kb = str(b)
            p.dma("sp", xt[b][:, :, :], g.XT16[:, :, t * 128:(t + 1) * 128], r=(("XT16", t),), w=("pxt" + kb,))
            p.dma("sp", x32[b][:, :], g.X32[t * 128:(t + 1) * 128, :], r=(("X32", t),), w=("px32" + kb,))
            p.dma("sp", pin[b][:, :], g.pp[li, t * 128:(t + 1) * 128, :], w=("ppin" + kb,))
            p.op("act", lambda e: e.copy(pinb[b][:, :], pin[b][:, :]), r=("ppin" + kb,), w=("ppinb" + kb,))
            for c in range(2):
                p.op("pe", lambda e, c=c: e.transpose(pst[:, c * 128:(c + 1) * 128], pinb[b][:, c * 128:(c + 1) * 128],
                                                      g.identb[:, :]), r=("ppinb" + kb, "identb"), w=("ppst",))
            p.op("dve", lambda e: e.tensor_copy(pT[b][:, :, :], pst[:, :].rearrange("p (c t) -> p c t", c=2)),
                 r=("ppst",), w=("ppT" + kb,))
            for hf in range(2):
                for kc in range(8):
                    mm(p, psg[:, hf, :], xt[b][:, kc, :], wg[:, kc, hf * 512:(hf + 1) * 512], kc == 0, kc == 7,
                       r=("pxt" + kb, "pwg"), w=("ppsg",))
            for hf in range(2):
                for kc in range(2):
                    mm(p, psp[:, hf, :], pT[b][:, kc, :], wp[:, kc, hf * 512:(hf + 1) * 512], kc == 0, kc == 1,
                       r=("ppT" + kb, "pwp"), w=("ppsp",))
            p.op("act", lambda e: e.activation(sg[b][:, :], psg[:, :, :].rearrange("p a f -> p (a f)"), AF.Sigmoid),
                 r=("ppsg",), w=("psg" + kb,))
            p.op("dve", lambda e: e.tensor_tensor(sg[b][:, :], sg[b][:, :], psp[:, :, :].rearrange("p a f -> p (a f)"),
                                                  ALU.mult), r=("psg" + kb, "ppsp"), w=("psg" + kb,))
            p.op("dve", lambda e: e.scalar_tensor_tensor(z[b][:, :], x32[b][:, :], ALPHA, sg[b][:, :], ALU.mult,
                                                         ALU.add), r=("px32" + kb, "psg" + kb), w=("pz" + kb,))
            ln.apply(z[b][:, :], "pz" + kb, t)


def stage_ple(g, li):
    run_streams(g.p, [stream_ple(g, li)])


ENABLED_STAGES = ("init", "lru", "swa", "gdn", "merge", "moe", "ple")


def kernel(**inputs):
    n = 8
    nc, g = build_program(nlayers=DEPTH, stages=ENABLED_STAGES)
    consts = make_consts()
    in_maps = []
    for c in range(n):
        m = {"x": np.ascontiguousarray(inputs["x"][c], dtype=np.float32), "consts": consts}
        if "ple" in ENABLED_STAGES:
            m["p"] = np.ascontiguousarray(np.asarray(inputs["p"])[:, c], dtype=np.float32)
        for name in needed_weights(ENABLED_STAGES):
            m[name] = np.ascontiguousarray(inputs[name], dtype=np.float32)
        in_maps.append(m)
    res = run_bass_kernel_spmd(nc, in_maps, core_ids=list(range(n)))
    return np.stack([np.asarray(r["out"], dtype=np.float32) for r in res.results], axis=0)
```
